# Optimizing a Trainium2 kernel written in Bass

```python
import jax, jax.numpy as jnp
from jax import lax
import numpy as np

D_MODEL = 1024
BATCH = 8
SEQ = 2048
DEPTH = 1
DEC_BATCH = 128
DEC_SEQ = 4
PAST_LEN = 16384
PAGE_SIZE = 128

HEAD_SIZE = 64
D_RWKV = D_MODEL
N_HEADS = D_RWKV // HEAD_SIZE
D_DECAY_LORA = 64
D_AAA_LORA = 64
D_GATE_LORA = 160
D_CONV = D_MODEL
CONV_WIDTH = 31
N_EXPERTS = 64
TOP_K = 6
D_EXPERT = 256
D_SHARED = 256
ROUTED_SCALE = 2.5
RMS_EPS = 1e-6
LN_EPS = 1e-5
GN_EPS = HEAD_SIZE * 1e-5
N_SHIFT = 3 * D_RWKV + D_DECAY_LORA + D_AAA_LORA + D_GATE_LORA
N_IN = N_SHIFT + 2 * D_CONV + 2 * D_MODEL

kernel_name = "rwkv7_conformer_moe_hybrid_step"


def rmsnorm(x, g):
    xf = x.astype(jnp.float32)
    y = xf * lax.rsqrt(jnp.mean(xf * xf, axis=-1, keepdims=True) + RMS_EPS)
    return (y * g.astype(jnp.float32)).astype(x.dtype)


def layernorm(x, g, b, eps):
    xf = x.astype(jnp.float32)
    mu = jnp.mean(xf, axis=-1, keepdims=True)
    var = jnp.mean(jnp.square(xf - mu), axis=-1, keepdims=True)
    y = (xf - mu) * lax.rsqrt(var + eps)
    return y * g.astype(jnp.float32) + b.astype(jnp.float32)


def wkv7_scan(S0, r, w, k, v, kk, a):
    tmajor = lambda t: jnp.moveaxis(t.astype(jnp.float32), 1, 0)

    def step(S, inp):
        rt, wt, kt, vt, kkt, at = inp
        sa = jnp.einsum('bhvk,bhk->bhv', S, -kkt)
        S = (S * wt[:, :, None, :] + sa[..., None] * (kkt * at)[:, :, None, :]
             + vt[..., None] * kt[:, :, None, :])
        return S, jnp.einsum('bhvk,bhk->bhv', S, rt)

    S, y = lax.scan(step, S0.astype(jnp.float32),
                    (tmajor(r), tmajor(w), tmajor(k), tmajor(v), tmajor(kk), tmajor(a)))
    return S, jnp.moveaxis(y, 0, 1)


def hybrid_layer(x, c, wkv0, shift0, conv0, p):
    B, T, _ = x.shape
    dt = x.dtype
    mod = (jax.nn.silu(c) @ p['w_ada'] + p['b_ada'])[:, None, :]
    sh_m, sc_m, gt_m, sh_f, sc_f, gt_f = jnp.split(mod, 6, axis=-1)

    h = rmsnorm(x, p['mix_pre_g']) * (1 + sc_m) + sh_m
    proj = h @ p['w_in']
    p_rw, p_cv, p_gt = jnp.split(proj, [N_SHIFT, N_SHIFT + 2 * D_CONV], axis=-1)

    prev = jnp.concatenate([shift0[:, None, :].astype(dt), p_rw[:, :-1]], axis=1)
    xs = p_rw + (prev - p_rw) * p['mu_shift']
    r, k, v, xw, xa, xg = jnp.split(
        xs, [D_RWKV, 2 * D_RWKV, 3 * D_RWKV, 3 * D_RWKV + D_DECAY_LORA,
             3 * D_RWKV + D_DECAY_LORA + D_AAA_LORA], axis=-1)
    w_log = -jax.nn.softplus(-(p['w0'] + jnp.tanh(xw) @ p['w_decay2'])) - 0.5
    decay = jnp.exp(-jnp.exp(w_log.astype(jnp.float32)))
    a = jax.nn.sigmoid(p['a0'] + xa @ p['w_aaa2'])
    g = jax.nn.sigmoid(xg) @ p['w_gate2']
    heads = lambda t: t.reshape(B, T, N_HEADS, HEAD_SIZE)
    kk = heads((k * p['k_k']).astype(jnp.float32))
    kk = kk / jnp.maximum(jnp.linalg.norm(kk, axis=-1, keepdims=True), 1e-12)
    k = k * (1 + (a - 1) * p['k_a'])
    rh, kh, vh = heads(r), heads(k), heads(v)
    wkv, y = wkv7_scan(wkv0, rh, heads(decay), kh, vh, kk, heads(a))
    mu = jnp.mean(y, axis=-1, keepdims=True)
    var = jnp.mean(jnp.square(y - mu), axis=-1, keepdims=True)
    y = ((y - mu) * lax.rsqrt(var + GN_EPS)).reshape(B, T, D_RWKV)
    y = y * p['gn_g'].astype(jnp.float32) + p['gn_b'].astype(jnp.float32)
    bonus = jnp.sum(rh.astype(jnp.float32) * kh.astype(jnp.float32) * p['r_k'].astype(jnp.float32),
                    axis=-1, keepdims=True) * vh.astype(jnp.float32)
    y = y + bonus.reshape(B, T, D_RWKV)
    y_a = (y.astype(dt) * g) @ p['w_o_rwkv']

    glu = p_cv[..., :D_CONV] * jax.nn.sigmoid(p_cv[..., D_CONV:])
    padded = jnp.concatenate([conv0.astype(dt), glu], axis=1)
    dw = lax.conv_general_dilated(
        padded, p['dw_w'][:, None, :], window_strides=(1,), padding='VALID',
        dimension_numbers=('NWC', 'WIO', 'NWC'), feature_group_count=D_CONV) + p['dw_b']
    new_conv = padded[:, -(CONV_WIDTH - 1):]
    u = jax.nn.silu(layernorm(dw, p['conv_ln_g'], p['conv_ln_b'], LN_EPS)).astype(dt)
    y_b = u @ p['w_conv_out']

    g_a, g_b = jnp.split(p_gt, 2, axis=-1)
    merged = jax.nn.sigmoid(g_a) * y_a + jax.nn.sigmoid(g_b) * y_b
    x = x + gt_m * rmsnorm(merged @ p['w_out'], p['mix_post_g'])

    hf = rmsnorm(x, p['ffn_pre_g']) * (1 + sc_f) + sh_f
    t = hf.reshape(B * T, D_MODEL)
    scores = jax.nn.sigmoid((t @ p['w_router']).astype(jnp.float32))
    _, idx = lax.top_k(scores + p['router_bias'].astype(jnp.float32), TOP_K)
    sel = jnp.take_along_axis(scores, idx, axis=-1)
    wts = sel / jnp.sum(sel, axis=-1, keepdims=True) * ROUTED_SCALE
    gates = jnp.einsum('tk,tke->te', wts, jax.nn.one_hot(idx, N_EXPERTS, dtype=jnp.float32)).astype(dt)
    out = (jax.nn.silu(t @ p['w_sh_gate']) * (t @ p['w_sh_up'])) @ p['w_sh_down']
    for e in range(N_EXPERTS):
        he = jax.nn.silu(t @ p['w_exp_gate'][e]) * (t @ p['w_exp_up'][e])
        out = out + gates[:, e:e + 1] * (he @ p['w_exp_down'][e])
    x = x + gt_f * rmsnorm(out.reshape(B, T, D_MODEL), p['ffn_post_g'])
    return (x, wkv.astype(wkv0.dtype), p_rw[:, -1].astype(shift0.dtype),
            new_conv.astype(conv0.dtype))


def setup_inputs(seed: int = 0) -> dict:
    key = jax.random.key(seed)
    ks = iter(jax.random.split(key, 48))
    nrm = lambda shape, s: jax.random.normal(next(ks), shape, jnp.float32) * s
    gain = lambda n: 1.0 + nrm((n,), 0.05)
    return {
        'x_prompt': nrm((BATCH, SEQ, D_MODEL), 1.0),
        'x_sample': nrm((DEC_BATCH, DEC_SEQ, D_MODEL), 1.0),
        'state_wkv': nrm((DEC_BATCH, N_HEADS, HEAD_SIZE, HEAD_SIZE), 1.0),
        'state_shift': nrm((DEC_BATCH, N_SHIFT), 1.0),
        'state_conv': nrm((DEC_BATCH, CONV_WIDTH - 1, D_CONV), 0.5),
        'c_prompt': nrm((BATCH, D_MODEL), 1.0),
        'c_sample': nrm((DEC_BATCH, D_MODEL), 1.0),
        'w_ada': nrm((D_MODEL, 6 * D_MODEL), 0.5 * D_MODEL ** -0.5),
        'b_ada': nrm((6 * D_MODEL,), 0.02),
        'mix_pre_g': gain(D_MODEL),
        'mix_post_g': gain(D_MODEL),
        'w_in': nrm((D_MODEL, N_IN), D_MODEL ** -0.5),
        'mu_shift': jax.random.uniform(next(ks), (N_SHIFT,), jnp.float32),
        'w0': jax.random.uniform(next(ks), (D_RWKV,), jnp.float32, minval=-6.5, maxval=-1.5),
        'w_decay2': nrm((D_DECAY_LORA, D_RWKV), 0.1 * D_DECAY_LORA ** -0.5),
        'a0': nrm((D_RWKV,), 0.3),
        'w_aaa2': nrm((D_AAA_LORA, D_RWKV), D_AAA_LORA ** -0.5),
        'w_gate2': nrm((D_GATE_LORA, D_RWKV), D_GATE_LORA ** -0.5),
        'k_k': 0.85 + nrm((D_RWKV,), 0.05),
        'k_a': gain(D_RWKV),
        'r_k': nrm((N_HEADS, HEAD_SIZE), 0.1),
        'gn_g': gain(D_RWKV),
        'gn_b': nrm((D_RWKV,), 0.02),
        'w_o_rwkv': nrm((D_RWKV, D_MODEL), D_RWKV ** -0.5),
        'dw_w': nrm((CONV_WIDTH, D_CONV), CONV_WIDTH ** -0.5),
        'dw_b': nrm((D_CONV,), 0.02),
        'conv_ln_g': gain(D_CONV),
        'conv_ln_b': nrm((D_CONV,), 0.02),
        'w_conv_out': nrm((D_CONV, D_MODEL), D_CONV ** -0.5),
        'w_out': nrm((D_MODEL, D_MODEL), D_MODEL ** -0.5),
        'ffn_pre_g': gain(D_MODEL),
        'ffn_post_g': gain(D_MODEL),
        'w_router': nrm((D_MODEL, N_EXPERTS), D_MODEL ** -0.5),
        'router_bias': nrm((N_EXPERTS,), 0.01),
        'w_exp_gate': nrm((N_EXPERTS, D_MODEL, D_EXPERT), D_MODEL ** -0.5),
        'w_exp_up': nrm((N_EXPERTS, D_MODEL, D_EXPERT), D_MODEL ** -0.5),
        'w_exp_down': nrm((N_EXPERTS, D_EXPERT, D_MODEL), D_EXPERT ** -0.5),
        'w_sh_gate': nrm((D_MODEL, D_SHARED), D_MODEL ** -0.5),
        'w_sh_up': nrm((D_MODEL, D_SHARED), D_MODEL ** -0.5),
        'w_sh_down': nrm((D_SHARED, D_MODEL), D_SHARED ** -0.5),
    }


def reference(x_prompt, x_sample, state_wkv, state_shift, state_conv, c_prompt, c_sample,
              w_ada, b_ada, mix_pre_g, mix_post_g, w_in, mu_shift, w0, w_decay2, a0, w_aaa2,
              w_gate2, k_k, k_a, r_k, gn_g, gn_b, w_o_rwkv, dw_w, dw_b, conv_ln_g, conv_ln_b,
              w_conv_out, w_out, ffn_pre_g, ffn_post_g, w_router, router_bias, w_exp_gate,
              w_exp_up, w_exp_down, w_sh_gate, w_sh_up, w_sh_down):
    p = {'w_ada': w_ada, 'b_ada': b_ada, 'mix_pre_g': mix_pre_g, 'mix_post_g': mix_post_g,
         'w_in': w_in, 'mu_shift': mu_shift, 'w0': w0, 'w_decay2': w_decay2, 'a0': a0,
         'w_aaa2': w_aaa2, 'w_gate2': w_gate2, 'k_k': k_k, 'k_a': k_a, 'r_k': r_k,
         'gn_g': gn_g, 'gn_b': gn_b, 'w_o_rwkv': w_o_rwkv, 'dw_w': dw_w, 'dw_b': dw_b,
         'conv_ln_g': conv_ln_g, 'conv_ln_b': conv_ln_b, 'w_conv_out': w_conv_out,
         'w_out': w_out, 'ffn_pre_g': ffn_pre_g, 'ffn_post_g': ffn_post_g,
         'w_router': w_router, 'router_bias': router_bias, 'w_exp_gate': w_exp_gate,
         'w_exp_up': w_exp_up, 'w_exp_down': w_exp_down, 'w_sh_gate': w_sh_gate,
         'w_sh_up': w_sh_up, 'w_sh_down': w_sh_down}
    Bp = x_prompt.shape[0]
    dt = x_prompt.dtype
    yp, sp = x_prompt, x_sample
    for _ in range(DEPTH):
        yp, wkv_p, shift_p, conv_p = hybrid_layer(
            yp, c_prompt,
            jnp.zeros((Bp, N_HEADS, HEAD_SIZE, HEAD_SIZE), dt),
            jnp.zeros((Bp, N_SHIFT), dt),
            jnp.zeros((Bp, CONV_WIDTH - 1, D_CONV), dt), p)
        sp, wkv_s, shift_s, conv_s = hybrid_layer(
            sp, c_sample, state_wkv, state_shift, state_conv, p)
    return (yp, sp, wkv_p, shift_p, conv_p, wkv_s, shift_s, conv_s)
```

```python
import numpy as np
from contextlib import ExitStack
import concourse.bass as bass
import concourse.mybir as mybir
from concourse.bass_utils import run_bass_kernel_spmd

F32 = mybir.dt.float32
BF16 = mybir.dt.bfloat16
AF = mybir.ActivationFunctionType
ALU = mybir.AluOpType
AX = mybir.AxisListType

NCORES = 8
D = 1024
SEQ = 2048
NS = 16
TS = 4
NTS = NS * TS
TT = SEQ + NTS
NSHIFT = 3360
NIN = 7456
NE = 64
DE = 256
HS = 64
NH = 16
CW = 31
BLOCKS = [(i * 512, 512, False) for i in range(4)] + [(SEQ, NTS, True)]

PCOLS = {}
_off = 0
for _n, _c in [("b_ada", 48), ("mix_pre_g", 8), ("mix_post_g", 8), ("ffn_pre_g", 8), ("ffn_post_g", 8),
               ("mu", 27), ("w0", 8), ("a0", 8), ("k_k", 8), ("k_a", 8), ("r_k", 8), ("gn_g", 8), ("gn_b", 8),
               ("dw_b", 8), ("ln_g", 8), ("ln_b", 8), ("dw_w", 8 * CW), ("rbias", 1)]:
    PCOLS[_n] = (_off, _c)
    _off += _c
NPC = _off

CCOLS = {}
_off = 0
for _n, _c in [("ident", 128), ("blk1", 128), ("m_su", 128), ("m_ui", 128), ("m_sl", 128), ("ones", 128),
               ("bd32", 128), ("of64", 128), ("of128", 128)]:
    CCOLS[_n] = (_off, _c)
    _off += _c
NCC = _off


def _consts():
    c = np.zeros((128, NCC), np.float32)
    p = np.arange(128)[:, None]
    f = np.arange(128)[None, :]
    def put(name, arr):
        o, n = CCOLS[name]
        c[:, o:o + n] = arr
    put("ident", (p == f))
    put("blk1", (p // 64 == f // 64))
    put("m_su", (p < f))
    put("m_ui", (p <= f))
    put("m_sl", (p > f))
    put("ones", np.ones((128, 128)))
    put("bd32", (p // 32 == f // 32))
    put("of64", (p // 64 == f // 64) & (p // 32 != f // 32))
    put("of128", (p // 64 != f // 64))
    return c


def _sel():
    sel = np.zeros((64, 64, 128), np.float32)
    for e in range(64):
        sel[e, e, :] = 1.0
    return sel.reshape(64, -1)


class Tok:
    __slots__ = ("sem", "val", "key")

    def __init__(self, sem, val, key):
        self.sem, self.val, self.key = sem, val, key


class KB:
    def __init__(self, nc, es):
        self.nc = nc
        self.eng = {"pe": nc.tensor, "act": nc.scalar, "dve": nc.vector, "pool": nc.gpsimd, "sp": nc.sync}
        self.csem = {}
        self.ccnt = {}
        for n in ["pe", "act", "dve", "pool"]:
            self.csem[n] = es.enter_context(nc.semaphore("c_" + n))
            self.ccnt[n] = 0
        self.ndsem = 40
        self.dsem = [es.enter_context(nc.semaphore("d%d" % i)) for i in range(self.ndsem)]
        self.dcnt = [0] * self.ndsem
        self.dpool = {"sp": list(range(0, 24)), "pool": list(range(24, 36)), "act": list(range(36, 40))}
        self.dnext = {"sp": 0, "pool": 0, "act": 0}
        self.waited = {n: {} for n in self.eng}
        self.last_w = {}
        self.readers = {}
        self.nins = 0

    def _wait(self, en, tok):
        w = self.waited[en]
        if w.get(tok.key, 0) >= tok.val:
            return
        self.eng[en].wait_ge(tok.sem, tok.val)
        w[tok.key] = tok.val

    def _deps(self, en, reads, writes, skip_pe_self=False):
        toks = []
        for b in reads:
            t = self.last_w.get(b)
            if t is not None:
                toks.append(t)
        for b in writes:
            t = self.last_w.get(b)
            if t is not None:
                toks.append(t)
            toks.extend(self.readers.get(b, ()))
        for t in toks:
            if skip_pe_self and t.key == "c_pe":
                continue
            self._wait(en, t)

    def _record(self, tok, reads, writes):
        for b in writes:
            self.last_w[b] = tok
            self.readers[b] = []
        for b in reads:
            self.readers.setdefault(b, []).append(tok)

    def op(self, en, fn, reads=(), writes=()):
        self._deps(en, reads, writes, skip_pe_self=(en == "pe"))
        ins = fn(self.eng[en])
        self.ccnt[en] += 1
        ins.then_inc(self.csem[en], 1)
        tok = Tok(self.csem[en], self.ccnt[en], "c_" + en)
        self._record(tok, reads, writes)
        self.nins += 1
        return tok

    def mm_group(self, fns, reads=(), writes=()):
        self._deps("pe", reads, writes, skip_pe_self=True)
        ins = None
        for fn in fns:
            ins = fn(self.eng["pe"])
            self.nins += 1
        self.ccnt["pe"] += 1
        ins.then_inc(self.csem["pe"], 1)
        tok = Tok(self.csem["pe"], self.ccnt["pe"], "c_pe")
        self._record(tok, reads, writes)
        return tok

    def dma(self, en, out, in_, reads=(), writes=(), slow=False):
        self._deps(en, reads, writes)
        pl = self.dpool[en]
        i = pl[self.dnext[en] % len(pl)]
        self.dnext[en] += 1
        if self.dcnt[i] > 0:
            self._wait(en, Tok(self.dsem[i], self.dcnt[i], "d%d" % i))
        self.dcnt[i] += 16
        if slow:
            self.eng[en].dma_start(out=out, in_=in_, allow_slow_non_contiguous=True).then_inc(self.dsem[i], 16)
        else:
            self.eng[en].dma_start(out=out, in_=in_).then_inc(self.dsem[i], 16)
        tok = Tok(self.dsem[i], self.dcnt[i], "d%d" % i)
        self._record(tok, reads, writes)
        self.nins += 1
        return tok

    def barrier(self):
        for en in self.eng:
            for n in self.csem:
                if self.ccnt[n] > 0:
                    self._wait(en, Tok(self.csem[n], self.ccnt[n], "c_" + n))
            for i in range(self.ndsem):
                if self.dcnt[i] > 0:
                    self._wait(en, Tok(self.dsem[i], self.dcnt[i], "d%d" % i))

    def finish(self, en="sp"):
        for i in range(self.ndsem):
            if self.dcnt[i] > 0:
                self._wait(en, Tok(self.dsem[i], self.dcnt[i], "d%d" % i))


def pc(P, name, j=None, n=1):
    o, c = PCOLS[name]
    if j is None:
        return P[:, o:o + c]
    return P[:, o + j:o + j + n]


def build(debug=None, stop_after=None):
    nc = bass.Bass("TRN2", target_bir_lowering=False)
    moe_on = stop_after is None
    dt_in = lambda name, shape, dt=F32: nc.dram_tensor(name, list(shape), dt, kind="ExternalInput").ap()
    dt_out = lambda name, shape, dt=F32: nc.dram_tensor(name, list(shape), dt, kind="ExternalOutput").ap()
    dt_scr = lambda name, shape, dt=F32: nc.dram_tensor(name, list(shape), dt).ap()

    x_p = dt_in("x_p", [SEQ, D])
    x_s = dt_in("x_s", [NS, TS, D])
    cT = dt_in("cT", [D, 17])
    shT = dt_in("shT", [NSHIFT, NS])
    scT = dt_in("scT", [D, 30 * NS])
    s_wkv = dt_in("s_wkv", [NS, NH, HS, HS])
    prm = dt_in("prm", [128, NPC])
    cst = dt_in("cst", [128, NCC])
    w_ada = dt_in("w_ada", [D, 6 * D])
    w_in = dt_in("w_in", [D, NIN])
    w_decay2 = dt_in("w_decay2", [64, D])
    w_aaa2 = dt_in("w_aaa2", [64, D])
    w_gate2 = dt_in("w_gate2", [160, D])
    w_o_rwkv = dt_in("w_o_rwkv", [D, D])
    w_conv_out = dt_in("w_conv_out", [D, D])
    w_out = dt_in("w_out", [D, D])
    w_router = dt_in("w_router", [D, NE])
    if moe_on:
        w_eg = dt_in("w_eg", [NE + 1, D, DE])
        w_eu = dt_in("w_eu", [NE + 1, D, DE])
        w_ed = dt_in("w_ed", [NE + 1, DE, D])
    y_p = dt_out("y_p", [SEQ, D])
    y_s = dt_out("y_s", [NS, TS, D])
    o_wkv_p = dt_out("o_wkv_p", [NH, HS, HS])
    o_sh_p = dt_out("o_sh_p", [1, NSHIFT])
    o_cv_p = dt_out("o_cv_p", [30, D])
    o_wkv_s = dt_out("o_wkv_s", [NS, NH, HS, HS])
    o_sh_s = dt_out("o_sh_s", [NS, NSHIFT])
    o_cv_s = dt_out("o_cv_s", [NS, 30, D])
    PW = 1 + SEQ + NS + NTS
    P_d = dt_scr("P_d", [NSHIFT, PW])
    dbg = {}
    if debug:
        for name, shape in debug.items():
            dbg[name] = dt_out("dbg_" + name, shape, BF16 if name in ("YI_d", "YI_s", "HF_d", "GT_d", "uT", "mT", "kT0", "AR0", "KK0", "BB0", "MT0", "Xs0", "Us0", "Vtm0") else F32)

    def pcol(c0, sample):
        return (1 + c0) if not sample else (1 + SEQ + NS + (c0 - SEQ))

    with ExitStack() as es:
        kb = KB(nc, es)
        sb = lambda n, sh, dt=F32: es.enter_context(nc.sbuf_tensor(n, list(sh), dt))
        P = sb("P", [128, NPC])
        C = sb("C", [128, NCC])
        Cb = sb("Cb", [128, NCC], BF16)
        modT = sb("modT", [128, 48, 17])
        Am = sb("Am", [128, 8, 17]); Gm = sb("Gm", [128, 8, 17])
        Af = sb("Af", [128, 8, 17]); Gf = sb("Gf", [128, 8, 17])

        kb.dma("sp", P[:], prm, writes=["P"])
        kb.dma("sp", C[:], cst, writes=["C"])
        kb.dma("pool", Cb[:], cst, writes=["Cb"])

        def cc(name, bf=False):
            o, n = CCOLS[name]
            return (Cb if bf else C)[:, o:o + n]

        with ExitStack() as ea:
            sba = lambda n, sh, dt=F32: ea.enter_context(nc.sbuf_tensor(n, list(sh), dt))
            cTs = sba("cTs", [128, 8, 17])
            cTb = sba("cTb", [128, 8, 17], BF16)
            wa = [sba("wa%d" % i, [128, 8, 512], BF16) for i in range(2)]
            psA = ea.enter_context(nc.psum_tensor("psA", [128, 2, 512], F32))
            kb.dma("sp", cTs[:], cT.rearrange("(k p) m -> p k m", p=128), writes=["cTs"])
            kb.op("act", lambda e: e.activation(out=cTb[:], in_=cTs[:], func=AF.Silu), reads=["cTs"], writes=["cTb"])
            for nb in range(12):
                s = nb % 2
                kb.dma("pool", wa[s][:], w_ada[:, nb * 512:(nb + 1) * 512].rearrange("(k p) n -> p k n", p=128),
                       writes=["wa%d" % s])
                for oo in range(4):
                    o = nb * 4 + oo
                    dst = psA[:, o // 24, (o % 24) * 17:(o % 24) * 17 + 17]
                    kb.mm_group([
                        (lambda e, k=k, oo=oo, dst=dst, s=s: e.matmul(dst, lhsT=wa[s][:, k, oo * 128:(oo + 1) * 128],
                                                                    rhs=cTb[:, k, :], start=(k == 0), stop=(k == 7)))
                        for k in range(8)], reads=["wa%d" % s, "cTb"], writes=["psA"])
            for half in range(2):
                o0 = half * 24
                kb.op("dve", lambda e, half=half, o0=o0: e.tensor_tensor(
                    out=modT[:, o0:o0 + 24, :], in0=psA[:, half, 0:24 * 17].rearrange("p (o m) -> p o m", m=17),
                    in1=pc(P, "b_ada")[:, o0:o0 + 24].unsqueeze(2).to_broadcast([128, 24, 17]), op=ALU.add),
                    reads=["psA", "P"], writes=["modT"])
            def bc(name):
                return pc(P, name).unsqueeze(2).to_broadcast([128, 8, 17])
            kb.op("dve", lambda e: e.scalar_tensor_tensor(out=Am[:], in0=modT[:, 8:16, :], scalar=1.0, in1=bc("mix_pre_g"),
                                                          op0=ALU.add, op1=ALU.mult), reads=["modT", "P"], writes=["Am"])
            kb.op("dve", lambda e: e.tensor_tensor(out=Gm[:], in0=modT[:, 16:24, :], in1=bc("mix_post_g"), op=ALU.mult),
                  reads=["modT", "P"], writes=["Gm"])
            kb.op("dve", lambda e: e.scalar_tensor_tensor(out=Af[:], in0=modT[:, 32:40, :], scalar=1.0, in1=bc("ffn_pre_g"),
                                                          op0=ALU.add, op1=ALU.mult), reads=["modT", "P"], writes=["Af"])
            kb.op("dve", lambda e: e.tensor_tensor(out=Gf[:], in0=modT[:, 40:48, :], in1=bc("ffn_post_g"), op=ALU.mult),
                  reads=["modT", "P"], writes=["Gf"])
            if "modT" in dbg:
                kb.dma("sp", dbg["modT"], modT[:], reads=["modT"])

        if stop_after == "A":
            kb.finish("sp"); return nc
        kb.barrier()
        GW = 30 + SEQ + 30 * NS + NTS
        G_d = dt_scr("G_d", [D, GW])
        SG_d = dt_scr("SG_d", [2 * D, TT], BF16)
        XT_d = dt_scr("XT_d", [D, TT])
        def gcol(c0, sample):
            return (30 + c0) if not sample else (30 + SEQ + 30 * NS + (c0 - SEQ))

        def bcast_mod(M, lo, sample, n):
            if not sample:
                return M[:, lo:lo + 8, 0:1].to_broadcast([128, 8, n])
            return M[:, lo:lo + 8, 1:17].unsqueeze(2).to_broadcast([128, 8, TS, NS])

        def view(ap, sample):
            return ap if not sample else ap.rearrange("p k (t s) -> p k t s", s=NS)

        with ExitStack() as eb:
            sbb = lambda n, sh, dt=F32: eb.enter_context(nc.sbuf_tensor(n, list(sh), dt))
            psb = lambda n, sh, dt=F32: eb.enter_context(nc.psum_tensor(n, list(sh), dt))
            wi = sbb("wi", [128, 8, NIN], BF16)
            zt = sbb("zt", [128, 8 * 30])
            xt = [sbb("xt%d" % i, [128, D]) for i in range(2)]
            xTb = sbb("xTb", [128, 8, 512])
            sq = sbb("sq", [128, 8, 512])
            rstd = sbb("rstd", [128, 512])
            hT = [sbb("hT%d" % i, [128, 8, 512], BF16) for i in range(2)]
            stg = [sbb("stg%d" % i, [128, 512]) for i in range(2)]
            stb = [sbb("stb%d" % i, [128, 512], BF16) for i in range(2)]
            sgt = [sbb("sgt%d" % i, [128, 512]) for i in range(1)]
            shrow = [sbb("shrow%d" % i, [64, 512]) for i in range(2)]
            raw = [sbb("raw%d" % i, [128, 512 + NS]) for i in range(2)]
            bnd = sbb("bnd", [128, 27, NS])
            psT = psb("psT", [128, 8, 128])
            psP = [psb("psP%d" % i, [128, 512]) for i in range(4)]
            psS = psb("psS", [128, 512])
            for q in range(15):
                c_lo = q * 512
                c_hi = min(NIN, c_lo + 512)
                kb.dma("pool", wi[:, :, c_lo:c_hi], w_in[:, c_lo:c_hi].rearrange("(k p) n -> p k n", p=128),
                       writes=[("wi", q)])
            def wi_keys(lo, hi):
                return [("wi", q) for q in range(lo // 512, (hi - 1) // 512 + 1)]
            kb.op("dve", lambda e: e.memset(zt[:], 0.0), writes=["zt"])
            kb.op("dve", lambda e: e.memset(bnd[:], 0.0), writes=["bnd"])
            kb.dma("sp", G_d[:, 0:30].rearrange("(k p) c -> p k c", p=128), zt[:].rearrange("p (k c) -> p k c", c=30),
                   reads=["zt"], writes=["G_hist"])
            kb.dma("sp", G_d[:, 30 + SEQ:30 + SEQ + 30 * NS], scT, writes=["G_hist"])
            xcount = 0
            pcount = 0
            for bi, (c0, n, sample) in enumerate(BLOCKS):
                hs = bi % 2
                m = 64 if sample else 128
                for i in range(n // m):
                    xs_ = xcount % 2
                    xcount += 1
                    if not sample:
                        kb.dma("sp", xt[xs_][:], x_p[c0 + i * 128:c0 + (i + 1) * 128, :], writes=[("xt", xs_)])
                    else:
                        for t in range(TS):
                            kb.dma("sp", xt[xs_][t * NS:(t + 1) * NS, :], x_s[:, t, :], writes=[("xt", xs_)])
                    kb.mm_group([
                        (lambda e, k=k, xs_=xs_: e.transpose(out=psT[:, k, 0:m], in_=xt[xs_][0:m, k * 128:(k + 1) * 128],
                                                            identity=cc("ident")[0:m, 0:m]))
                        for k in range(8)], reads=[("xt", xs_), "C"], writes=["psT"])
                    kb.op("act", lambda e, i=i: e.activation(out=xTb[:, :, i * m:(i + 1) * m], in_=psT[:, :, 0:m], func=AF.Copy),
                          reads=["psT"], writes=["xTb"])
                kb.dma("sp", XT_d[:, c0:c0 + n].rearrange("(k p) c -> p k c", p=128), xTb[:, :, 0:n], reads=["xTb"],
                       writes=[("XT", bi)])
                kb.op("act", lambda e: e.activation(out=sq[:, :, 0:n], in_=xTb[:, :, 0:n], func=AF.Square),
                      reads=["xTb"], writes=["sq"])
                kb.op("dve", lambda e: e.tensor_reduce(out=rstd[:, 0:n], in_=sq[:, :, 0:n].rearrange("p k n -> p n k"), axis=AX.X, op=ALU.add),
                      reads=["sq"], writes=["rstd"])
                kb.mm_group([lambda e: e.matmul(psS[:, 0:n], lhsT=cc("ones"), rhs=rstd[:, 0:n], start=True, stop=True)],
                            reads=["rstd", "C"], writes=["psS"])
                kb.op("act", lambda e: e.activation(out=rstd[:, 0:n], in_=psS[:, 0:n], func=AF.Ln, scale=1.0 / D, bias=1e-6),
                      reads=["psS"], writes=["rstd"])
                kb.op("act", lambda e: e.activation(out=rstd[:, 0:n], in_=rstd[:, 0:n], func=AF.Exp, scale=-0.5),
                      reads=["rstd"], writes=["rstd"])
                kb.op("dve", lambda e: e.tensor_tensor(out=sq[:, :, 0:n], in0=xTb[:, :, 0:n],
                                                      in1=rstd[:, 0:n].unsqueeze(1).to_broadcast([128, 8, n]), op=ALU.mult),
                      reads=["xTb", "rstd", "sq"], writes=["sq"])
                kb.op("dve", lambda e: e.tensor_tensor(out=view(sq[:, :, 0:n], sample), in0=view(sq[:, :, 0:n], sample),
                                                      in1=bcast_mod(Am, 0, sample, n), op=ALU.mult),
                      reads=["sq", "Am"], writes=["sq"])
                kb.op("dve", lambda e: e.tensor_tensor(out=view(hT[hs][:, :, 0:n], sample), in0=view(sq[:, :, 0:n], sample),
                                                      in1=bcast_mod(modT, 0, sample, n), op=ALU.add),
                      reads=["sq", "modT"], writes=[("hT", hs)])

                def proj(col_lo, mrows, hs=hs, n=n):
                    nonlocal pcount
                    ps = pcount % 4
                    pcount += 1
                    kb.mm_group([
                        (lambda e, k=k, ps=ps: e.matmul(psP[ps][0:mrows, 0:n], lhsT=wi[:, k, col_lo:col_lo + mrows],
                                                        rhs=hT[hs][:, k, 0:n], start=(k == 0), stop=(k == 7)))
                        for k in range(8)], reads=[("hT", hs)] + wi_keys(col_lo, col_lo + mrows), writes=[("psP", ps)])
                    return ps
                wd_ = NS if sample else 1
                if sample:
                    kb.dma("sp", bnd[:, 0:26, :], shT[0:3328, :].rearrange("(k p) c -> p k c", p=128), writes=["bnd"])
                    kb.dma("sp", bnd[0:32, 26, :], shT[3328:3360, :], writes=["bnd"])
                for ot in range(27):
                    mrows = 128 if ot < 26 else 32
                    ps = proj(ot * 128, mrows)
                    sg = ot % 2
                    rw_ = raw[ot % 2]
                    rk_ = ("raw", ot % 2)
                    kb.op("act", lambda e, ot=ot, rw_=rw_: e.activation(out=rw_[0:mrows, 0:wd_], in_=bnd[0:mrows, ot, 0:wd_], func=AF.Copy),
                          reads=["bnd"], writes=[rk_])
                    kb.op("act", lambda e, ps=ps, rw_=rw_: e.activation(out=rw_[0:mrows, wd_:wd_ + n], in_=psP[ps][0:mrows, 0:n],
                                                                      func=AF.Copy), reads=[("psP", ps)], writes=[rk_])
                    if not sample and bi < 3:
                        kb.op("act", lambda e, ot=ot, rw_=rw_: e.activation(out=bnd[0:mrows, ot, 0:1], in_=rw_[0:mrows, n:n + 1], func=AF.Copy),
                              reads=[rk_], writes=["bnd"])
                    kb.op("pool", lambda e, rw_=rw_, sg=sg: e.tensor_tensor(out=stg[sg][0:mrows, 0:n], in0=rw_[0:mrows, 0:n],
                                                                           in1=rw_[0:mrows, wd_:wd_ + n], op=ALU.subtract),
                          reads=[rk_], writes=[("stg", sg)])
                    kb.op("dve", lambda e, rw_=rw_, sg=sg, ot=ot: e.scalar_tensor_tensor(
                        out=stg[sg][0:mrows, 0:n], in0=stg[sg][0:mrows, 0:n], scalar=pc(P, "mu", ot)[0:mrows, :],
                        in1=rw_[0:mrows, wd_:wd_ + n], op0=ALU.mult, op1=ALU.add), reads=[rk_, ("stg", sg), "P"], writes=[("stg", sg)])
                    pcl = pcol(c0, sample)
                    kb.dma("sp", P_d[ot * 128:ot * 128 + mrows, pcl:pcl + n], stg[sg][0:mrows, 0:n], reads=[("stg", sg)],
                           writes=[("P", bi)])
                for j in range(8):
                    pv = proj(NSHIFT + j * 128, 128)
                    pg = proj(NSHIFT + D + j * 128, 128)
                    s2 = 0
                    kb.op("act", lambda e, pg=pg, s2=s2: e.activation(out=sgt[s2][:, 0:n], in_=psP[pg][:, 0:n], func=AF.Sigmoid),
                          reads=[("psP", pg)], writes=[("sgt", s2)])
                    sg = j % 2
                    kb.op("dve", lambda e, pv=pv, s2=s2, sg=sg: e.tensor_tensor(out=stg[sg][:, 0:n], in0=psP[pv][:, 0:n],
                                                                             in1=sgt[s2][:, 0:n], op=ALU.mult),
                          reads=[("psP", pv), ("sgt", s2)], writes=[("stg", sg)])
                    gcl = gcol(c0, sample)
                    kb.dma("sp", G_d[j * 128:(j + 1) * 128, gcl:gcl + n], stg[sg][:, 0:n], reads=[("stg", sg)],
                           writes=[("G", bi)])
                for j in range(16):
                    pg = proj(NSHIFT + 2 * D + j * 128, 128)
                    s2 = j % 2
                    kb.op("act", lambda e, pg=pg, s2=s2: e.activation(out=stb[s2][:, 0:n], in_=psP[pg][:, 0:n], func=AF.Sigmoid),
                          reads=[("psP", pg)], writes=[("stb", s2)])
                    kb.dma("sp", SG_d[j * 128:(j + 1) * 128, c0:c0 + n], stb[s2][:, 0:n], reads=[("stb", s2)],
                           writes=[("SG", bi)])
                if bi == 3 or sample:
                    mm = 64 if sample else 1
                    lcol = 0 if sample else 511
                    for cb in range(7):
                        w_ = min(512, NSHIFT - cb * 512)
                        ps = pcount % 4
                        pcount += 1
                        kb.mm_group([
                            (lambda e, k=k, ps=ps: e.matmul(psP[ps][0:mm, 0:w_], lhsT=hT[hs][:, k, lcol:lcol + mm],
                                                            rhs=wi[:, k, cb * 512:cb * 512 + w_], start=(k == 0), stop=(k == 7)))
                            for k in range(8)], reads=[("hT", hs)] + wi_keys(cb * 512, cb * 512 + w_), writes=[("psP", ps)])
                        sr = cb % 2
                        kb.op("act", lambda e, ps=ps, sr=sr: e.activation(out=shrow[sr][0:mm, 0:w_],
                                                                   in_=psP[ps][0:mm, 0:w_], func=AF.Copy),
                              reads=[("psP", ps)], writes=[("shrow", sr)])
                        if sample:
                            kb.dma("sp", o_sh_s[:, cb * 512:cb * 512 + w_], shrow[sr][48:64, 0:w_], reads=[("shrow", sr)],
                                   writes=["o_sh_s"])
                        else:
                            kb.dma("sp", o_sh_p[:, cb * 512:cb * 512 + w_], shrow[sr][0:1, 0:w_], reads=[("shrow", sr)],
                                   writes=["o_sh_p"])
            if "P_d" in dbg:
                kb.dma("sp", dbg["P_d"], P_d, reads=[("P", b) for b in range(5)] + ["P_hist"])
            if "G_d" in dbg:
                kb.dma("sp", dbg["G_d"], G_d, reads=[("G", b) for b in range(5)] + ["G_hist"])
        if stop_after == "B":
            kb.finish("sp"); return nc
        kb.barrier()

        YI_d = dt_scr("YI_d", [D, TT], BF16)
        RW_d = dt_scr("RW_d", [5, 2, NTS, 8, HS])
        GN_EPS = HS * 1e-5
        v_s = sb("v_s", [128, 8, NTS]); bon_s = sb("bon_s", [128, 8, NTS]); g_s = sb("g_s", [128, 8, NTS], BF16)
        def gn_out(ysrc, bsrc, gsrc, n_, col0, rkeys, RS):
            psR, nxt = RS["psR"], RS["nxt"]
            setA = (RS["tp"][1], RS["tp"][2], RS["yib"], "xk", "xv", "yib")
            setB = RS.get("setB")

            def gn_tile(j, S_):
                d_, sq_, yb_, dk, qk, yk = S_
                p1 = nxt()
                yield
                kb.mm_group([lambda e: e.matmul(psR[p1][:, 0:n_], lhsT=cc("blk1"), rhs=ysrc[:, j, 0:n_],
                                                start=True, stop=True)], reads=["C"] + rkeys, writes=[("psR", p1)])
                yield
                kb.op("dve", lambda e: e.scalar_tensor_tensor(out=d_[:, 0:n_], in0=psR[p1][:, 0:n_], scalar=-1.0 / HS,
                                                               in1=ysrc[:, j, 0:n_], op0=ALU.mult, op1=ALU.add),
                      reads=[("psR", p1), "C"] + rkeys, writes=[dk])
                yield
                kb.op("act", lambda e: e.activation(out=sq_[:, 0:n_], in_=d_[:, 0:n_], func=AF.Square),
                      reads=[dk], writes=[qk])
                p2 = nxt()
                yield
                kb.mm_group([lambda e: e.matmul(psR[p2][:, 0:n_], lhsT=cc("blk1"), rhs=sq_[:, 0:n_],
                                                start=True, stop=True)], reads=["C", qk], writes=[("psR", p2)])
                yield
                kb.op("act", lambda e: e.activation(out=sq_[:, 0:n_], in_=psR[p2][:, 0:n_], func=AF.Ln, scale=1.0 / HS, bias=GN_EPS),
                      reads=[("psR", p2), qk], writes=[qk])
                yield
                kb.op("act", lambda e: e.activation(out=sq_[:, 0:n_], in_=sq_[:, 0:n_], func=AF.Exp, scale=-0.5), reads=[qk], writes=[qk])
                yield
                kb.op("dve", lambda e: e.tensor_tensor(out=d_[:, 0:n_], in0=d_[:, 0:n_], in1=sq_[:, 0:n_], op=ALU.mult),
                      reads=[dk, qk], writes=[dk])
                yield
                kb.op("dve", lambda e: e.tensor_scalar(out=d_[:, 0:n_], in0=d_[:, 0:n_], scalar1=pc(P, "gn_g", j),
                                                      scalar2=pc(P, "gn_b", j), op0=ALU.mult, op1=ALU.add),
                      reads=[dk, "P"], writes=[dk])
                yield
                kb.op("dve", lambda e: e.tensor_tensor(out=d_[:, 0:n_], in0=d_[:, 0:n_], in1=bsrc[:, j, 0:n_], op=ALU.add),
                      reads=[dk] + rkeys, writes=[dk])
                yield
                kb.op("dve", lambda e: e.tensor_tensor(out=yb_[:, 0:n_], in0=d_[:, 0:n_], in1=gsrc[:, j, 0:n_], op=ALU.mult),
                      reads=[dk] + rkeys, writes=[yk])
                yield
                kb.dma("sp", YI_d[j * 128:(j + 1) * 128, col0:col0 + n_], yb_[:, 0:n_], reads=[yk], writes=[("YI", col0)])

            if setB is None:
                for j in range(8):
                    for _ in gn_tile(j, setA):
                        pass
            else:
                for jp in range(4):
                    gs_ = [gn_tile(2 * jp, setA), gn_tile(2 * jp + 1, setB)]
                    while gs_:
                        for g_ in list(gs_):
                            try:
                                next(g_)
                            except StopIteration:
                                gs_.remove(g_)

        with ExitStack() as ec:
            sbc = lambda n, sh, dt=F32: ec.enter_context(nc.sbuf_tensor(n, list(sh), dt))
            psc = lambda n, sh, dt=F32: ec.enter_context(nc.psum_tensor(n, list(sh), dt))
            wlo = sbc("wlo", [128, D], BF16)
            wg2 = sbc("wg2", [128, 2, D], BF16)
            Hf = sbc("Hf", [128, 8, 128]); Hb = sbc("Hb", [128, 8, 128], BF16)
            m01 = sbc("m01", [128, 512])
            AR = sbc("AR", [128, 8, 4, 2, 128], BF16)
            kT = sbc("kT", [128, 8, 512], BF16); bT = sbc("bT", [128, 8, 512], BF16)
            v16 = sbc("v16", [128, 8, 512], BF16); g16 = sbc("g16", [128, 8, 512], BF16)
            bon = sbc("bon", [128, 8, 512], BF16); yT = sbc("yT", [128, 8, 512]); Pc = sbc("Pc", [128, 8, 4])
            ld = [sbc("ld%d" % i, [128, 512]) for i in range(3)]
            tp = [sbc("tp%d" % i, [128, 512]) for i in range(9)]
            tpB = [sbc("tpB%d" % i, [128, 512]) for i in range(6)]
            twa = sbc("twa", [128, 512], BF16); sxg = sbc("sxg", [128, 2, 512], BF16)
            Vtm = sbc("Vtm", [128, D], BF16); Ktm = sbc("Ktm", [128, D], BF16); Btm = sbc("Btm", [128, D], BF16)
            KK_ = sbc("KK_", [128, 16, 2, 128], BF16); RB_ = sbc("RB_", [128, 16, 128], BF16)
            L0f = sbc("L0f", [128, 8, 128], BF16); N0f = sbc("N0f", [128, 8, 128], BF16)
            Pw = [sbc("Pw%d" % i, [128, 8, 128], BF16) for i in range(2)]
            Qw = [sbc("Qw%d" % i, [128, 8, 128], BF16) for i in range(2)]
            Ml = [sbc("Ml%d" % i, [128, 8, 128], BF16) for i in range(2)]
            MT = [sbc("MT%d" % i, [128, 8, 128], BF16) for i in range(2)]
            MTb = sbc("MTb", [128, 16, 128], BF16)
            Xs = sbc("Xs", [128, D], BF16); Us = sbc("Us", [128, D], BF16)
            yib = sbc("yib", [128, 512], BF16); yibB = sbc("yibB", [128, 512], BF16)
            rows_s2 = [sbc("rows_s%d" % i, [64, 128]) for i in range(2)]
            psR = [psc("psR%d" % i, [128, 512]) for i in range(6)]
            psTr = psc("psTr", [128, D], BF16)
            rcount = 0
            def nxt():
                nonlocal rcount
                r_ = rcount % 6
                rcount += 1
                return r_
            kb.dma("pool", wlo[0:64, :], w_decay2, writes=["wlo"])
            kb.dma("pool", wlo[64:128, :], w_aaa2, writes=["wlo"])
            kb.dma("pool", wg2[:, 0, :], w_gate2[0:128, :], writes=["wg2"])
            kb.dma("pool", wg2[0:32, 1, :], w_gate2[128:160, :], writes=["wg2"])
            kb.op("dve", lambda e: e.memset(Hf[:], 0.0), writes=["Hf"])
            kb.op("dve", lambda e: e.memset(Hb[:], 0.0), writes=["Hb"])
            kb.op("dve", lambda e: e.memset(m01[:], 1.0), writes=["m01"])
            kb.op("dve", lambda e: e.memset(m01[:].rearrange("p (c t) -> p c t", t=128)[:, :, 0:1], 0.0), writes=["m01"])
            E05 = float(np.exp(-0.5))

            def load_xs(ti, rows, bi, c0, n, sample, dst, slot, dkey):
                pcl = pcol(c0, sample)
                kb.dma("sp", dst, P_d[ti * 128:ti * 128 + rows, pcl:pcl + n], reads=[("P", bi)], writes=[dkey])

            xr, xk, xv, lw, av, kk, ksq, tk, cs = tp
            for bi, (c0, n, sample) in enumerate(BLOCKS):
                nch = 1 if sample else n // 128
                load_xs(24, 128, bi, c0, n, sample, ksq[:, 0:n], 0, "ksq")
                kb.op("act", lambda e: e.activation(out=twa[0:64, 0:n], in_=ksq[0:64, 0:n], func=AF.Tanh),
                      reads=["ksq"], writes=["twa"])
                kb.op("act", lambda e: e.activation(out=twa[64:128, 0:n], in_=ksq[64:128, 0:n], func=AF.Copy),
                      reads=["ksq"], writes=["twa"])
                load_xs(25, 128, bi, c0, n, sample, tk[:, 0:n], 1, "tk")
                kb.op("act", lambda e: e.activation(out=sxg[:, 0, 0:n], in_=tk[:, 0:n], func=AF.Sigmoid),
                      reads=["tk"], writes=["sxg"])
                load_xs(26, 32, bi, c0, n, sample, cs[0:32, 0:n], 2, "cs")
                kb.op("act", lambda e: e.activation(out=sxg[0:32, 1, 0:n], in_=cs[0:32, 0:n], func=AF.Sigmoid),
                      reads=["cs"], writes=["sxg"])
                def tile_gen(j):
                    jc = slice(j * 128, (j + 1) * 128)
                    if j % 2 == 0:
                        xr, xk, xv = tp[0], tp[1], tp[2]
                        xrk, xkk, xvk = "xr", "xk", "xv"
                        lw, av, kk, ksq, tk, cs = tp[3:9]
                        klw, kav, kkk, kksq, ktk, kcs = "lw", "av", "kk", "ksq", "tk", "cs"
                    else:
                        xr, xk, xv = ld[0], ld[1], ld[2]
                        xrk, xkk, xvk = "xr2", "xk2", "xv2"
                        lw, av, kk, ksq, tk, cs = tpB
                        klw, kav, kkk, kksq, ktk, kcs = "lw2", "av2", "kk2", "ksq2", "tk2", "cs2"
                    yield
                    load_xs(j, 128, bi, c0, n, sample, xr[:, 0:n], 0, xrk)
                    yield
                    load_xs(8 + j, 128, bi, c0, n, sample, xk[:, 0:n], 1, xkk)
                    yield
                    load_xs(16 + j, 128, bi, c0, n, sample, xv[:, 0:n], 2, xvk)
                    pw = nxt()
                    yield
                    kb.mm_group([lambda e, pw=pw: e.matmul(psR[pw][:, 0:n], lhsT=wlo[0:64, jc], rhs=twa[0:64, 0:n],
                                                          start=True, stop=True)], reads=["wlo", "twa"], writes=[("psR", pw)])
                    yield
                    kb.op("act", lambda e, pw=pw: e.activation(out=lw[:, 0:n], in_=psR[pw][:, 0:n], func=AF.Sigmoid,
                                                               bias=pc(P, "w0", j)), reads=[("psR", pw), "P"], writes=[klw])
                    pa = nxt()
                    yield
                    kb.mm_group([lambda e, pa=pa: e.matmul(psR[pa][:, 0:n], lhsT=wlo[64:128, jc], rhs=twa[64:128, 0:n],
                                                          start=True, stop=True)], reads=["wlo", "twa"], writes=[("psR", pa)])
                    yield
                    kb.op("act", lambda e, pa=pa: e.activation(out=av[:, 0:n], in_=psR[pa][:, 0:n], func=AF.Sigmoid,
                                                               bias=pc(P, "a0", j)), reads=[("psR", pa), "P"], writes=[kav])
                    pg = nxt()
                    yield
                    kb.mm_group([lambda e, pg=pg: e.matmul(psR[pg][:, 0:n], lhsT=wg2[:, 0, jc], rhs=sxg[:, 0, 0:n],
                                                          start=True, stop=False),
                                 lambda e, pg=pg: e.matmul(psR[pg][:, 0:n], lhsT=wg2[0:32, 1, jc], rhs=sxg[0:32, 1, 0:n],
                                                          start=False, stop=True)], reads=["wg2", "sxg"], writes=[("psR", pg)])
                    gdst = g_s[:, j, :] if sample else g16[:, j, 0:n]
                    yield
                    kb.op("act", lambda e, pg=pg, gdst=gdst: e.activation(out=gdst, in_=psR[pg][:, 0:n], func=AF.Copy),
                          reads=[("psR", pg)], writes=["g16"])
                    yield
                    kb.op("dve", lambda e: e.tensor_scalar_mul(out=kk[:, 0:n], in0=xk[:, 0:n], scalar1=pc(P, "k_k", j)),
                          reads=["P", xkk], writes=[kkk])
                    yield
                    kb.op("act", lambda e: e.activation(out=ksq[:, 0:n], in_=kk[:, 0:n], func=AF.Square),
                          reads=[kkk], writes=[kksq])
                    pn = nxt()
                    yield
                    kb.mm_group([lambda e, pn=pn: e.matmul(psR[pn][:, 0:n], lhsT=cc("blk1"), rhs=ksq[:, 0:n],
                                                          start=True, stop=True)], reads=["C", kksq], writes=[("psR", pn)])
                    yield
                    kb.op("act", lambda e, pn=pn: e.activation(out=ksq[:, 0:n], in_=psR[pn][:, 0:n], func=AF.Ln, bias=1e-24),
                          reads=[("psR", pn)], writes=[kksq])
                    yield
                    kb.op("act", lambda e: e.activation(out=ksq[:, 0:n], in_=ksq[:, 0:n], func=AF.Exp, scale=-0.5),
                          reads=[kksq], writes=[kksq])
                    yield
                    kb.op("dve", lambda e: e.tensor_tensor(out=kk[:, 0:n], in0=kk[:, 0:n], in1=ksq[:, 0:n], op=ALU.mult),
                          reads=[kkk, kksq], writes=[kkk])
                    yield
                    kb.op("dve", lambda e: e.tensor_scalar(out=tk[:, 0:n], in0=av[:, 0:n], scalar1=pc(P, "k_a", j),
                                                          scalar2=pc(P, "k_a", j), op0=ALU.mult, op1=ALU.subtract),
                          reads=[kav, "P"], writes=[ktk])
                    yield
                    kb.op("dve", lambda e: e.scalar_tensor_tensor(out=xk[:, 0:n], in0=tk[:, 0:n], scalar=1.0, in1=xk[:, 0:n],
                                                                   op0=ALU.add, op1=ALU.mult), reads=[ktk, xkk], writes=[xkk])
                    yield
                    kb.op("dve", lambda e: e.scalar_tensor_tensor(out=tk[:, 0:n], in0=xr[:, 0:n], scalar=pc(P, "r_k", j),
                                                                   in1=xk[:, 0:n], op0=ALU.mult, op1=ALU.mult),
                          reads=[xkk, xrk, "P", ktk], writes=[ktk])
                    pb = nxt()
                    yield
                    kb.mm_group([lambda e, pb=pb: e.matmul(psR[pb][:, 0:n], lhsT=cc("blk1"), rhs=tk[:, 0:n],
                                                          start=True, stop=True)], reads=["C", ktk], writes=[("psR", pb)])
                    bdst = bon_s[:, j, :] if sample else bon[:, j, 0:n]
                    yield
                    kb.op("dve", lambda e, pb=pb, bdst=bdst: e.tensor_tensor(out=bdst, in0=psR[pb][:, 0:n], in1=xv[:, 0:n],
                                                                             op=ALU.mult), reads=[("psR", pb), xvk], writes=["bon"])
                    if not sample:
                        yield
                        kb.op("act", lambda e: e.activation(out=v16[:, j, 0:n], in_=xv[:, 0:n], func=AF.Copy),
                              reads=[xvk], writes=["v16"])
                        yield
                        kb.op("dve", lambda e: e.tensor_tensor_scan(out=cs[:, 0:n], data0=m01[:, 0:n], data1=lw[:, 0:n],
                                                                   initial=0.0, op0=ALU.mult, op1=ALU.add),
                              reads=["m01", klw], writes=[kcs])
                        eP, eN, ePm = ksq, tk, lw
                        yield
                        kb.op("dve", lambda e: e.tensor_tensor(out=ePm[:, 0:n], in0=cs[:, 0:n], in1=lw[:, 0:n], op=ALU.subtract),
                              reads=[kcs, klw], writes=[klw])
                        yield
                        kb.op("act", lambda e: e.activation(out=ePm[:, 0:n], in_=ePm[:, 0:n], func=AF.Exp, scale=-E05), reads=[klw], writes=[klw])
                        yield
                        kb.op("act", lambda e: e.activation(out=eP[:, 0:n], in_=cs[:, 0:n], func=AF.Exp, scale=-E05),
                              reads=[kcs], writes=[kksq])
                        yield
                        kb.op("act", lambda e: e.activation(out=eN[:, 0:n], in_=cs[:, 0:n], func=AF.Exp, scale=E05),
                              reads=[kcs], writes=[ktk])
                        yield
                        kb.op("dve", lambda e: e.tensor_copy(out=Pc[:, j, :], in_=eP[:, 0:n].rearrange("p (c t) -> p c t", t=128)[:, :, 127]),
                              reads=[kksq], writes=["Pc"])
                        arv = AR[:, j, :, :, :]
                        yield
                        kb.op("dve", lambda e: e.tensor_tensor(out=arv[:, :, 1, :], in0=xr[:, 0:n].rearrange("p (c t) -> p c t", t=128),
                                                              in1=eP[:, 0:n].rearrange("p (c t) -> p c t", t=128), op=ALU.mult),
                              reads=[kksq, xrk], writes=["AR"])
                        yield
                        kb.op("dve", lambda e: e.scalar_tensor_tensor(out=arv[:, :, 0, :], in0=kk[:, 0:n].rearrange("p (c t) -> p c t", t=128),
                                                                       scalar=-1.0, in1=ePm[:, 0:n].rearrange("p (c t) -> p c t", t=128),
                                                                       op0=ALU.mult, op1=ALU.mult), reads=[kkk, klw], writes=["AR"])
                        yield
                        kb.op("pool", lambda e: e.tensor_tensor(out=kT[:, j, 0:n], in0=xk[:, 0:n], in1=eN[:, 0:n], op=ALU.mult),
                              reads=[xkk, ktk], writes=["kT"])
                        yield
                        kb.op("dve", lambda e: e.tensor_tensor(out=kk[:, 0:n], in0=kk[:, 0:n], in1=av[:, 0:n], op=ALU.mult),
                              reads=[kkk, kav], writes=[kkk])
                        yield
                        kb.op("pool", lambda e: e.tensor_tensor(out=bT[:, j, 0:n], in0=kk[:, 0:n], in1=eN[:, 0:n], op=ALU.mult),
                              reads=[kkk, ktk], writes=["bT"])
                    else:
                        yield
                        kb.op("act", lambda e: e.activation(out=v_s[:, j, :], in_=xv[:, 0:n], func=AF.Copy), reads=[xvk], writes=["v_s"])
                        yield
                        kb.op("act", lambda e: e.activation(out=cs[:, 0:n], in_=lw[:, 0:n], func=AF.Exp, scale=-E05), reads=[klw], writes=[kcs])
                        yield
                        kb.op("dve", lambda e: e.tensor_tensor(out=av[:, 0:n], in0=kk[:, 0:n], in1=av[:, 0:n], op=ALU.mult),
                              reads=[kkk, kav], writes=[kav])
                        yield
                        kb.op("dve", lambda e: e.tensor_scalar_mul(out=kk[:, 0:n], in0=kk[:, 0:n], scalar1=-1.0),
                              reads=[kkk, kav], writes=[kkk])
                        for qi, (src, skey) in enumerate([(cs, kcs), (kk, kkk), (av, kav), (xk, xkk), (xr, xrk)]):
                            pt = nxt()
                            yield
                            kb.mm_group([lambda e, pt=pt, src=src: e.transpose(out=psR[pt][0:n, 0:128], in_=src[:, 0:n],
                                                                               identity=cc("ident"))],
                                        reads=["C", skey], writes=[("psR", pt)])
                            yield
                            kb.op("act", lambda e, pt=pt: e.activation(out=rows_s2[j % 2][:, 0:128], in_=psR[pt][0:n, 0:128], func=AF.Copy),
                                  reads=[("psR", pt)], writes=[("rows_s", j % 2)])
                            yield
                            for hp_ in range(2):
                                kb.dma("sp", RW_d[qi, hp_, :, j, :], rows_s2[j % 2][:, hp_ * 64:(hp_ + 1) * 64],
                                       reads=[("rows_s", j % 2)], writes=["RW"])
                for jp in range(4):
                    gs_ = [tile_gen(2 * jp), tile_gen(2 * jp + 1)]
                    while gs_:
                        for g_ in list(gs_):
                            try:
                                next(g_)
                            except StopIteration:
                                gs_.remove(g_)
                if stop_after == "Cprep":
                    kb.finish("sp"); return nc
                if sample:
                    continue
                for c in range(nch):
                    ccol = slice(c * 128, (c + 1) * 128)
                    for src, dstt, nm in [(v16, Vtm, "Vtm"), (kT, Ktm, "Ktm"), (bT, Btm, "Btm")]:
                        kb.mm_group([(lambda e, j=j, src=src: e.transpose(out=psTr[:, j * 128:(j + 1) * 128], in_=src[:, j, ccol],
                                                                          identity=cc("ident", True))) for j in range(8)],
                                    reads=["Cb", "v16", "kT", "bT"], writes=["psTr"])
                        kb.op("act", lambda e, dstt=dstt: e.activation(out=dstt[:], in_=psTr[:], func=AF.Copy),
                              reads=["psTr"], writes=[nm])
                    if stop_after == "Ctr":
                        kb.finish("sp"); return nc
                    msk2 = C[:, CCOLS["m_su"][0]:CCOLS["m_su"][0] + 256].rearrange("p (a t) -> p a t", a=2)
                    for hq in range(4):
                        pkE, pkO = nxt(), nxt()
                        fns = []
                        for h in range(4 * hq, 4 * hq + 4):
                            bank = pkE if h % 2 == 0 else pkO
                            slot = (h % 4) // 2
                            fns.append(lambda e, h=h, bank=bank, slot=slot: e.matmul(
                                psR[bank][:, slot * 256:slot * 256 + 256],
                                lhsT=kT[(h % 2) * 64:(h % 2) * 64 + 64, h // 2, ccol],
                                rhs=AR[(h % 2) * 64:(h % 2) * 64 + 64, h // 2, c, :, :].rearrange("p a t -> p (a t)"),
                                start=True, stop=True))
                        kb.mm_group(fns, reads=["kT", "AR"], writes=[("psR", pkE), ("psR", pkO)])
                        for par, bank in ((0, pkE), (1, pkO)):
                            kb.op("dve", lambda e, bank=bank, hq=hq, par=par: e.tensor_tensor(
                                out=KK_[:, 4 * hq + par:4 * hq + 4:2, :, :],
                                in0=psR[bank][:].rearrange("p (h a t) -> p h a t", h=2, a=2),
                                in1=msk2.unsqueeze(1).to_broadcast([128, 2, 2, 128]), op=ALU.mult),
                                reads=[("psR", bank), "C"], writes=["KK"])
                    for g8 in range(2):
                        h0 = 8 * g8
                        for (kind, dstA, mname, wkeys, off) in [("rb", RB_, "m_ui", ["RB"], h0),
                                                                ("l0", L0f, "m_sl", [("L0f", 0), ("L0f", 1)], 0),
                                                                ("n0", N0f, "m_su", [("N0f", 0), ("N0f", 1)], 0)]:
                            pkE, pkO = nxt(), nxt()
                            fns = []
                            for h in range(h0, h0 + 8):
                                bank = pkE if h % 2 == 0 else pkO
                                slot = (h - h0) // 2
                                pr = slice((h % 2) * 64, (h % 2) * 64 + 64)
                                a_ap = AR[pr, h // 2, c, 0, :]
                                r_apx = AR[pr, h // 2, c, 1, :]
                                b_ap = bT[pr, h // 2, ccol]
                                l_ap, r_ap = {"rb": (b_ap, r_apx), "l0": (a_ap, b_ap), "n0": (b_ap, a_ap)}[kind]
                                fns.append(lambda e, bank=bank, slot=slot, l_ap=l_ap, r_ap=r_ap: e.matmul(
                                    psR[bank][:, slot * 128:slot * 128 + 128], lhsT=l_ap, rhs=r_ap, start=True, stop=True))
                            kb.mm_group(fns, reads=["bT", "AR"], writes=[("psR", pkE), ("psR", pkO)])
                            for par, bank in ((0, pkE), (1, pkO)):
                                kb.op("dve", lambda e, bank=bank, dstA=dstA, par=par, mname=mname, off=off: e.tensor_tensor(
                                    out=dstA[:, off + par:off + 8:2, :], in0=psR[bank][:].rearrange("p (h t) -> p h t", h=4),
                                    in1=cc(mname).unsqueeze(1).to_broadcast([128, 4, 128]), op=ALU.mult),
                                    reads=[("psR", bank), "C"], writes=wkeys)
                        hsl = lambda hq: slice(4 * hq, 4 * hq + 4)
                        def msk(nm):
                            return cc(nm, True).unsqueeze(1).to_broadcast([128, 4, 128])
                        for hq in range(2):
                            kb.op("pool", lambda e, hq=hq: e.tensor_tensor(out=Pw[0][:, hsl(hq), :], in0=L0f[:, hsl(hq), :], in1=msk("bd32"), op=ALU.mult),
                                  reads=[("L0f", hq), "Cb"], writes=[("Pw", 0, hq)])
                            kb.op("pool", lambda e, hq=hq: e.tensor_tensor(out=Qw[0][:, hsl(hq), :], in0=N0f[:, hsl(hq), :], in1=msk("bd32"), op=ALU.mult),
                                  reads=[("N0f", hq), "Cb"], writes=[("Qw", 0, hq)])
                            kb.op("dve", lambda e, hq=hq: e.tensor_tensor(out=Ml[0][:, hsl(hq), :], in0=Pw[0][:, hsl(hq), :], in1=msk("ident"), op=ALU.add),
                                  reads=[("Pw", 0, hq), "Cb"], writes=[("Ml", 0, hq)])
                            kb.op("dve", lambda e, hq=hq: e.tensor_tensor(out=MT[0][:, hsl(hq), :], in0=Qw[0][:, hsl(hq), :], in1=msk("ident"), op=ALU.add),
                                  reads=[("Qw", 0, hq), "Cb"], writes=[("MT", 0, hq)])
                        def mm4(dst_bank, lhs, rhs, hq, rkeys):
                            kb.mm_group([(lambda e, i=i: e.matmul(psR[dst_bank][:, i * 128:(i + 1) * 128], lhsT=lhs[:, 4 * hq + i, :],
                                                                 rhs=rhs[:, 4 * hq + i, :], start=True, stop=True)) for i in range(4)],
                                        reads=rkeys, writes=[("psR", dst_bank)])
                        def evac_copy(dst, dkey, bank, hq):
                            kb.op("act", lambda e: e.activation(out=dst[:, hsl(hq), :], in_=psR[bank][:].rearrange("p (h t) -> p h t", h=4), func=AF.Copy),
                                  reads=[("psR", bank)], writes=[dkey])
                        def evac_add(dst, dkey, bank, addsrc, akey, hq):
                            kb.op("dve", lambda e: e.tensor_tensor(out=dst[:, hsl(hq), :], in0=psR[bank][:].rearrange("p (h t) -> p h t", h=4),
                                                                  in1=addsrc[:, hsl(hq), :], op=ALU.add),
                                  reads=[("psR", bank), akey], writes=[dkey])
                        cur = 0
                        for it in range(4):
                            nx = 1 - cur
                            for hq in range(2):
                                kP, kQ, kV, kU = ("Pw", cur, hq), ("Qw", cur, hq), ("Ml", cur, hq), ("MT", cur, hq)
                                kP2, kQ2, kV2, kU2 = ("Pw", nx, hq), ("Qw", nx, hq), ("Ml", nx, hq), ("MT", nx, hq)
                                b1 = nxt(); mm4(b1, Qw[cur], Pw[cur], hq, [kQ, kP]); evac_copy(Pw[nx], kP2, b1, hq)
                                b2 = nxt(); mm4(b2, Pw[cur], Qw[cur], hq, [kQ, kP]); evac_copy(Qw[nx], kQ2, b2, hq)
                                b3 = nxt(); mm4(b3, Pw[nx], MT[cur], hq, [kP2, kU]); evac_add(MT[nx], kU2, b3, MT[cur], kU, hq)
                                b4 = nxt(); mm4(b4, Qw[nx], Ml[cur], hq, [kQ2, kV]); evac_add(Ml[nx], kV2, b4, Ml[cur], kV, hq)
                            cur = nx
                        for lvl, mname in ((64, "of64"), (128, "of128")):
                            nx = 1 - cur
                            for hq in range(2):
                                kV, kU = ("Ml", cur, hq), ("MT", cur, hq)
                                kV2, kU2 = ("Ml", nx, hq), ("MT", nx, hq)
                                kb.op("pool", lambda e, hq=hq, mname=mname: e.tensor_tensor(out=Pw[0][:, hsl(hq), :], in0=L0f[:, hsl(hq), :], in1=msk(mname), op=ALU.mult),
                                      reads=[("L0f", hq), "Cb"], writes=[("Pw", 0, hq)])
                                b1 = nxt(); mm4(b1, Pw[0], MT[cur], hq, [("Pw", 0, hq), kU]); evac_copy(Pw[1], ("Pw", 1, hq), b1, hq)
                                b2 = nxt(); mm4(b2, Ml[cur], Pw[1], hq, [kV, ("Pw", 1, hq)])
                                if lvl == 128:
                                    kb.op("dve", lambda e, b2=b2, hq=hq, cur=cur, h0=h0: e.tensor_tensor(
                                        out=MTb[:, h0 + 4 * hq:h0 + 4 * hq + 4, :], in0=psR[b2][:].rearrange("p (h t) -> p h t", h=4),
                                        in1=MT[cur][:, hsl(hq), :], op=ALU.add), reads=[("psR", b2), kU], writes=["MTb"])
                                else:
                                    evac_add(MT[nx], kU2, b2, MT[cur], kU, hq)
                                    kb.op("pool", lambda e, hq=hq, mname=mname: e.tensor_tensor(out=Qw[0][:, hsl(hq), :], in0=N0f[:, hsl(hq), :], in1=msk(mname), op=ALU.mult),
                                          reads=[("N0f", hq), "Cb"], writes=[("Qw", 0, hq)])
                                    b3 = nxt(); mm4(b3, Qw[0], Ml[cur], hq, [("Qw", 0, hq), kV]); evac_copy(Qw[1], ("Qw", 1, hq), b3, hq)
                                    b4 = nxt(); mm4(b4, MT[cur], Qw[1], hq, [kU, ("Qw", 1, hq)]); evac_add(Ml[nx], kV2, b4, Ml[cur], kV, hq)
                            cur = nx
                    MTf = MTb
                    mtk = "MTb"
                    if stop_after == "Cinv":
                        kb.finish("sp"); return nc
                    for hh in range(2):
                        px = nxt()
                        fns = []
                        for h in range(8 * hh, 8 * hh + 8):
                            j_, hp_ = h // 2, h % 2
                            o = psR[px][:, (h % 8) * 64:(h % 8) * 64 + 64]
                            fns.append(lambda e, h=h, o=o: e.matmul(o, lhsT=KK_[:, h, 0, :], rhs=Vtm[:, h * 64:(h + 1) * 64],
                                                                    start=True, stop=False))
                            fns.append(lambda e, j_=j_, hp_=hp_, o=o: e.matmul(o, lhsT=AR[:, j_, c, 0, :],
                                                                               rhs=Hb[:, j_, hp_ * 64:(hp_ + 1) * 64],
                                                                               start=False, stop=True))
                        kb.mm_group(fns, reads=["KK", "Vtm", "AR", "Hb"], writes=[("psR", px)])
                        kb.op("act", lambda e, px=px, hh=hh: e.activation(out=Xs[:, hh * 512:(hh + 1) * 512], in_=psR[px][:], func=AF.Copy),
                              reads=[("psR", px)], writes=["Xs"])
                    for hh in range(2):
                        pu = nxt()
                        kb.mm_group([(lambda e, h=h, pu=pu: e.matmul(psR[pu][:, (h % 8) * 64:(h % 8) * 64 + 64], lhsT=MTf[:, h, :],
                                                                    rhs=Xs[:, h * 64:(h + 1) * 64], start=True, stop=True))
                                     for h in range(8 * hh, 8 * hh + 8)], reads=[mtk, "Xs"], writes=[("psR", pu)])
                        kb.op("act", lambda e, pu=pu, hh=hh: e.activation(out=Us[:, hh * 512:(hh + 1) * 512], in_=psR[pu][:], func=AF.Copy),
                              reads=[("psR", pu)], writes=["Us"])
                    for jq in range(2):
                        py = nxt()
                        fns = []
                        for j_ in range(4 * jq, 4 * jq + 4):
                            for hp_ in range(2):
                                h = 2 * j_ + hp_
                                o = psR[py][hp_ * 64:(hp_ + 1) * 64, (j_ % 4) * 128:(j_ % 4) * 128 + 128]
                                tpos = (0, hp_ * 64)
                                fns.append(lambda e, o=o, j_=j_, hp_=hp_, tpos=tpos: e.matmul(
                                    o, lhsT=Hb[:, j_, hp_ * 64:(hp_ + 1) * 64], rhs=AR[:, j_, c, 1, :], start=True, stop=False,
                                    tile_position=tpos))
                                fns.append(lambda e, o=o, h=h, tpos=tpos: e.matmul(
                                    o, lhsT=Us[:, h * 64:(h + 1) * 64], rhs=RB_[:, h, :], start=False, stop=False, tile_position=tpos))
                                fns.append(lambda e, o=o, h=h, tpos=tpos: e.matmul(
                                    o, lhsT=Vtm[:, h * 64:(h + 1) * 64], rhs=KK_[:, h, 1, :], start=False, stop=True, tile_position=tpos))
                        kb.mm_group(fns, reads=["Hb", "AR", "Us", "RB", "Vtm", "KK"], writes=[("psR", py)])
                        kb.op("act", lambda e, py=py, jq=jq: e.activation(out=yT[:, 4 * jq:4 * jq + 4, ccol],
                                                                          in_=psR[py][:].rearrange("p (j t) -> p j t", j=4), func=AF.Copy),
                              reads=[("psR", py)], writes=["yT"])
                    if stop_after == "Cy":
                        kb.finish("sp"); return nc
                    for jq in range(2):
                        ph = nxt()
                        fns = []
                        for j_ in range(4 * jq, 4 * jq + 4):
                            o = psR[ph][:, (j_ % 4) * 128:(j_ % 4) * 128 + 128]
                            fns.append(lambda e, o=o, j_=j_: e.matmul(o, lhsT=Btm[:, j_ * 128:(j_ + 1) * 128], rhs=Us[:, j_ * 128:(j_ + 1) * 128],
                                                                      start=True, stop=False))
                            fns.append(lambda e, o=o, j_=j_: e.matmul(o, lhsT=Ktm[:, j_ * 128:(j_ + 1) * 128], rhs=Vtm[:, j_ * 128:(j_ + 1) * 128],
                                                                      start=False, stop=True))
                        kb.mm_group(fns, reads=["Btm", "Us", "Ktm", "Vtm"], writes=[("psR", ph)])
                        hv = Hf[:, 4 * jq:4 * jq + 4, :]
                        kb.op("dve", lambda e, ph=ph, hv=hv: e.tensor_tensor(
                            out=tp[0][:].rearrange("p (j t) -> p j t", j=4), in0=psR[ph][:].rearrange("p (j t) -> p j t", j=4),
                            in1=cc("blk1").unsqueeze(1).to_broadcast([128, 4, 128]), op=ALU.mult),
                            reads=[("psR", ph), "C"], writes=["xr"])
                        kb.op("dve", lambda e, hv=hv: e.tensor_tensor(out=tp[0][:].rearrange("p (j t) -> p j t", j=4),
                                                                     in0=tp[0][:].rearrange("p (j t) -> p j t", j=4), in1=hv, op=ALU.add),
                              reads=["xr", "Hf"], writes=["xr"])
                        kb.op("dve", lambda e, hv=hv, jq=jq: e.tensor_tensor(
                            out=hv, in0=tp[0][:].rearrange("p (j t) -> p j t", j=4),
                            in1=Pc[:, 4 * jq:4 * jq + 4, c:c + 1].to_broadcast([128, 4, 128]), op=ALU.mult),
                            reads=["xr", "Pc"], writes=["Hf"])
                        kb.op("act", lambda e, hv=hv, jq=jq: e.activation(out=Hb[:, 4 * jq:4 * jq + 4, :], in_=hv, func=AF.Copy),
                              reads=["Hf"], writes=["Hb"])
                if bi == 0 and "yT0" in dbg:
                    kb.dma("sp", dbg["yT0"], yT[:], reads=["yT"])
                    kb.dma("sp", dbg["bon0"], bon[:], reads=["bon"])
                    kb.dma("sp", dbg["kT0"], kT[:], reads=["kT"])
                    kb.dma("sp", dbg["AR0"], AR[:].rearrange("p j c a t -> p (j c a t)"), reads=["AR"])
                gn_out(yT, bon, g16, n, c0, ["yT", "bon", "g16"], dict(psR=psR, nxt=nxt, tp=tp, yib=yib,
                                                                     setB=(ld[1], ld[2], yibB, "xk2", "xv2", "yibB")))
            for jq in range(2):
                pw_ = nxt()
                kb.mm_group([(lambda e, j=j, pw_=pw_: e.transpose(out=psR[pw_][:, (j % 4) * 128:(j % 4) * 128 + 128], in_=Hf[:, j, :],
                                                                  identity=cc("ident"))) for j in range(4 * jq, 4 * jq + 4)],
                            reads=["Hf", "C"], writes=[("psR", pw_)])
                kb.op("act", lambda e, pw_=pw_, jq=jq: e.activation(out=yT[:, 4 * jq:4 * jq + 4, 0:128],
                                                                    in_=psR[pw_][:].rearrange("p (j t) -> p j t", j=4), func=AF.Copy),
                      reads=[("psR", pw_)], writes=["yT"])
            for hp_ in range(2):
                kb.dma("sp", o_wkv_p.rearrange("(j a) v k -> a v j k", a=2)[hp_],
                       yT[hp_ * 64:(hp_ + 1) * 64, :, hp_ * 64:(hp_ + 1) * 64], reads=["yT"], writes=["o_wkv_p"])
            if "YI_d" in dbg:
                kb.dma("sp", dbg["YI_d"], YI_d[:, 0:SEQ], reads=[("YI", b[0]) for b in BLOCKS[:4]])
        if stop_after == "C":
            kb.finish("sp"); return nc
        kb.barrier()

        with ExitStack() as e2:
            sb2 = lambda n, sh, dt=F32: e2.enter_context(nc.sbuf_tensor(n, list(sh), dt))
            ps2_ = lambda n, sh, dt=F32: e2.enter_context(nc.psum_tensor(n, list(sh), dt))
            HALF = NS // 4
            St = [sb2("St%d" % i, [128, HALF, 8, HS]) for i in range(2)]
            opnd = [[sb2("op%d_%d" % (i, q), [128, HALF, 8, HS], F32 if q == 0 else BF16) for q in range(5)] for i in range(2)]
            tmpS = [sb2("tmpS%d" % i, [128, HALF, 8, HS]) for i in range(2)]
            sa = [sb2("sa%d" % i, [128, HALF, 8]) for i in range(2)]
            y_sT = sb2("y_sT", [128, 8, NTS])
            tp2 = [sb2("tq%d" % i, [128, NTS]) for i in range(3)]
            yib2 = sb2("yib2", [128, NTS], BF16)
            psR2 = [ps2_("psQ%d" % i, [128, 512]) for i in range(2)]
            r2 = [0]
            def nxt2():
                r2[0] += 1
                return r2[0] % 2
            for qd in range(4):
                hf_ = qd % 2
                en = "dve"
                s0 = qd * HALF
                S_ = St[hf_]
                sk = ("St", hf_)
                for hp_ in range(2):
                    kb.dma("sp", S_[hp_ * 64:(hp_ + 1) * 64, :, :, :],
                           s_wkv[s0:s0 + HALF].rearrange("s (g a) v k -> a v s g k", a=2)[hp_], writes=[sk])
                for t in range(TS):
                    r0 = t * NS + s0
                    for q in range(5):
                        for hp_ in range(2):
                            src = RW_d[q, hp_, r0:r0 + HALF, :, :]
                            kb.dma("sp" if q == 0 else "pool", opnd[hf_][q][hp_ * 64:(hp_ + 1) * 64, :, :, :],
                                   src.unsqueeze(0).to_broadcast([64, HALF, 8, HS]), reads=["RW"], writes=[("op", hf_, q)])
                    W_, K_, B_, Kk_, R_ = opnd[hf_]
                    T_ = tmpS[hf_]
                    tk_ = ("tmpS", hf_)
                    sak = ("sa", hf_)
                    vb = v_s[:, :, r0:r0 + HALF].rearrange("p g s -> p s g").unsqueeze(3).to_broadcast([128, HALF, 8, HS])
                    kb.op(en, lambda e: e.tensor_tensor(out=T_[:], in0=S_[:], in1=K_[:], op=ALU.mult),
                          reads=[sk, ("op", hf_, 1)], writes=[tk_])
                    kb.op("dve", lambda e: e.tensor_reduce(out=sa[hf_][:], in_=T_[:], axis=AX.X, op=ALU.add), reads=[tk_], writes=[sak])
                    kb.op("pool", lambda e: e.tensor_tensor(out=S_[:], in0=S_[:], in1=W_[:], op=ALU.mult),
                          reads=[sk, ("op", hf_, 0)], writes=[sk])
                    kb.op(en, lambda e: e.tensor_tensor(out=T_[:], in0=B_[:], in1=sa[hf_][:].unsqueeze(3).to_broadcast([128, HALF, 8, HS]),
                                                        op=ALU.mult), reads=[sak, ("op", hf_, 2)], writes=[tk_])
                    kb.op(en, lambda e: e.tensor_tensor(out=S_[:], in0=S_[:], in1=T_[:], op=ALU.add), reads=[sk, tk_], writes=[sk])
                    kb.op(en, lambda e: e.tensor_tensor(out=T_[:], in0=Kk_[:], in1=vb, op=ALU.mult),
                          reads=["v_s", ("op", hf_, 3)], writes=[tk_])
                    kb.op(en, lambda e: e.tensor_tensor(out=S_[:], in0=S_[:], in1=T_[:], op=ALU.add), reads=[sk, tk_], writes=[sk])
                    kb.op(en, lambda e: e.tensor_tensor(out=T_[:], in0=S_[:], in1=R_[:], op=ALU.mult),
                          reads=[sk, ("op", hf_, 4)], writes=[tk_])
                    kb.op("dve", lambda e: e.tensor_reduce(out=y_sT[:, :, r0:r0 + HALF].rearrange("p g s -> p s g"), in_=T_[:],
                                                        axis=AX.X, op=ALU.add), reads=[tk_], writes=["y_sT"])
                for hp_ in range(2):
                    kb.dma("sp", o_wkv_s[s0:s0 + HALF].rearrange("s (g a) v k -> a v s g k", a=2)[hp_],
                           S_[hp_ * 64:(hp_ + 1) * 64, :, :, :], reads=[sk], writes=["o_wkv_s"])
            gn_out(y_sT, bon_s, g_s, NTS, SEQ, ["y_sT", "bon", "g16"], dict(psR=psR2, nxt=nxt2, tp=tp2, yib=yib2))
            if "YI_s" in dbg:
                kb.dma("sp", dbg["YI_s"], YI_d[:, SEQ:TT], reads=[("YI", SEQ)])
        if stop_after == "C2":
            kb.finish("sp"); return nc
        kb.barrier()

        X1_d = dt_scr("X1_d", [D, TT])
        HF_d = dt_scr("HF_d", [D, TT], BF16)
        GT_d = dt_scr("GT_d", [NE, TT], BF16)
        rb_row = nc.dram_tensor("rbias_row", [1, NE], F32, kind="ExternalInput").ap()
        sc_raw = nc.dram_tensor("sc_raw", [NS, 30, D], F32, kind="ExternalInput").ap()
        with ExitStack() as ed:
            sbd = lambda n, sh, dt=F32: ed.enter_context(nc.sbuf_tensor(n, list(sh), dt))
            psd = lambda n, sh, dt=F32: ed.enter_context(nc.psum_tensor(n, list(sh), dt))
            wor = sbd("wor", [128, 8, D], BF16); wco = sbd("wco", [128, 8, D], BF16); wou = sbd("wou", [128, 8, D], BF16)
            wr = sbd("wr", [128, 8, NE])
            rbt = sbd("rbt", [128, NE])
            gl = [sbd("gl%d" % i, [128, 544]) for i in range(2)]
            glb = [sbd("glb%d" % i, [128, 544], BF16) for i in range(2)]
            dg = [sbd("dg%d" % i, [128, CW, 128], BF16) for i in range(2)]
            dwT = sbd("dwT", [128, 8, 512]); sqd = sbd("sqd", [128, 8, 512])
            uT = sbd("uT", [128, 8, 512], BF16); yiT = sbd("yiT", [128, 8, 512], BF16); mT = sbd("mT", [128, 8, 512], BF16)
            zT = sbd("zT", [128, 8, 512]); xTd = sbd("xTd", [128, 8, 512]); hfT = sbd("hfT", [128, 8, 512], BF16)
            sga = [sbd("sga%d" % i, [128, 512], BF16) for i in range(2)]
            sgb = [sbd("sgb%d" % i, [128, 512], BF16) for i in range(2)]
            t1 = sbd("t1", [128, 512]); t2 = sbd("t2", [128, 512]); rs = sbd("rs", [128, 512])
            cvrow = sbd("cvrow", [128, D])
            sc = sbd("sc", [128, NE]); bsd = sbd("bsd", [128, NE]); top8 = sbd("top8", [128, 8]); ssum = sbd("ssum", [128, 1])
            gtb = sbd("gtb", [NE, 128], BF16)
            psD = [psd("psD%d" % i, [128, 512]) for i in range(6)]
            psX = psd("psX", [128, D])
            dcount = [0]
            def nxd():
                dcount[0] += 1
                return dcount[0] % 6
            for (wt, src, nm) in [(wor, w_o_rwkv, "wor"), (wco, w_conv_out, "wco"), (wou, w_out, "wou")]:
                for hh in range(2):
                    kb.dma("pool", wt[:, :, hh * 512:(hh + 1) * 512],
                           src[:, hh * 512:(hh + 1) * 512].rearrange("(k p) n -> p k n", p=128), writes=[nm])
            kb.dma("sp", wr[:], w_router.rearrange("(k p) n -> p k n", p=128), writes=["wr"])
            kb.dma("sp", rbt[:], rb_row.partition_broadcast(128), writes=["rbt"])

            def stats_rstd(src, n, eps, div, skey):
                kb.op("act", lambda e: e.activation(out=sqd[:, :, 0:n], in_=src[:, :, 0:n], func=AF.Square), reads=[skey], writes=["sqd"])
                p_ = nxd()
                kb.op("dve", lambda e: e.tensor_reduce(out=t2[:, 0:n], in_=sqd[:, :, 0:n].rearrange("p k n -> p n k"), axis=AX.X, op=ALU.add),
                      reads=["sqd"], writes=["t2"])
                kb.mm_group([lambda e, p_=p_: e.matmul(psD[p_][:, 0:n], lhsT=cc("ones"), rhs=t2[:, 0:n], start=True, stop=True)],
                            reads=["t2", "C"], writes=[("psD", p_)])
                kb.op("act", lambda e, p_=p_: e.activation(out=rs[:, 0:n], in_=psD[p_][:, 0:n], func=AF.Ln, scale=1.0 / div, bias=eps),
                      reads=[("psD", p_)], writes=["rs"])
                kb.op("act", lambda e: e.activation(out=rs[:, 0:n], in_=rs[:, 0:n], func=AF.Exp, scale=-0.5), reads=["rs"], writes=["rs"])

            for bi, (c0, n, sample) in enumerate(BLOCKS):
                wd = NS if sample else 1
                hist = 30 * wd
                gcl = gcol(c0, sample)
                for j in range(8):
                    g_ = gl[j % 2]
                    gk = ("gl", j % 2)
                    kb.dma("sp", g_[:, 0:hist + n], G_d[j * 128:(j + 1) * 128, gcl - hist:gcl + n],
                           reads=[("G", bi), "G_hist"] + ([("G", bi - 1)] if (bi > 0 and not sample) else []), writes=[gk])
                    ds_ = j % 2
                    kb.op("act", lambda e, g_=g_, ds_=ds_: e.activation(out=glb[ds_][:, 0:hist + n], in_=g_[:, 0:hist + n], func=AF.Copy),
                          reads=[gk], writes=[("glb", ds_)])
                    kb.op("dve", lambda e, j=j, ds_=ds_: e.tensor_tensor(
                        out=dg[ds_][:], in0=cc("ident").unsqueeze(1).to_broadcast([128, CW, 128]),
                        in1=pc(P, "dw_w", j * CW, CW).unsqueeze(2).to_broadcast([128, CW, 128]), op=ALU.mult),
                        reads=["C", "P"], writes=[("dg", ds_)])
                    pcv = nxd()
                    kb.mm_group([(lambda e, tap=tap, pcv=pcv, ds_=ds_: e.matmul(psD[pcv][:, 0:n], lhsT=dg[ds_][:, tap, :],
                                                                                rhs=glb[ds_][:, tap * wd:tap * wd + n],
                                                                                start=(tap == 0), stop=(tap == CW - 1)))
                                 for tap in range(CW)], reads=[("dg", ds_), ("glb", ds_)], writes=[("psD", pcv)])
                    kb.op("act", lambda e, pcv=pcv, j=j: e.activation(out=dwT[:, j, 0:n], in_=psD[pcv][:, 0:n], func=AF.Identity,
                                                                      bias=pc(P, "dw_b", j)), reads=[("psD", pcv), "P"], writes=[("dwT", j)])
                    if bi == 3:
                        kb.mm_group([lambda e, g_=g_, j=j: e.transpose(out=psX[0:32, j * 128:(j + 1) * 128], in_=g_[:, 510:542],
                                                                       identity=cc("ident"))], reads=[gk, "C"], writes=["psX"])
                    if sample:
                        kb.mm_group([lambda e, g_=g_, j=j: e.transpose(out=psX[0:NTS, j * 128:(j + 1) * 128], in_=g_[:, hist:hist + NTS],
                                                                       identity=cc("ident"))], reads=[gk, "C"], writes=["psX"])
                if sample:
                    kb.op("act", lambda e: e.activation(out=cvrow[0:NTS, :], in_=psX[0:NTS, :], func=AF.Copy), reads=["psX"], writes=["cvrow"])
                    for t in range(TS):
                        kb.dma("sp", o_cv_s[:, 26 + t, :], cvrow[t * NS:(t + 1) * NS, :], reads=["cvrow"], writes=["o_cv_s"])
                    kb.dma("sp", o_cv_s[:, 0:26, :], sc_raw[:, 4:30, :], writes=["o_cv_s_h"])
                if bi == 3:
                    kb.op("act", lambda e: e.activation(out=cvrow[0:32, :], in_=psX[0:32, :], func=AF.Copy), reads=["psX"], writes=["cvrow"])
                    kb.dma("sp", o_cv_p, cvrow[2:32, :], reads=["cvrow"], writes=["o_cv_p"])
                dkeys = [("dwT", j) for j in range(8)]
                pm_ = nxd()
                kb.op("dve", lambda e: e.tensor_reduce(out=t2[:, 0:n], in_=dwT[:, :, 0:n].rearrange("p k n -> p n k"), axis=AX.X, op=ALU.add),
                      reads=dkeys, writes=["t2"])
                kb.mm_group([lambda e, pm_=pm_: e.matmul(psD[pm_][:, 0:n], lhsT=cc("ones"), rhs=t2[:, 0:n], start=True, stop=True)],
                            reads=["t2", "C"], writes=[("psD", pm_)])
                kb.op("dve", lambda e, pm_=pm_: e.tensor_scalar_mul(out=t1[:, 0:n], in0=psD[pm_][:, 0:n], scalar1=-1.0 / D),
                      reads=[("psD", pm_)], writes=["t1"])
                kb.op("dve", lambda e: e.tensor_tensor(out=dwT[:, :, 0:n], in0=dwT[:, :, 0:n],
                                                      in1=t1[:, 0:n].unsqueeze(1).to_broadcast([128, 8, n]), op=ALU.add),
                      reads=dkeys + ["t1"], writes=dkeys)
                stats_rstd(dwT, n, 1e-5, D, ("dwT", 0))
                kb.op("dve", lambda e: e.tensor_tensor(out=dwT[:, :, 0:n], in0=dwT[:, :, 0:n],
                                                      in1=rs[:, 0:n].unsqueeze(1).to_broadcast([128, 8, n]), op=ALU.mult),
                      reads=dkeys + ["rs", "sqd"], writes=dkeys)
                for j in range(8):
                    kb.op("act", lambda e, j=j: e.activation(out=uT[:, j, 0:n], in_=dwT[:, j, 0:n], func=AF.Silu,
                                                             scale=pc(P, "ln_g", j), bias=pc(P, "ln_b", j)),
                          reads=[("dwT", j), "P"], writes=["uT"])
                if "uT" in dbg and bi == 0:
                    kb.dma("sp", dbg["uT"], uT[:], reads=["uT"])
                kb.dma("sp", yiT[:, :, 0:n], YI_d[:, c0:c0 + n].rearrange("(k p) c -> p k c", p=128), reads=[("YI", c0)], writes=["yiT"])
                for o in range(8):
                    oc = slice(o * 128, (o + 1) * 128)
                    pa_, pb_ = nxd(), nxd()
                    kb.mm_group([(lambda e, k=k, pa_=pa_: e.matmul(psD[pa_][:, 0:n], lhsT=wor[:, k, oc], rhs=yiT[:, k, 0:n],
                                                                  start=(k == 0), stop=(k == 7))) for k in range(8)],
                                reads=["wor", "yiT"], writes=[("psD", pa_)])
                    kb.mm_group([(lambda e, k=k, pb_=pb_: e.matmul(psD[pb_][:, 0:n], lhsT=wco[:, k, oc], rhs=uT[:, k, 0:n],
                                                                  start=(k == 0), stop=(k == 7))) for k in range(8)],
                                reads=["wco", "uT"], writes=[("psD", pb_)])
                    s_ = o % 2
                    kb.dma("sp", sga[s_][:, 0:n], SG_d[o * 128:(o + 1) * 128, c0:c0 + n], reads=[("SG", bi)], writes=[("sga", s_)])
                    kb.dma("sp", sgb[s_][:, 0:n], SG_d[D + o * 128:D + (o + 1) * 128, c0:c0 + n], reads=[("SG", bi)], writes=[("sgb", s_)])
                    kb.op("dve", lambda e, pa_=pa_, s_=s_: e.tensor_tensor(out=t1[:, 0:n], in0=psD[pa_][:, 0:n], in1=sga[s_][:, 0:n], op=ALU.mult),
                          reads=[("psD", pa_), ("sga", s_)], writes=["t1"])
                    kb.op("dve", lambda e, pb_=pb_, s_=s_: e.tensor_tensor(out=t2[:, 0:n], in0=psD[pb_][:, 0:n], in1=sgb[s_][:, 0:n], op=ALU.mult),
                          reads=[("psD", pb_), ("sgb", s_)], writes=["t2"])
                    kb.op("dve", lambda e, o=o: e.tensor_tensor(out=mT[:, o, 0:n], in0=t1[:, 0:n], in1=t2[:, 0:n], op=ALU.add),
                          reads=["t1", "t2"], writes=["mT"])
                if "mT" in dbg and bi == 0:
                    kb.dma("sp", dbg["mT"], mT[:], reads=["mT"])
                for o in range(8):
                    oc = slice(o * 128, (o + 1) * 128)
                    pz = nxd()
                    kb.mm_group([(lambda e, k=k, pz=pz: e.matmul(psD[pz][:, 0:n], lhsT=wou[:, k, oc], rhs=mT[:, k, 0:n],
                                                                start=(k == 0), stop=(k == 7))) for k in range(8)],
                                reads=["wou", "mT"], writes=[("psD", pz)])
                    kb.op("act", lambda e, pz=pz, o=o: e.activation(out=zT[:, o, 0:n], in_=psD[pz][:, 0:n], func=AF.Copy),
                          reads=[("psD", pz)], writes=["zT"])
                stats_rstd(zT, n, 1e-6, D, "zT")
                kb.dma("sp", xTd[:, :, 0:n], XT_d[:, c0:c0 + n].rearrange("(k p) c -> p k c", p=128), reads=[("XT", bi)], writes=["xTd"])
                kb.op("dve", lambda e: e.tensor_tensor(out=zT[:, :, 0:n], in0=zT[:, :, 0:n],
                                                      in1=rs[:, 0:n].unsqueeze(1).to_broadcast([128, 8, n]), op=ALU.mult),
                      reads=["zT", "rs", "sqd"], writes=["zT"])
                kb.op("dve", lambda e: e.tensor_tensor(out=view(zT[:, :, 0:n], sample), in0=view(zT[:, :, 0:n], sample),
                                                      in1=bcast_mod(Gm, 0, sample, n), op=ALU.mult), reads=["zT", "Gm"], writes=["zT"])
                kb.op("dve", lambda e: e.tensor_tensor(out=xTd[:, :, 0:n], in0=xTd[:, :, 0:n], in1=zT[:, :, 0:n], op=ALU.add),
                      reads=["zT", "xTd"], writes=["xTd"])
                kb.dma("sp", X1_d[:, c0:c0 + n].rearrange("(k p) c -> p k c", p=128), xTd[:, :, 0:n], reads=["xTd"], writes=[("X1", bi)])
                stats_rstd(xTd, n, 1e-6, D, "xTd")
                kb.op("dve", lambda e: e.tensor_tensor(out=zT[:, :, 0:n], in0=xTd[:, :, 0:n],
                                                      in1=rs[:, 0:n].unsqueeze(1).to_broadcast([128, 8, n]), op=ALU.mult),
                      reads=["xTd", "rs", "zT", "sqd"], writes=["zT"])
                kb.op("dve", lambda e: e.tensor_tensor(out=view(zT[:, :, 0:n], sample), in0=view(zT[:, :, 0:n], sample),
                                                      in1=bcast_mod(Af, 0, sample, n), op=ALU.mult), reads=["zT", "Af"], writes=["zT"])
                kb.op("dve", lambda e: e.tensor_tensor(out=view(zT[:, :, 0:n], sample), in0=view(zT[:, :, 0:n], sample),
                                                      in1=bcast_mod(modT, 24, sample, n), op=ALU.add), reads=["zT", "modT"], writes=["zT"])
                kb.op("act", lambda e: e.activation(out=hfT[:, :, 0:n], in_=zT[:, :, 0:n], func=AF.Copy), reads=["zT"], writes=["hfT"])
                kb.dma("sp", HF_d[:, c0:c0 + n].rearrange("(k p) c -> p k c", p=128), hfT[:, :, 0:n], reads=["hfT"], writes=[("HF", bi)])
                m_ = 64 if sample else 128
                for i in range(n // m_):
                    tc_ = slice(i * m_, (i + 1) * m_)
                    pr_ = nxd()
                    kb.mm_group([(lambda e, k=k, pr_=pr_: e.matmul(psD[pr_][0:m_, 0:NE], lhsT=zT[:, k, tc_], rhs=wr[:, k, :],
                                                                  start=(k == 0), stop=(k == 7))) for k in range(8)],
                                reads=["zT", "wr"], writes=[("psD", pr_)])
                    kb.op("act", lambda e, pr_=pr_: e.activation(out=sc[0:m_, :], in_=psD[pr_][0:m_, 0:NE], func=AF.Sigmoid),
                          reads=[("psD", pr_)], writes=["sc"])
                    kb.op("dve", lambda e: e.tensor_tensor(out=bsd[0:m_, :], in0=sc[0:m_, :], in1=rbt[0:m_, :], op=ALU.add),
                          reads=["sc", "rbt"], writes=["bsd"])
                    kb.op("dve", lambda e: e.max(out=top8[0:m_, :], in_=bsd[0:m_, :]), reads=["bsd"], writes=["top8"])
                    kb.op("dve", lambda e: e.tensor_scalar(out=bsd[0:m_, :], in0=bsd[0:m_, :], scalar1=top8[0:m_, 5:6], scalar2=None,
                                                          op0=ALU.is_ge), reads=["bsd", "top8"], writes=["bsd"])
                    kb.op("dve", lambda e: e.tensor_tensor(out=sc[0:m_, :], in0=sc[0:m_, :], in1=bsd[0:m_, :], op=ALU.mult),
                          reads=["sc", "bsd"], writes=["sc"])
                    kb.op("dve", lambda e: e.tensor_reduce(out=ssum[0:m_, :], in_=sc[0:m_, :], axis=AX.X, op=ALU.add),
                          reads=["sc"], writes=["ssum"])
                    kb.op("dve", lambda e: e.reciprocal(out=ssum[0:m_, :], in_=ssum[0:m_, :]), reads=["ssum"], writes=["ssum"])
                    kb.op("dve", lambda e: e.tensor_scalar(out=sc[0:m_, :], in0=sc[0:m_, :], scalar1=ssum[0:m_, 0:1], scalar2=2.5,
                                                          op0=ALU.mult, op1=ALU.mult), reads=["sc", "ssum"], writes=["sc"])
                    pt_ = nxd()
                    kb.mm_group([lambda e, pt_=pt_: e.transpose(out=psD[pt_][0:NE, 0:m_], in_=sc[0:m_, :], identity=cc("ident")[0:m_, 0:m_])],
                                reads=["sc", "C"], writes=[("psD", pt_)])
                    kb.op("act", lambda e, pt_=pt_: e.activation(out=gtb[:, 0:m_], in_=psD[pt_][0:NE, 0:m_], func=AF.Copy),
                          reads=[("psD", pt_)], writes=["gtb"])
                    kb.dma("sp", GT_d[:, c0 + i * m_:c0 + (i + 1) * m_], gtb[:, 0:m_], reads=["gtb"], writes=[("GT", bi)])
            if "X1_d" in dbg:
                kb.dma("sp", dbg["X1_d"], X1_d, reads=[("X1", b) for b in range(5)])
            if "HF_d" in dbg:
                kb.dma("sp", dbg["HF_d"], HF_d, reads=[("HF", b) for b in range(5)])
            if "GT_d" in dbg:
                kb.dma("sp", dbg["GT_d"], GT_d, reads=[("GT", b) for b in range(5)])
        if stop_after == "DE":
            kb.finish("sp"); return nc
        kb.barrier()

        sel_in = nc.dram_tensor("sel", [NE, NE * 128], F32, kind="ExternalInput").ap()
        TB = [(i * 512, 512) for i in range(4)] + [(SEQ, NTS)]
        with ExitStack() as ef:
            sbf = lambda n, sh, dt=F32: ef.enter_context(nc.sbuf_tensor(n, list(sh), dt))
            psf = lambda n, sh, dt=F32: ef.enter_context(nc.psum_tensor(n, list(sh), dt))
            hfA = sbf("hfA", [128, 8, TT], BF16)
            gTA = sbf("gTA", [NE, TT], BF16)
            acc = sbf("acc", [128, 8, TT])
            selt = [sbf("selt%d" % i, [NE, 128], BF16) for i in range(3)]
            NWB = 3
            ew = ExitStack()
            sbw = lambda n, sh, dt=F32: ew.enter_context(nc.sbuf_tensor(n, list(sh), dt))
            wg_ = [sbw("wxg%d" % i, [128, 8, DE], BF16) for i in range(NWB)]
            wu_ = [sbw("wxu%d" % i, [128, 8, DE], BF16) for i in range(NWB)]
            wd_ = [sbw("wxd%d" % i, [128, 2, D], BF16) for i in range(NWB)]
            sG = [sbw("sG%d" % i, [128, 512]) for i in range(2)]
            gsb = [sbw("gsb%d" % i, [128, 512]) for i in range(2)]
            he32 = [sbw("he32_%d" % i, [128, 512]) for i in range(2)]
            heT2 = [[[sbw("heT%d_%d_%d" % (hb, g, mi), [128, 512], BF16) for mi in range(2)] for g in range(2)] for hb in range(2)]
            psM = [psf("psM%d" % i, [128, 512]) for i in range(4)]
            psO = [psf("psO%d" % i, [128, 512]) for i in range(4)]
            mcount = [0]
            def nxm():
                mcount[0] += 1
                return mcount[0] % 4
            kb.dma("sp", hfA[:], HF_d.rearrange("(k p) c -> p k c", p=128), reads=[("HF", b) for b in range(5)], writes=["hfA"])
            kb.dma("sp", gTA[:], GT_d, reads=[("GT", b) for b in range(5)], writes=["gTA"])
            kb.op("pool", lambda e: e.memset(acc[:], 0.0), writes=["acc"])
            NEX = NE + 1
            groups = [(2 * g, min(2 * g + 2, NEX)) for g in range((NEX + 1) // 2)]
            def load_w(e):
                sl = e % NWB
                if e < NE:
                    kb.dma("pool", selt[e % 3][:], sel_in[:, e * 128:(e + 1) * 128], writes=[("selt", e % 3)])
                kb.dma("pool", wg_[sl][:], w_eg[e].rearrange("(k p) n -> p k n", p=128), writes=[("wg", sl)])
                kb.dma("pool", wu_[sl][:], w_eu[e].rearrange("(k p) n -> p k n", p=128), writes=[("wu", sl)])
                kb.dma("pool", wd_[sl][:], w_ed[e].rearrange("(m p) n -> p m n", p=128), writes=[("wd", sl)])
            e_loaded = 0
            for e in range(min(NWB, NEX)):
                load_w(e); e_loaded += 1
            tcount = 0
            bcount = 0
            pending = None
            for (ea, eb) in groups:
                for (c0, n) in TB:
                    cs_ = slice(c0, c0 + n)
                    hb = bcount % 2
                    bcount += 1
                    heT = heT2[hb]
                    for gi, e in enumerate(range(ea, eb)):
                        sl = e % NWB
                        if e < NE:
                            pgt = nxm()
                            kb.mm_group([lambda e_, e=e, pgt=pgt: e_.matmul(psM[pgt][:, 0:n], lhsT=selt[e % 3][:, :],
                                                                           rhs=gTA[:, cs_], start=True, stop=True)],
                                        reads=[("selt", e % 3), "gTA"], writes=[("psM", pgt)])
                            gsl = e % 2
                            kb.op("act", lambda e_, pgt=pgt, gsl=gsl: e_.activation(out=gsb[gsl][:, 0:n], in_=psM[pgt][:, 0:n], func=AF.Copy),
                                  reads=[("psM", pgt)], writes=[("gsb", gsl)])
                        for mi in range(2):
                            mc = slice(mi * 128, (mi + 1) * 128)
                            pg_, pu_ = nxm(), nxm()
                            kb.mm_group([(lambda e_, k=k, pg_=pg_, sl=sl: e_.matmul(psM[pg_][:, 0:n], lhsT=wg_[sl][:, k, mc], rhs=hfA[:, k, cs_],
                                                                                   start=(k == 0), stop=(k == 7))) for k in range(8)],
                                        reads=[("wg", sl), "hfA"], writes=[("psM", pg_)])
                            kb.mm_group([(lambda e_, k=k, pu_=pu_, sl=sl: e_.matmul(psM[pu_][:, 0:n], lhsT=wu_[sl][:, k, mc], rhs=hfA[:, k, cs_],
                                                                                   start=(k == 0), stop=(k == 7))) for k in range(8)],
                                        reads=[("wu", sl), "hfA"], writes=[("psM", pu_)])
                            ts_ = tcount % 2
                            tcount += 1
                            kb.op("act", lambda e_, pg_=pg_, ts_=ts_: e_.activation(out=sG[ts_][:, 0:n], in_=psM[pg_][:, 0:n], func=AF.Silu),
                                  reads=[("psM", pg_)], writes=[("sG", ts_)])
                            if e < NE:
                                kb.op("dve", lambda e_, pu_=pu_, ts_=ts_: e_.tensor_tensor(out=he32[ts_][:, 0:n], in0=psM[pu_][:, 0:n],
                                                                                         in1=sG[ts_][:, 0:n], op=ALU.mult),
                                      reads=[("psM", pu_), ("sG", ts_)], writes=[("he32", ts_)])
                                kb.op("pool", lambda e_, gsl=gsl, ts_=ts_, gi=gi, mi=mi: e_.tensor_tensor(
                                    out=heT[gi][mi][:, 0:n], in0=gsb[gsl][:, 0:n], in1=he32[ts_][:, 0:n], op=ALU.mult),
                                    reads=[("gsb", gsl), ("he32", ts_)], writes=[("heT", hb, gi, mi)])
                            else:
                                kb.op("dve", lambda e_, pu_=pu_, ts_=ts_, gi=gi, mi=mi: e_.tensor_tensor(
                                    out=heT[gi][mi][:, 0:n], in0=psM[pu_][:, 0:n], in1=sG[ts_][:, 0:n], op=ALU.mult),
                                    reads=[("psM", pu_), ("sG", ts_)], writes=[("heT", hb, gi, mi)])
                    def do_down(ea=ea, eb=eb, c0=c0, n=n, cs_=cs_, hb=hb, heT=heT):
                        ng = eb - ea
                        for op_ in range(2):
                            for oo in range(4):
                                o = op_ * 4 + oo
                                oc = slice(o * 128, (o + 1) * 128)
                                fns = []
                                cnt = 0
                                for gi, e in enumerate(range(ea, eb)):
                                    sl = e % NWB
                                    for mi in range(2):
                                        first, lastm = (cnt == 0), (cnt == 2 * ng - 1)
                                        cnt += 1
                                        fns.append(lambda e_, sl=sl, mi=mi, gi=gi, oo=oo, first=first, lastm=lastm: e_.matmul(
                                            psO[oo][:, 0:n], lhsT=wd_[sl][:, mi, oc], rhs=heT[gi][mi][:, 0:n], start=first, stop=lastm))
                                kb.mm_group(fns, reads=[("wd", e % NWB) for e in range(ea, eb)] + [("heT", hb, gi, mi) for gi in range(ng) for mi in range(2)],
                                            writes=[("psO", oo)])
                                en = "dve" if oo % 2 == 0 else "dve"
                                kb.op(en, lambda e_, o=o, oo=oo: e_.tensor_tensor(out=acc[:, o, cs_], in0=psO[oo][:, 0:n], in1=acc[:, o, cs_], op=ALU.add),
                                      reads=[("psO", oo), "acc"], writes=["acc"])
                    if pending is not None:
                        pending()
                    pending = do_down
                if pending is not None:
                    pending()
                    pending = None
                for e in range(ea, eb):
                    if e_loaded < NEX:
                        load_w(e_loaded); e_loaded += 1
            if "acc" in dbg:
                kb.dma("sp", dbg["acc"], acc[:], reads=["acc"])
            ew.close()
            kb.barrier()
            sqf = sbf("sqf", [128, 8, 512]); x1f = sbf("x1f", [128, 8, 512]); rsf = sbf("rsf", [128, 512])
            yrow = [sbf("yrow%d" % i, [128, D]) for i in range(1)]
            for bi, (c0, n, sample) in enumerate(BLOCKS):
                cs_ = slice(c0, c0 + n)
                kb.op("act", lambda e: e.activation(out=sqf[:, :, 0:n], in_=acc[:, :, cs_], func=AF.Square), reads=["acc"], writes=["sqf"])
                p_ = nxm()
                kb.op("dve", lambda e: e.tensor_reduce(out=rsf[:, 0:n], in_=sqf[:, :, 0:n].rearrange("p k n -> p n k"), axis=AX.X, op=ALU.add),
                      reads=["sqf"], writes=["rsf"])
                kb.mm_group([lambda e, p_=p_: e.matmul(psM[p_][:, 0:n], lhsT=cc("ones"), rhs=rsf[:, 0:n], start=True, stop=True)],
                            reads=["rsf", "C"], writes=[("psM", p_)])
                kb.op("act", lambda e, p_=p_: e.activation(out=rsf[:, 0:n], in_=psM[p_][:, 0:n], func=AF.Ln, scale=1.0 / D, bias=1e-6),
                      reads=[("psM", p_)], writes=["rsf"])
                kb.op("act", lambda e: e.activation(out=rsf[:, 0:n], in_=rsf[:, 0:n], func=AF.Exp, scale=-0.5), reads=["rsf"], writes=["rsf"])
                kb.dma("sp", x1f[:, :, 0:n], X1_d[:, cs_].rearrange("(k p) c -> p k c", p=128), reads=[("X1", bi)], writes=["x1f"])
                kb.op("dve", lambda e: e.tensor_tensor(out=sqf[:, :, 0:n], in0=acc[:, :, cs_],
                                                      in1=rsf[:, 0:n].unsqueeze(1).to_broadcast([128, 8, n]), op=ALU.mult),
                      reads=["acc", "rsf", "sqf"], writes=["sqf"])
                kb.op("dve", lambda e: e.tensor_tensor(out=view(sqf[:, :, 0:n], sample), in0=view(sqf[:, :, 0:n], sample),
                                                      in1=bcast_mod(Gf, 0, sample, n), op=ALU.mult), reads=["sqf", "Gf"], writes=["sqf"])
                kb.op("dve", lambda e: e.tensor_tensor(out=x1f[:, :, 0:n], in0=x1f[:, :, 0:n], in1=sqf[:, :, 0:n], op=ALU.add),
                      reads=["sqf", "x1f"], writes=["x1f"])
                m_ = 64 if sample else 128
                for i in range(n // m_):
                    ys_ = 0
                    for hh in range(2):
                        pt_ = psO[(2 * i + hh) % 4]
                        ptk = ("psO", (2 * i + hh) % 4)
                        kb.mm_group([(lambda e, k=k, pt_=pt_: e.transpose(out=pt_[0:m_, (k % 4) * 128:(k % 4) * 128 + 128],
                                                                          in_=x1f[:, k, i * m_:(i + 1) * m_], identity=cc("ident")))
                                     for k in range(4 * hh, 4 * hh + 4)], reads=["x1f", "C"], writes=[ptk])
                        kb.op("act" if hh == 0 else "dve",
                              (lambda e, pt_=pt_, hh=hh, ys_=ys_: e.activation(out=yrow[ys_][0:m_, hh * 512:(hh + 1) * 512], in_=pt_[0:m_, :], func=AF.Copy))
                              if hh == 0 else
                              (lambda e, pt_=pt_, hh=hh, ys_=ys_: e.tensor_copy(out=yrow[ys_][0:m_, hh * 512:(hh + 1) * 512], in_=pt_[0:m_, :])),
                              reads=[ptk], writes=[("yrow", ys_, hh)])
                    if not sample:
                        kb.dma("sp", y_p[c0 + i * 128:c0 + (i + 1) * 128, :], yrow[ys_][:, :], reads=[("yrow", ys_, 0), ("yrow", ys_, 1)],
                               writes=["y_p"])
                    else:
                        for t in range(TS):
                            kb.dma("sp", y_s[:, t, :], yrow[ys_][t * NS:(t + 1) * NS, :], reads=[("yrow", ys_, 0), ("yrow", ys_, 1)],
                                   writes=["y_s"])
        kb.finish("sp")
    return nc


def _prep_inputs(inp):
    f = lambda a: np.ascontiguousarray(np.asarray(a, dtype=np.float32))
    def col8(v):
        return f(v).reshape(-1, 128).T
    prm = np.zeros((128, NPC), np.float32)
    def put(name, arr):
        o, c = PCOLS[name]
        prm[:arr.shape[0], o:o + arr.shape[1]] = arr
    put("b_ada", f(inp["b_ada"]).reshape(48, 128).T)
    for n, k in [("mix_pre_g", "mix_pre_g"), ("mix_post_g", "mix_post_g"), ("ffn_pre_g", "ffn_pre_g"),
                 ("ffn_post_g", "ffn_post_g"), ("w0", "w0"), ("a0", "a0"), ("k_k", "k_k"), ("k_a", "k_a"),
                 ("gn_g", "gn_g"), ("gn_b", "gn_b"), ("dw_b", "dw_b"), ("ln_g", "conv_ln_g"), ("ln_b", "conv_ln_b")]:
        put(n, col8(inp[k]))
    put("r_k", col8(f(inp["r_k"]).reshape(-1)))
    mu = np.zeros(27 * 128, np.float32); mu[:NSHIFT] = f(inp["mu_shift"])
    put("mu", mu.reshape(27, 128).T)
    dww = f(inp["dw_w"])
    put("dw_w", dww.T.reshape(8, 128, CW).transpose(1, 0, 2).reshape(128, 8 * CW))
    rb = np.zeros((128, 1), np.float32)
    put("rbias", rb)
    cst = _consts()
    w_eg = np.concatenate([f(inp["w_exp_gate"]), f(inp["w_sh_gate"])[None]], 0)
    w_eu = np.concatenate([f(inp["w_exp_up"]), f(inp["w_sh_up"])[None]], 0)
    w_ed = np.concatenate([f(inp["w_exp_down"]), f(inp["w_sh_down"])[None]], 0)
    shared = {"sel": _sel(), "prm": prm, "cst": cst, "w_ada": f(inp["w_ada"]), "w_in": f(inp["w_in"]),
              "w_decay2": f(inp["w_decay2"]), "w_aaa2": f(inp["w_aaa2"]), "w_gate2": f(inp["w_gate2"]),
              "w_o_rwkv": f(inp["w_o_rwkv"]), "w_conv_out": f(inp["w_conv_out"]), "w_out": f(inp["w_out"]),
              "w_router": f(inp["w_router"]), "w_eg": w_eg, "w_eu": w_eu, "w_ed": w_ed,
              "rbias_row": f(inp["router_bias"]).reshape(1, NE)}
    maps = []
    for c in range(NCORES):
        sl = slice(c * NS, (c + 1) * NS)
        m = dict(shared)
        m["x_p"] = f(inp["x_prompt"][c])
        m["x_s"] = f(inp["x_sample"][sl])
        cTm = np.concatenate([f(inp["c_prompt"][c])[None], f(inp["c_sample"][sl])], 0).T
        m["cT"] = np.ascontiguousarray(cTm)
        m["shT"] = np.ascontiguousarray(f(inp["state_shift"][sl]).T)
        m["scT"] = np.ascontiguousarray(f(inp["state_conv"][sl]).transpose(2, 1, 0).reshape(D, 30 * NS))
        m["s_wkv"] = f(inp["state_wkv"][sl])
        m["sc_raw"] = f(inp["state_conv"][sl])
        maps.append(m)
    return maps


def kernel(**inputs):
    maps = _prep_inputs(inputs)
    nc = build()
    names = set(nc_input_names(nc))
    maps = [{k: v for k, v in m.items() if k in names} for m in maps]
    res = run_bass_kernel_spmd(nc, maps, core_ids=list(range(NCORES)))
    r = res.results
    y_p = np.stack([r[c]["y_p"] for c in range(NCORES)], 0)
    y_s = np.concatenate([r[c]["y_s"] for c in range(NCORES)], 0)
    wkv_p = np.stack([r[c]["o_wkv_p"] for c in range(NCORES)], 0)
    sh_p = np.concatenate([r[c]["o_sh_p"] for c in range(NCORES)], 0)
    cv_p = np.stack([r[c]["o_cv_p"] for c in range(NCORES)], 0)
    wkv_s = np.concatenate([r[c]["o_wkv_s"] for c in range(NCORES)], 0)
    sh_s = np.concatenate([r[c]["o_sh_s"] for c in range(NCORES)], 0)
    cv_s = np.concatenate([r[c]["o_cv_s"] for c in range(NCORES)], 0)
    return (y_p, y_s, wkv_p, sh_p, cv_p, wkv_s, sh_s, cv_s)


def nc_input_names(nc):
    return _IN_NAMES


_IN_NAMES = ["sc_raw", "sel", "rbias_row", "x_p", "x_s", "cT", "shT", "scT", "s_wkv", "prm", "cst", "w_ada", "w_in", "w_decay2", "w_aaa2",
             "w_gate2", "w_o_rwkv", "w_conv_out", "w_out", "w_router", "w_eg", "w_eu", "w_ed"]
```

```python
import numpy as np
from contextlib import ExitStack
import concourse.bass as bass
import concourse.mybir as mybir
from concourse.bass_utils import run_bass_kernel_spmd

F32 = mybir.dt.float32
BF16 = mybir.dt.bfloat16
AF = mybir.ActivationFunctionType
ALU = mybir.AluOpType
AX = mybir.AxisListType

NCORES = 8
D = 1024
SEQ = 2048
NS = 16
TS = 4
NTS = NS * TS
TT = SEQ + NTS
NSHIFT = 3360
NIN = 7456
NE = 64
DE = 256
HS = 64
NH = 16
CW = 31
BLOCKS = [(i * 512, 512, False) for i in range(4)] + [(SEQ, NTS, True)]

PCOLS = {}
_off = 0
for _n, _c in [("b_ada", 48), ("mix_pre_g", 8), ("mix_post_g", 8), ("ffn_pre_g", 8), ("ffn_post_g", 8),
               ("mu", 27), ("w0", 8), ("a0", 8), ("k_k", 8), ("k_a", 8), ("r_k", 8), ("gn_g", 8), ("gn_b", 8),
               ("dw_b", 8), ("ln_g", 8), ("ln_b", 8), ("dw_w", 8 * CW), ("rbias", 1)]:
    PCOLS[_n] = (_off, _c)
    _off += _c
NPC = _off

CCOLS = {}
_off = 0
for _n, _c in [("ident", 128), ("blk1", 128), ("m_su", 128), ("m_ui", 128), ("m_sl", 128), ("ones", 128),
               ("bd32", 128), ("of64", 128), ("of128", 128)]:
    CCOLS[_n] = (_off, _c)
    _off += _c
NCC = _off


def _consts():
    c = np.zeros((128, NCC), np.float32)
    p = np.arange(128)[:, None]
    f = np.arange(128)[None, :]
    def put(name, arr):
        o, n = CCOLS[name]
        c[:, o:o + n] = arr
    put("ident", (p == f))
    put("blk1", (p // 64 == f // 64))
    put("m_su", (p < f))
    put("m_ui", (p <= f))
    put("m_sl", (p > f))
    put("ones", np.ones((128, 128)))
    put("bd32", (p // 32 == f // 32))
    put("of64", (p // 64 == f // 64) & (p // 32 != f // 32))
    put("of128", (p // 64 != f // 64))
    return c


def _sel():
    sel = np.zeros((64, 64, 128), np.float32)
    for e in range(64):
        sel[e, e, :] = 1.0
    return sel.reshape(64, -1)


class Tok:
    __slots__ = ("sem", "val", "key")

    def __init__(self, sem, val, key):
        self.sem, self.val, self.key = sem, val, key


class KB:
    def __init__(self, nc, es):
        self.nc = nc
        self.eng = {"pe": nc.tensor, "act": nc.scalar, "dve": nc.vector, "pool": nc.gpsimd, "sp": nc.sync}
        self.csem = {}
        self.ccnt = {}
        for n in ["pe", "act", "dve", "pool"]:
            self.csem[n] = es.enter_context(nc.semaphore("c_" + n))
            self.ccnt[n] = 0
        self.ndsem = 40
        self.dsem = [es.enter_context(nc.semaphore("d%d" % i)) for i in range(self.ndsem)]
        self.dcnt = [0] * self.ndsem
        self.dpool = {"sp": list(range(0, 24)), "pool": list(range(24, 36)), "act": list(range(36, 40))}
        self.dnext = {"sp": 0, "pool": 0, "act": 0}
        self.waited = {n: {} for n in self.eng}
        self.last_w = {}
        self.readers = {}
        self.nins = 0

    def _wait(self, en, tok):
        w = self.waited[en]
        if w.get(tok.key, 0) >= tok.val:
            return
        self.eng[en].wait_ge(tok.sem, tok.val)
        w[tok.key] = tok.val

    def _deps(self, en, reads, writes, skip_pe_self=False):
        toks = []
        for b in reads:
            t = self.last_w.get(b)
            if t is not None:
                toks.append(t)
        for b in writes:
            t = self.last_w.get(b)
            if t is not None:
                toks.append(t)
            toks.extend(self.readers.get(b, ()))
        for t in toks:
            if skip_pe_self and t.key == "c_pe":
                continue
            self._wait(en, t)

    def _record(self, tok, reads, writes):
        for b in writes:
            self.last_w[b] = tok
            self.readers[b] = []
        for b in reads:
            self.readers.setdefault(b, []).append(tok)

    def op(self, en, fn, reads=(), writes=()):
        self._deps(en, reads, writes, skip_pe_self=(en == "pe"))
        ins = fn(self.eng[en])
        self.ccnt[en] += 1
        ins.then_inc(self.csem[en], 1)
        tok = Tok(self.csem[en], self.ccnt[en], "c_" + en)
        self._record(tok, reads, writes)
        self.nins += 1
        return tok

    def mm_group(self, fns, reads=(), writes=()):
        self._deps("pe", reads, writes, skip_pe_self=True)
        ins = None
        for fn in fns:
            ins = fn(self.eng["pe"])
            self.nins += 1
        self.ccnt["pe"] += 1
        ins.then_inc(self.csem["pe"], 1)
        tok = Tok(self.csem["pe"], self.ccnt["pe"], "c_pe")
        self._record(tok, reads, writes)
        return tok

    def dma(self, en, out, in_, reads=(), writes=(), slow=False):
        self._deps(en, reads, writes)
        pl = self.dpool[en]
        i = pl[self.dnext[en] % len(pl)]
        self.dnext[en] += 1
        if self.dcnt[i] > 0:
            self._wait(en, Tok(self.dsem[i], self.dcnt[i], "d%d" % i))
        self.dcnt[i] += 16
        if slow:
            self.eng[en].dma_start(out=out, in_=in_, allow_slow_non_contiguous=True).then_inc(self.dsem[i], 16)
        else:
            self.eng[en].dma_start(out=out, in_=in_).then_inc(self.dsem[i], 16)
        tok = Tok(self.dsem[i], self.dcnt[i], "d%d" % i)
        self._record(tok, reads, writes)
        self.nins += 1
        return tok

    def barrier(self):
        for en in self.eng:
            for n in self.csem:
                if self.ccnt[n] > 0:
                    self._wait(en, Tok(self.csem[n], self.ccnt[n], "c_" + n))
            for i in range(self.ndsem):
                if self.dcnt[i] > 0:
                    self._wait(en, Tok(self.dsem[i], self.dcnt[i], "d%d" % i))

    def finish(self, en="sp"):
        for i in range(self.ndsem):
            if self.dcnt[i] > 0:
                self._wait(en, Tok(self.dsem[i], self.dcnt[i], "d%d" % i))


def pc(P, name, j=None, n=1):
    o, c = PCOLS[name]
    if j is None:
        return P[:, o:o + c]
    return P[:, o + j:o + j + n]


def build(debug=None, stop_after=None):
    nc = bass.Bass("TRN2", target_bir_lowering=False)
    moe_on = stop_after is None
    dt_in = lambda name, shape, dt=F32: nc.dram_tensor(name, list(shape), dt, kind="ExternalInput").ap()
    dt_out = lambda name, shape, dt=F32: nc.dram_tensor(name, list(shape), dt, kind="ExternalOutput").ap()
    dt_scr = lambda name, shape, dt=F32: nc.dram_tensor(name, list(shape), dt).ap()

    x_p = dt_in("x_p", [SEQ, D])
    x_s = dt_in("x_s", [NS, TS, D])
    cT = dt_in("cT", [D, 17])
    shT = dt_in("shT", [NSHIFT, NS])
    scT = dt_in("scT", [D, 30 * NS])
    s_wkv = dt_in("s_wkv", [NS, NH, HS, HS])
    prm = dt_in("prm", [128, NPC])
    cst = dt_in("cst", [128, NCC])
    w_ada = dt_in("w_ada", [D, 6 * D])
    w_in = dt_in("w_in", [D, NIN])
    w_decay2 = dt_in("w_decay2", [64, D])
    w_aaa2 = dt_in("w_aaa2", [64, D])
    w_gate2 = dt_in("w_gate2", [160, D])
    w_o_rwkv = dt_in("w_o_rwkv", [D, D])
    w_conv_out = dt_in("w_conv_out", [D, D])
    w_out = dt_in("w_out", [D, D])
    w_router = dt_in("w_router", [D, NE])
    if moe_on:
        w_eg = dt_in("w_eg", [NE + 1, D, DE])
        w_eu = dt_in("w_eu", [NE + 1, D, DE])
        w_ed = dt_in("w_ed", [NE + 1, DE, D])
    y_p = dt_out("y_p", [SEQ, D])
    y_s = dt_out("y_s", [NS, TS, D])
    o_wkv_p = dt_out("o_wkv_p", [NH, HS, HS])
    o_sh_p = dt_out("o_sh_p", [1, NSHIFT])
    o_cv_p = dt_out("o_cv_p", [30, D])
    o_wkv_s = dt_out("o_wkv_s", [NS, NH, HS, HS])
    o_sh_s = dt_out("o_sh_s", [NS, NSHIFT])
    o_cv_s = dt_out("o_cv_s", [NS, 30, D])
    PW = 1 + SEQ + NS + NTS
    P_d = dt_scr("P_d", [NSHIFT, PW])
    dbg = {}
    if debug:
        for name, shape in debug.items():
            dbg[name] = dt_out("dbg_" + name, shape, BF16 if name in ("YI_d", "YI_s", "HF_d", "GT_d", "uT", "mT", "kT0", "AR0", "KK0", "BB0", "MT0", "Xs0", "Us0", "Vtm0") else F32)

    def pcol(c0, sample):
        return (1 + c0) if not sample else (1 + SEQ + NS + (c0 - SEQ))

    with ExitStack() as es:
        kb = KB(nc, es)
        sb = lambda n, sh, dt=F32: es.enter_context(nc.sbuf_tensor(n, list(sh), dt))
        P = sb("P", [128, NPC])
        C = sb("C", [128, NCC])
        Cb = sb("Cb", [128, NCC], BF16)
        modT = sb("modT", [128, 48, 17])
        Am = sb("Am", [128, 8, 17]); Gm = sb("Gm", [128, 8, 17])
        Af = sb("Af", [128, 8, 17]); Gf = sb("Gf", [128, 8, 17])

        kb.dma("sp", P[:], prm, writes=["P"])
        kb.dma("sp", C[:], cst, writes=["C"])
        kb.dma("pool", Cb[:], cst, writes=["Cb"])

        def cc(name, bf=False):
            o, n = CCOLS[name]
            return (Cb if bf else C)[:, o:o + n]

        with ExitStack() as ea:
            sba = lambda n, sh, dt=F32: ea.enter_context(nc.sbuf_tensor(n, list(sh), dt))
            cTs = sba("cTs", [128, 8, 17])
            cTb = sba("cTb", [128, 8, 17], BF16)
            wa = [sba("wa%d" % i, [128, 8, 512], BF16) for i in range(2)]
            psA = ea.enter_context(nc.psum_tensor("psA", [128, 2, 512], F32))
            kb.dma("sp", cTs[:], cT.rearrange("(k p) m -> p k m", p=128), writes=["cTs"])
            kb.op("act", lambda e: e.activation(out=cTb[:], in_=cTs[:], func=AF.Silu), reads=["cTs"], writes=["cTb"])
            for nb in range(12):
                s = nb % 2
                kb.dma("pool", wa[s][:], w_ada[:, nb * 512:(nb + 1) * 512].rearrange("(k p) n -> p k n", p=128),
                       writes=["wa%d" % s])
                for oo in range(4):
                    o = nb * 4 + oo
                    dst = psA[:, o // 24, (o % 24) * 17:(o % 24) * 17 + 17]
                    kb.mm_group([
                        (lambda e, k=k, oo=oo, dst=dst, s=s: e.matmul(dst, lhsT=wa[s][:, k, oo * 128:(oo + 1) * 128],
                                                                    rhs=cTb[:, k, :], start=(k == 0), stop=(k == 7)))
                        for k in range(8)], reads=["wa%d" % s, "cTb"], writes=["psA"])
            for half in range(2):
                o0 = half * 24
                kb.op("dve", lambda e, half=half, o0=o0: e.tensor_tensor(
                    out=modT[:, o0:o0 + 24, :], in0=psA[:, half, 0:24 * 17].rearrange("p (o m) -> p o m", m=17),
                    in1=pc(P, "b_ada")[:, o0:o0 + 24].unsqueeze(2).to_broadcast([128, 24, 17]), op=ALU.add),
                    reads=["psA", "P"], writes=["modT"])
            def bc(name):
                return pc(P, name).unsqueeze(2).to_broadcast([128, 8, 17])
            kb.op("dve", lambda e: e.scalar_tensor_tensor(out=Am[:], in0=modT[:, 8:16, :], scalar=1.0, in1=bc("mix_pre_g"),
                                                          op0=ALU.add, op1=ALU.mult), reads=["modT", "P"], writes=["Am"])
            kb.op("dve", lambda e: e.tensor_tensor(out=Gm[:], in0=modT[:, 16:24, :], in1=bc("mix_post_g"), op=ALU.mult),
                  reads=["modT", "P"], writes=["Gm"])
            kb.op("dve", lambda e: e.scalar_tensor_tensor(out=Af[:], in0=modT[:, 32:40, :], scalar=1.0, in1=bc("ffn_pre_g"),
                                                          op0=ALU.add, op1=ALU.mult), reads=["modT", "P"], writes=["Af"])
            kb.op("dve", lambda e: e.tensor_tensor(out=Gf[:], in0=modT[:, 40:48, :], in1=bc("ffn_post_g"), op=ALU.mult),
                  reads=["modT", "P"], writes=["Gf"])
            if "modT" in dbg:
                kb.dma("sp", dbg["modT"], modT[:], reads=["modT"])

        if stop_after == "A":
            kb.finish("sp"); return nc
        kb.barrier()
        GW = 30 + SEQ + 30 * NS + NTS
        G_d = dt_scr("G_d", [D, GW])
        SG_d = dt_scr("SG_d", [2 * D, TT], BF16)
        XT_d = dt_scr("XT_d", [D, TT])
        def gcol(c0, sample):
            return (30 + c0) if not sample else (30 + SEQ + 30 * NS + (c0 - SEQ))

        def bcast_mod(M, lo, sample, n):
            if not sample:
                return M[:, lo:lo + 8, 0:1].to_broadcast([128, 8, n])
            return M[:, lo:lo + 8, 1:17].unsqueeze(2).to_broadcast([128, 8, TS, NS])

        def view(ap, sample):
            return ap if not sample else ap.rearrange("p k (t s) -> p k t s", s=NS)

        with ExitStack() as eb:
            sbb = lambda n, sh, dt=F32: eb.enter_context(nc.sbuf_tensor(n, list(sh), dt))
            psb = lambda n, sh, dt=F32: eb.enter_context(nc.psum_tensor(n, list(sh), dt))
            wi = sbb("wi", [128, 8, NIN], BF16)
            zt = sbb("zt", [128, 8 * 30])
            xt = [sbb("xt%d" % i, [128, D]) for i in range(2)]
            xTb = sbb("xTb", [128, 8, 512])
            sq = sbb("sq", [128, 8, 512])
            rstd = sbb("rstd", [128, 512])
            hT = [sbb("hT%d" % i, [128, 8, 512], BF16) for i in range(2)]
            stg = [sbb("stg%d" % i, [128, 512]) for i in range(2)]
            stb = [sbb("stb%d" % i, [128, 512], BF16) for i in range(2)]
            sgt = [sbb("sgt%d" % i, [128, 512]) for i in range(1)]
            shrow = [sbb("shrow%d" % i, [64, 512]) for i in range(2)]
            raw = [sbb("raw%d" % i, [128, 512 + NS]) for i in range(2)]
            bnd = sbb("bnd", [128, 27, NS])
            psT = psb("psT", [128, 8, 128])
            psP = [psb("psP%d" % i, [128, 512]) for i in range(4)]
            psS = psb("psS", [128, 512])
            for q in range(15):
                c_lo = q * 512
                c_hi = min(NIN, c_lo + 512)
                kb.dma("pool", wi[:, :, c_lo:c_hi], w_in[:, c_lo:c_hi].rearrange("(k p) n -> p k n", p=128),
                       writes=[("wi", q)])
            def wi_keys(lo, hi):
                return [("wi", q) for q in range(lo // 512, (hi - 1) // 512 + 1)]
            kb.op("dve", lambda e: e.memset(zt[:], 0.0), writes=["zt"])
            kb.op("dve", lambda e: e.memset(bnd[:], 0.0), writes=["bnd"])
            kb.dma("sp", G_d[:, 0:30].rearrange("(k p) c -> p k c", p=128), zt[:].rearrange("p (k c) -> p k c", c=30),
                   reads=["zt"], writes=["G_hist"])
            kb.dma("sp", G_d[:, 30 + SEQ:30 + SEQ + 30 * NS], scT, writes=["G_hist"])
            xcount = 0
            pcount = 0
            for bi, (c0, n, sample) in enumerate(BLOCKS):
                hs = bi % 2
                m = 64 if sample else 128
                for i in range(n // m):
                    xs_ = xcount % 2
                    xcount += 1
                    if not sample:
                        kb.dma("sp", xt[xs_][:], x_p[c0 + i * 128:c0 + (i + 1) * 128, :], writes=[("xt", xs_)])
                    else:
                        for t in range(TS):
                            kb.dma("sp", xt[xs_][t * NS:(t + 1) * NS, :], x_s[:, t, :], writes=[("xt", xs_)])
                    kb.mm_group([
                        (lambda e, k=k, xs_=xs_: e.transpose(out=psT[:, k, 0:m], in_=xt[xs_][0:m, k * 128:(k + 1) * 128],
                                                            identity=cc("ident")[0:m, 0:m]))
                        for k in range(8)], reads=[("xt", xs_), "C"], writes=["psT"])
                    kb.op("act", lambda e, i=i: e.activation(out=xTb[:, :, i * m:(i + 1) * m], in_=psT[:, :, 0:m], func=AF.Copy),
                          reads=["psT"], writes=["xTb"])
                kb.dma("sp", XT_d[:, c0:c0 + n].rearrange("(k p) c -> p k c", p=128), xTb[:, :, 0:n], reads=["xTb"],
                       writes=[("XT", bi)])
                kb.op("act", lambda e: e.activation(out=sq[:, :, 0:n], in_=xTb[:, :, 0:n], func=AF.Square),
                      reads=["xTb"], writes=["sq"])
                kb.op("dve", lambda e: e.tensor_reduce(out=rstd[:, 0:n], in_=sq[:, :, 0:n].rearrange("p k n -> p n k"), axis=AX.X, op=ALU.add),
                      reads=["sq"], writes=["rstd"])
                kb.mm_group([lambda e: e.matmul(psS[:, 0:n], lhsT=cc("ones"), rhs=rstd[:, 0:n], start=True, stop=True)],
                            reads=["rstd", "C"], writes=["psS"])
                kb.op("act", lambda e: e.activation(out=rstd[:, 0:n], in_=psS[:, 0:n], func=AF.Ln, scale=1.0 / D, bias=1e-6),
                      reads=["psS"], writes=["rstd"])
                kb.op("act", lambda e: e.activation(out=rstd[:, 0:n], in_=rstd[:, 0:n], func=AF.Exp, scale=-0.5),
                      reads=["rstd"], writes=["rstd"])
                kb.op("dve", lambda e: e.tensor_tensor(out=sq[:, :, 0:n], in0=xTb[:, :, 0:n],
                                                      in1=rstd[:, 0:n].unsqueeze(1).to_broadcast([128, 8, n]), op=ALU.mult),
                      reads=["xTb", "rstd", "sq"], writes=["sq"])
                kb.op("dve", lambda e: e.tensor_tensor(out=view(sq[:, :, 0:n], sample), in0=view(sq[:, :, 0:n], sample),
                                                      in1=bcast_mod(Am, 0, sample, n), op=ALU.mult),
                      reads=["sq", "Am"], writes=["sq"])
                kb.op("dve", lambda e: e.tensor_tensor(out=view(hT[hs][:, :, 0:n], sample), in0=view(sq[:, :, 0:n], sample),
                                                      in1=bcast_mod(modT, 0, sample, n), op=ALU.add),
                      reads=["sq", "modT"], writes=[("hT", hs)])

                def proj(col_lo, mrows, hs=hs, n=n):
                    nonlocal pcount
                    ps = pcount % 4
                    pcount += 1
                    kb.mm_group([
                        (lambda e, k=k, ps=ps: e.matmul(psP[ps][0:mrows, 0:n], lhsT=wi[:, k, col_lo:col_lo + mrows],
                                                        rhs=hT[hs][:, k, 0:n], start=(k == 0), stop=(k == 7)))
                        for k in range(8)], reads=[("hT", hs)] + wi_keys(col_lo, col_lo + mrows), writes=[("psP", ps)])
                    return ps
                wd_ = NS if sample else 1
                if sample:
                    kb.dma("sp", bnd[:, 0:26, :], shT[0:3328, :].rearrange("(k p) c -> p k c", p=128), writes=["bnd"])
                    kb.dma("sp", bnd[0:32, 26, :], shT[3328:3360, :], writes=["bnd"])
                for ot in range(27):
                    mrows = 128 if ot < 26 else 32
                    ps = proj(ot * 128, mrows)
                    sg = ot % 2
                    rw_ = raw[ot % 2]
                    rk_ = ("raw", ot % 2)
                    kb.op("act", lambda e, ot=ot, rw_=rw_: e.activation(out=rw_[0:mrows, 0:wd_], in_=bnd[0:mrows, ot, 0:wd_], func=AF.Copy),
                          reads=["bnd"], writes=[rk_])
                    kb.op("act", lambda e, ps=ps, rw_=rw_: e.activation(out=rw_[0:mrows, wd_:wd_ + n], in_=psP[ps][0:mrows, 0:n],
                                                                      func=AF.Copy), reads=[("psP", ps)], writes=[rk_])
                    if not sample and bi < 3:
                        kb.op("act", lambda e, ot=ot, rw_=rw_: e.activation(out=bnd[0:mrows, ot, 0:1], in_=rw_[0:mrows, n:n + 1], func=AF.Copy),
                              reads=[rk_], writes=["bnd"])
                    kb.op("pool", lambda e, rw_=rw_, sg=sg: e.tensor_tensor(out=stg[sg][0:mrows, 0:n], in0=rw_[0:mrows, 0:n],
                                                                           in1=rw_[0:mrows, wd_:wd_ + n], op=ALU.subtract),
                          reads=[rk_], writes=[("stg", sg)])
                    kb.op("dve", lambda e, rw_=rw_, sg=sg, ot=ot: e.scalar_tensor_tensor(
                        out=stg[sg][0:mrows, 0:n], in0=stg[sg][0:mrows, 0:n], scalar=pc(P, "mu", ot)[0:mrows, :],
                        in1=rw_[0:mrows, wd_:wd_ + n], op0=ALU.mult, op1=ALU.add), reads=[rk_, ("stg", sg), "P"], writes=[("stg", sg)])
                    pcl = pcol(c0, sample)
                    kb.dma("sp", P_d[ot * 128:ot * 128 + mrows, pcl:pcl + n], stg[sg][0:mrows, 0:n], reads=[("stg", sg)],
                           writes=[("P", bi)])
                for j in range(8):
                    pv = proj(NSHIFT + j * 128, 128)
                    pg = proj(NSHIFT + D + j * 128, 128)
                    s2 = 0
                    kb.op("act", lambda e, pg=pg, s2=s2: e.activation(out=sgt[s2][:, 0:n], in_=psP[pg][:, 0:n], func=AF.Sigmoid),
                          reads=[("psP", pg)], writes=[("sgt", s2)])
                    sg = j % 2
                    kb.op("dve", lambda e, pv=pv, s2=s2, sg=sg: e.tensor_tensor(out=stg[sg][:, 0:n], in0=psP[pv][:, 0:n],
                                                                             in1=sgt[s2][:, 0:n], op=ALU.mult),
                          reads=[("psP", pv), ("sgt", s2)], writes=[("stg", sg)])
                    gcl = gcol(c0, sample)
                    kb.dma("sp", G_d[j * 128:(j + 1) * 128, gcl:gcl + n], stg[sg][:, 0:n], reads=[("stg", sg)],
                           writes=[("G", bi)])
                for j in range(16):
                    pg = proj(NSHIFT + 2 * D + j * 128, 128)
                    s2 = j % 2
                    kb.op("act", lambda e, pg=pg, s2=s2: e.activation(out=stb[s2][:, 0:n], in_=psP[pg][:, 0:n], func=AF.Sigmoid),
                          reads=[("psP", pg)], writes=[("stb", s2)])
                    kb.dma("sp", SG_d[j * 128:(j + 1) * 128, c0:c0 + n], stb[s2][:, 0:n], reads=[("stb", s2)],
                           writes=[("SG", bi)])
                if bi == 3 or sample:
                    mm = 64 if sample else 1
                    lcol = 0 if sample else 511
                    for cb in range(7):
                        w_ = min(512, NSHIFT - cb * 512)
                        ps = pcount % 4
                        pcount += 1
                        kb.mm_group([
                            (lambda e, k=k, ps=ps: e.matmul(psP[ps][0:mm, 0:w_], lhsT=hT[hs][:, k, lcol:lcol + mm],
                                                            rhs=wi[:, k, cb * 512:cb * 512 + w_], start=(k == 0), stop=(k == 7)))
                            for k in range(8)], reads=[("hT", hs)] + wi_keys(cb * 512, cb * 512 + w_), writes=[("psP", ps)])
                        sr = cb % 2
                        kb.op("act", lambda e, ps=ps, sr=sr: e.activation(out=shrow[sr][0:mm, 0:w_],
                                                                   in_=psP[ps][0:mm, 0:w_], func=AF.Copy),
                              reads=[("psP", ps)], writes=[("shrow", sr)])
                        if sample:
                            kb.dma("sp", o_sh_s[:, cb * 512:cb * 512 + w_], shrow[sr][48:64, 0:w_], reads=[("shrow", sr)],
                                   writes=["o_sh_s"])
                        else:
                            kb.dma("sp", o_sh_p[:, cb * 512:cb * 512 + w_], shrow[sr][0:1, 0:w_], reads=[("shrow", sr)],
                                   writes=["o_sh_p"])
            if "P_d" in dbg:
                kb.dma("sp", dbg["P_d"], P_d, reads=[("P", b) for b in range(5)] + ["P_hist"])
            if "G_d" in dbg:
                kb.dma("sp", dbg["G_d"], G_d, reads=[("G", b) for b in range(5)] + ["G_hist"])
        if stop_after == "B":
            kb.finish("sp"); return nc
        kb.barrier()

        YI_d = dt_scr("YI_d", [D, TT], BF16)
        RW_d = dt_scr("RW_d", [5, 2, NTS, 8, HS])
        GN_EPS = HS * 1e-5
        v_s = sb("v_s", [128, 8, NTS]); bon_s = sb("bon_s", [128, 8, NTS]); g_s = sb("g_s", [128, 8, NTS], BF16)
        def gn_out(ysrc, bsrc, gsrc, n_, col0, rkeys, RS):
            psR, nxt = RS["psR"], RS["nxt"]
            setA = (RS["tp"][1], RS["tp"][2], RS["yib"], "xk", "xv", "yib")
            setB = RS.get("setB")

            def gn_tile(j, S_):
                d_, sq_, yb_, dk, qk, yk = S_
                p1 = nxt()
                yield
                kb.mm_group([lambda e: e.matmul(psR[p1][:, 0:n_], lhsT=cc("blk1"), rhs=ysrc[:, j, 0:n_],
                                                start=True, stop=True)], reads=["C"] + rkeys, writes=[("psR", p1)])
                yield
                kb.op("dve", lambda e: e.scalar_tensor_tensor(out=d_[:, 0:n_], in0=psR[p1][:, 0:n_], scalar=-1.0 / HS,
                                                               in1=ysrc[:, j, 0:n_], op0=ALU.mult, op1=ALU.add),
                      reads=[("psR", p1), "C"] + rkeys, writes=[dk])
                yield
                kb.op("act", lambda e: e.activation(out=sq_[:, 0:n_], in_=d_[:, 0:n_], func=AF.Square),
                      reads=[dk], writes=[qk])
                p2 = nxt()
                yield
                kb.mm_group([lambda e: e.matmul(psR[p2][:, 0:n_], lhsT=cc("blk1"), rhs=sq_[:, 0:n_],
                                                start=True, stop=True)], reads=["C", qk], writes=[("psR", p2)])
                yield
                kb.op("act", lambda e: e.activation(out=sq_[:, 0:n_], in_=psR[p2][:, 0:n_], func=AF.Ln, scale=1.0 / HS, bias=GN_EPS),
                      reads=[("psR", p2), qk], writes=[qk])
                yield
                kb.op("act", lambda e: e.activation(out=sq_[:, 0:n_], in_=sq_[:, 0:n_], func=AF.Exp, scale=-0.5), reads=[qk], writes=[qk])
                yield
                kb.op("dve", lambda e: e.tensor_tensor(out=d_[:, 0:n_], in0=d_[:, 0:n_], in1=sq_[:, 0:n_], op=ALU.mult),
                      reads=[dk, qk], writes=[dk])
                yield
                kb.op("dve", lambda e: e.tensor_scalar(out=d_[:, 0:n_], in0=d_[:, 0:n_], scalar1=pc(P, "gn_g", j),
                                                      scalar2=pc(P, "gn_b", j), op0=ALU.mult, op1=ALU.add),
                      reads=[dk, "P"], writes=[dk])
                yield
                kb.op("dve", lambda e: e.tensor_tensor(out=d_[:, 0:n_], in0=d_[:, 0:n_], in1=bsrc[:, j, 0:n_], op=ALU.add),
                      reads=[dk] + rkeys, writes=[dk])
                yield
                kb.op("dve", lambda e: e.tensor_tensor(out=yb_[:, 0:n_], in0=d_[:, 0:n_], in1=gsrc[:, j, 0:n_], op=ALU.mult),
                      reads=[dk] + rkeys, writes=[yk])
                yield
                kb.dma("sp", YI_d[j * 128:(j + 1) * 128, col0:col0 + n_], yb_[:, 0:n_], reads=[yk], writes=[("YI", col0)])

            if setB is None:
                for j in range(8):
                    for _ in gn_tile(j, setA):
                        pass
            else:
                for jp in range(4):
                    gs_ = [gn_tile(2 * jp, setA), gn_tile(2 * jp + 1, setB)]
                    while gs_:
                        for g_ in list(gs_):
                            try:
                                next(g_)
                            except StopIteration:
                                gs_.remove(g_)

        with ExitStack() as ec:
            sbc = lambda n, sh, dt=F32: ec.enter_context(nc.sbuf_tensor(n, list(sh), dt))
            psc = lambda n, sh, dt=F32: ec.enter_context(nc.psum_tensor(n, list(sh), dt))
            wlo = sbc("wlo", [128, D], BF16)
            wg2 = sbc("wg2", [128, 2, D], BF16)
            Hf = sbc("Hf", [128, 8, 128]); Hb = sbc("Hb", [128, 8, 128], BF16)
            m01 = sbc("m01", [128, 512])
            AR = sbc("AR", [128, 8, 4, 2, 128], BF16)
            kT = sbc("kT", [128, 8, 512], BF16); bT = sbc("bT", [128, 8, 512], BF16)
            v16 = sbc("v16", [128, 8, 512], BF16); g16 = sbc("g16", [128, 8, 512], BF16)
            bon = sbc("bon", [128, 8, 512], BF16); yT = sbc("yT", [128, 8, 512]); Pc = sbc("Pc", [128, 8, 4])
            ld = [sbc("ld%d" % i, [128, 512]) for i in range(3)]
            tp = [sbc("tp%d" % i, [128, 512]) for i in range(9)]
            tpB = [sbc("tpB%d" % i, [128, 512]) for i in range(6)]
            twa = sbc("twa", [128, 512], BF16); sxg = sbc("sxg", [128, 2, 512], BF16)
            Vtm = sbc("Vtm", [128, D], BF16); Ktm = sbc("Ktm", [128, D], BF16); Btm = sbc("Btm", [128, D], BF16)
            KK_ = sbc("KK_", [128, 16, 2, 128], BF16); RB_ = sbc("RB_", [128, 16, 128], BF16)
            L0f = sbc("L0f", [128, 8, 128], BF16); N0f = sbc("N0f", [128, 8, 128], BF16)
            Pw = [sbc("Pw%d" % i, [128, 8, 128], BF16) for i in range(2)]
            Qw = [sbc("Qw%d" % i, [128, 8, 128], BF16) for i in range(2)]
            Ml = [sbc("Ml%d" % i, [128, 8, 128], BF16) for i in range(2)]
            MT = [sbc("MT%d" % i, [128, 8, 128], BF16) for i in range(2)]
            MTb = sbc("MTb", [128, 16, 128], BF16)
            Xs = sbc("Xs", [128, D], BF16); Us = sbc("Us", [128, D], BF16)
            yib = sbc("yib", [128, 512], BF16); yibB = sbc("yibB", [128, 512], BF16)
            rows_s2 = [sbc("rows_s%d" % i, [64, 128]) for i in range(2)]
            psR = [psc("psR%d" % i, [128, 512]) for i in range(6)]
            psTr = psc("psTr", [128, D], BF16)
            rcount = 0
            def nxt():
                nonlocal rcount
                r_ = rcount % 6
                rcount += 1
                return r_
            kb.dma("pool", wlo[0:64, :], w_decay2, writes=["wlo"])
            kb.dma("pool", wlo[64:128, :], w_aaa2, writes=["wlo"])
            kb.dma("pool", wg2[:, 0, :], w_gate2[0:128, :], writes=["wg2"])
            kb.dma("pool", wg2[0:32, 1, :], w_gate2[128:160, :], writes=["wg2"])
            kb.op("dve", lambda e: e.memset(Hf[:], 0.0), writes=["Hf"])
            kb.op("dve", lambda e: e.memset(Hb[:], 0.0), writes=["Hb"])
            kb.op("dve", lambda e: e.memset(m01[:], 1.0), writes=["m01"])
            kb.op("dve", lambda e: e.memset(m01[:].rearrange("p (c t) -> p c t", t=128)[:, :, 0:1], 0.0), writes=["m01"])
            E05 = float(np.exp(-0.5))

            def load_xs(ti, rows, bi, c0, n, sample, dst, slot, dkey):
                pcl = pcol(c0, sample)
                kb.dma("sp", dst, P_d[ti * 128:ti * 128 + rows, pcl:pcl + n], reads=[("P", bi)], writes=[dkey])

            xr, xk, xv, lw, av, kk, ksq, tk, cs = tp
            for bi, (c0, n, sample) in enumerate(BLOCKS):
                nch = 1 if sample else n // 128
                load_xs(24, 128, bi, c0, n, sample, ksq[:, 0:n], 0, "ksq")
                kb.op("act", lambda e: e.activation(out=twa[0:64, 0:n], in_=ksq[0:64, 0:n], func=AF.Tanh),
                      reads=["ksq"], writes=["twa"])
                kb.op("act", lambda e: e.activation(out=twa[64:128, 0:n], in_=ksq[64:128, 0:n], func=AF.Copy),
                      reads=["ksq"], writes=["twa"])
                load_xs(25, 128, bi, c0, n, sample, tk[:, 0:n], 1, "tk")
                kb.op("act", lambda e: e.activation(out=sxg[:, 0, 0:n], in_=tk[:, 0:n], func=AF.Sigmoid),
                      reads=["tk"], writes=["sxg"])
                load_xs(26, 32, bi, c0, n, sample, cs[0:32, 0:n], 2, "cs")
                kb.op("act", lambda e: e.activation(out=sxg[0:32, 1, 0:n], in_=cs[0:32, 0:n], func=AF.Sigmoid),
                      reads=["cs"], writes=["sxg"])
                def tile_gen(j):
                    jc = slice(j * 128, (j + 1) * 128)
                    if j % 2 == 0:
                        xr, xk, xv = tp[0], tp[1], tp[2]
                        xrk, xkk, xvk = "xr", "xk", "xv"
                        lw, av, kk, ksq, tk, cs = tp[3:9]
                        klw, kav, kkk, kksq, ktk, kcs = "lw", "av", "kk", "ksq", "tk", "cs"
                    else:
                        xr, xk, xv = ld[0], ld[1], ld[2]
                        xrk, xkk, xvk = "xr2", "xk2", "xv2"
                        lw, av, kk, ksq, tk, cs = tpB
                        klw, kav, kkk, kksq, ktk, kcs = "lw2", "av2", "kk2", "ksq2", "tk2", "cs2"
                    yield
                    load_xs(j, 128, bi, c0, n, sample, xr[:, 0:n], 0, xrk)
                    yield
                    load_xs(8 + j, 128, bi, c0, n, sample, xk[:, 0:n], 1, xkk)
                    yield
                    load_xs(16 + j, 128, bi, c0, n, sample, xv[:, 0:n], 2, xvk)
                    pw = nxt()
                    yield
                    kb.mm_group([lambda e, pw=pw: e.matmul(psR[pw][:, 0:n], lhsT=wlo[0:64, jc], rhs=twa[0:64, 0:n],
                                                          start=True, stop=True)], reads=["wlo", "twa"], writes=[("psR", pw)])
                    yield
                    kb.op("act", lambda e, pw=pw: e.activation(out=lw[:, 0:n], in_=psR[pw][:, 0:n], func=AF.Sigmoid,
                                                               bias=pc(P, "w0", j)), reads=[("psR", pw), "P"], writes=[klw])
                    pa = nxt()
                    yield
                    kb.mm_group([lambda e, pa=pa: e.matmul(psR[pa][:, 0:n], lhsT=wlo[64:128, jc], rhs=twa[64:128, 0:n],
                                                          start=True, stop=True)], reads=["wlo", "twa"], writes=[("psR", pa)])
                    yield
                    kb.op("act", lambda e, pa=pa: e.activation(out=av[:, 0:n], in_=psR[pa][:, 0:n], func=AF.Sigmoid,
                                                               bias=pc(P, "a0", j)), reads=[("psR", pa), "P"], writes=[kav])
                    pg = nxt()
                    yield
                    kb.mm_group([lambda e, pg=pg: e.matmul(psR[pg][:, 0:n], lhsT=wg2[:, 0, jc], rhs=sxg[:, 0, 0:n],
                                                          start=True, stop=False),
                                 lambda e, pg=pg: e.matmul(psR[pg][:, 0:n], lhsT=wg2[0:32, 1, jc], rhs=sxg[0:32, 1, 0:n],
                                                          start=False, stop=True)], reads=["wg2", "sxg"], writes=[("psR", pg)])
                    gdst = g_s[:, j, :] if sample else g16[:, j, 0:n]
                    yield
                    kb.op("act", lambda e, pg=pg, gdst=gdst: e.activation(out=gdst, in_=psR[pg][:, 0:n], func=AF.Copy),
                          reads=[("psR", pg)], writes=["g16"])
                    yield
                    kb.op("dve", lambda e: e.tensor_scalar_mul(out=kk[:, 0:n], in0=xk[:, 0:n], scalar1=pc(P, "k_k", j)),
                          reads=["P", xkk], writes=[kkk])
                    yield
                    kb.op("act", lambda e: e.activation(out=ksq[:, 0:n], in_=kk[:, 0:n], func=AF.Square),
                          reads=[kkk], writes=[kksq])
                    pn = nxt()
                    yield
                    kb.mm_group([lambda e, pn=pn: e.matmul(psR[pn][:, 0:n], lhsT=cc("blk1"), rhs=ksq[:, 0:n],
                                                          start=True, stop=True)], reads=["C", kksq], writes=[("psR", pn)])
                    yield
                    kb.op("act", lambda e, pn=pn: e.activation(out=ksq[:, 0:n], in_=psR[pn][:, 0:n], func=AF.Ln, bias=1e-24),
                          reads=[("psR", pn)], writes=[kksq])
                    yield
                    kb.op("act", lambda e: e.activation(out=ksq[:, 0:n], in_=ksq[:, 0:n], func=AF.Exp, scale=-0.5),
                          reads=[kksq], writes=[kksq])
                    yield
                    kb.op("dve", lambda e: e.tensor_tensor(out=kk[:, 0:n], in0=kk[:, 0:n], in1=ksq[:, 0:n], op=ALU.mult),
                          reads=[kkk, kksq], writes=[kkk])
                    yield
                    kb.op("dve", lambda e: e.tensor_scalar(out=tk[:, 0:n], in0=av[:, 0:n], scalar1=pc(P, "k_a", j),
                                                          scalar2=pc(P, "k_a", j), op0=ALU.mult, op1=ALU.subtract),
                          reads=[kav, "P"], writes=[ktk])
                    yield
                    kb.op("dve", lambda e: e.scalar_tensor_tensor(out=xk[:, 0:n], in0=tk[:, 0:n], scalar=1.0, in1=xk[:, 0:n],
                                                                   op0=ALU.add, op1=ALU.mult), reads=[ktk, xkk], writes=[xkk])
                    yield
                    kb.op("dve", lambda e: e.scalar_tensor_tensor(out=tk[:, 0:n], in0=xr[:, 0:n], scalar=pc(P, "r_k", j),
                                                                   in1=xk[:, 0:n], op0=ALU.mult, op1=ALU.mult),
                          reads=[xkk, xrk, "P", ktk], writes=[ktk])
                    pb = nxt()
                    yield
                    kb.mm_group([lambda e, pb=pb: e.matmul(psR[pb][:, 0:n], lhsT=cc("blk1"), rhs=tk[:, 0:n],
                                                          start=True, stop=True)], reads=["C", ktk], writes=[("psR", pb)])
                    bdst = bon_s[:, j, :] if sample else bon[:, j, 0:n]
                    yield
                    kb.op("dve", lambda e, pb=pb, bdst=bdst: e.tensor_tensor(out=bdst, in0=psR[pb][:, 0:n], in1=xv[:, 0:n],
                                                                             op=ALU.mult), reads=[("psR", pb), xvk], writes=["bon"])
                    if not sample:
                        yield
                        kb.op("act", lambda e: e.activation(out=v16[:, j, 0:n], in_=xv[:, 0:n], func=AF.Copy),
                              reads=[xvk], writes=["v16"])
                        yield
                        kb.op("dve", lambda e: e.tensor_tensor_scan(out=cs[:, 0:n], data0=m01[:, 0:n], data1=lw[:, 0:n],
                                                                   initial=0.0, op0=ALU.mult, op1=ALU.add),
                              reads=["m01", klw], writes=[kcs])
                        eP, eN, ePm = ksq, tk, lw
                        yield
                        kb.op("dve", lambda e: e.tensor_tensor(out=ePm[:, 0:n], in0=cs[:, 0:n], in1=lw[:, 0:n], op=ALU.subtract),
                              reads=[kcs, klw], writes=[klw])
                        yield
                        kb.op("act", lambda e: e.activation(out=ePm[:, 0:n], in_=ePm[:, 0:n], func=AF.Exp, scale=-E05), reads=[klw], writes=[klw])
                        yield
                        kb.op("act", lambda e: e.activation(out=eP[:, 0:n], in_=cs[:, 0:n], func=AF.Exp, scale=-E05),
                              reads=[kcs], writes=[kksq])
                        yield
                        kb.op("act", lambda e: e.activation(out=eN[:, 0:n], in_=cs[:, 0:n], func=AF.Exp, scale=E05),
                              reads=[kcs], writes=[ktk])
                        yield
                        kb.op("dve", lambda e: e.tensor_copy(out=Pc[:, j, :], in_=eP[:, 0:n].rearrange("p (c t) -> p c t", t=128)[:, :, 127]),
                              reads=[kksq], writes=["Pc"])
                        arv = AR[:, j, :, :, :]
                        yield
                        kb.op("dve", lambda e: e.tensor_tensor(out=arv[:, :, 1, :], in0=xr[:, 0:n].rearrange("p (c t) -> p c t", t=128),
                                                              in1=eP[:, 0:n].rearrange("p (c t) -> p c t", t=128), op=ALU.mult),
                              reads=[kksq, xrk], writes=["AR"])
                        yield
                        kb.op("dve", lambda e: e.scalar_tensor_tensor(out=arv[:, :, 0, :], in0=kk[:, 0:n].rearrange("p (c t) -> p c t", t=128),
                                                                       scalar=-1.0, in1=ePm[:, 0:n].rearrange("p (c t) -> p c t", t=128),
                                                                       op0=ALU.mult, op1=ALU.mult), reads=[kkk, klw], writes=["AR"])
                        yield
                        kb.op("pool", lambda e: e.tensor_tensor(out=kT[:, j, 0:n], in0=xk[:, 0:n], in1=eN[:, 0:n], op=ALU.mult),
                              reads=[xkk, ktk], writes=["kT"])
                        yield
                        kb.op("dve", lambda e: e.tensor_tensor(out=kk[:, 0:n], in0=kk[:, 0:n], in1=av[:, 0:n], op=ALU.mult),
                              reads=[kkk, kav], writes=[kkk])
                        yield
                        kb.op("pool", lambda e: e.tensor_tensor(out=bT[:, j, 0:n], in0=kk[:, 0:n], in1=eN[:, 0:n], op=ALU.mult),
                              reads=[kkk, ktk], writes=["bT"])
                    else:
                        yield
                        kb.op("act", lambda e: e.activation(out=v_s[:, j, :], in_=xv[:, 0:n], func=AF.Copy), reads=[xvk], writes=["v_s"])
                        yield
                        kb.op("act", lambda e: e.activation(out=cs[:, 0:n], in_=lw[:, 0:n], func=AF.Exp, scale=-E05), reads=[klw], writes=[kcs])
                        yield
                        kb.op("dve", lambda e: e.tensor_tensor(out=av[:, 0:n], in0=kk[:, 0:n], in1=av[:, 0:n], op=ALU.mult),
                              reads=[kkk, kav], writes=[kav])
                        yield
                        kb.op("dve", lambda e: e.tensor_scalar_mul(out=kk[:, 0:n], in0=kk[:, 0:n], scalar1=-1.0),
                              reads=[kkk, kav], writes=[kkk])
                        for qi, (src, skey) in enumerate([(cs, kcs), (kk, kkk), (av, kav), (xk, xkk), (xr, xrk)]):
                            pt = nxt()
                            yield
                            kb.mm_group([lambda e, pt=pt, src=src: e.transpose(out=psR[pt][0:n, 0:128], in_=src[:, 0:n],
                                                                               identity=cc("ident"))],
                                        reads=["C", skey], writes=[("psR", pt)])
                            yield
                            kb.op("act", lambda e, pt=pt: e.activation(out=rows_s2[j % 2][:, 0:128], in_=psR[pt][0:n, 0:128], func=AF.Copy),
                                  reads=[("psR", pt)], writes=[("rows_s", j % 2)])
                            yield
                            for hp_ in range(2):
                                kb.dma("sp", RW_d[qi, hp_, :, j, :], rows_s2[j % 2][:, hp_ * 64:(hp_ + 1) * 64],
                                       reads=[("rows_s", j % 2)], writes=["RW"])
                for jp in range(4):
                    gs_ = [tile_gen(2 * jp), tile_gen(2 * jp + 1)]
                    while gs_:
                        for g_ in list(gs_):
                            try:
                                next(g_)
                            except StopIteration:
                                gs_.remove(g_)
                if stop_after == "Cprep":
                    kb.finish("sp"); return nc
                if sample:
                    continue
                for c in range(nch):
                    ccol = slice(c * 128, (c + 1) * 128)
                    for src, dstt, nm in [(v16, Vtm, "Vtm"), (kT, Ktm, "Ktm"), (bT, Btm, "Btm")]:
                        kb.mm_group([(lambda e, j=j, src=src: e.transpose(out=psTr[:, j * 128:(j + 1) * 128], in_=src[:, j, ccol],
                                                                          identity=cc("ident", True))) for j in range(8)],
                                    reads=["Cb", "v16", "kT", "bT"], writes=["psTr"])
                        kb.op("act", lambda e, dstt=dstt: e.activation(out=dstt[:], in_=psTr[:], func=AF.Copy),
                              reads=["psTr"], writes=[nm])
                    if stop_after == "Ctr":
                        kb.finish("sp"); return nc
                    msk2 = C[:, CCOLS["m_su"][0]:CCOLS["m_su"][0] + 256].rearrange("p (a t) -> p a t", a=2)
                    for hq in range(4):
                        pkE, pkO = nxt(), nxt()
                        fns = []
                        for h in range(4 * hq, 4 * hq + 4):
                            bank = pkE if h % 2 == 0 else pkO
                            slot = (h % 4) // 2
                            fns.append(lambda e, h=h, bank=bank, slot=slot: e.matmul(
                                psR[bank][:, slot * 256:slot * 256 + 256],
                                lhsT=kT[(h % 2) * 64:(h % 2) * 64 + 64, h // 2, ccol],
                                rhs=AR[(h % 2) * 64:(h % 2) * 64 + 64, h // 2, c, :, :].rearrange("p a t -> p (a t)"),
                                start=True, stop=True))
                        kb.mm_group(fns, reads=["kT", "AR"], writes=[("psR", pkE), ("psR", pkO)])
                        for par, bank in ((0, pkE), (1, pkO)):
                            kb.op("dve", lambda e, bank=bank, hq=hq, par=par: e.tensor_tensor(
                                out=KK_[:, 4 * hq + par:4 * hq + 4:2, :, :],
                                in0=psR[bank][:].rearrange("p (h a t) -> p h a t", h=2, a=2),
                                in1=msk2.unsqueeze(1).to_broadcast([128, 2, 2, 128]), op=ALU.mult),
                                reads=[("psR", bank), "C"], writes=["KK"])
                    for g8 in range(2):
                        h0 = 8 * g8
                        for (kind, dstA, mname, wkeys, off) in [("rb", RB_, "m_ui", ["RB"], h0),
                                                                ("l0", L0f, "m_sl", [("L0f", 0), ("L0f", 1)], 0),
                                                                ("n0", N0f, "m_su", [("N0f", 0), ("N0f", 1)], 0)]:
                            pkE, pkO = nxt(), nxt()
                            fns = []
                            for h in range(h0, h0 + 8):
                                bank = pkE if h % 2 == 0 else pkO
                                slot = (h - h0) // 2
                                pr = slice((h % 2) * 64, (h % 2) * 64 + 64)
                                a_ap = AR[pr, h // 2, c, 0, :]
                                r_apx = AR[pr, h // 2, c, 1, :]
                                b_ap = bT[pr, h // 2, ccol]
                                l_ap, r_ap = {"rb": (b_ap, r_apx), "l0": (a_ap, b_ap), "n0": (b_ap, a_ap)}[kind]
                                fns.append(lambda e, bank=bank, slot=slot, l_ap=l_ap, r_ap=r_ap: e.matmul(
                                    psR[bank][:, slot * 128:slot * 128 + 128], lhsT=l_ap, rhs=r_ap, start=True, stop=True))
                            kb.mm_group(fns, reads=["bT", "AR"], writes=[("psR", pkE), ("psR", pkO)])
                            for par, bank in ((0, pkE), (1, pkO)):
                                kb.op("dve", lambda e, bank=bank, dstA=dstA, par=par, mname=mname, off=off: e.tensor_tensor(
                                    out=dstA[:, off + par:off + 8:2, :], in0=psR[bank][:].rearrange("p (h t) -> p h t", h=4),
                                    in1=cc(mname).unsqueeze(1).to_broadcast([128, 4, 128]), op=ALU.mult),
                                    reads=[("psR", bank), "C"], writes=wkeys)
                        hsl = lambda hq: slice(4 * hq, 4 * hq + 4)
                        def msk(nm):
                            return cc(nm, True).unsqueeze(1).to_broadcast([128, 4, 128])
                        for hq in range(2):
                            kb.op("pool", lambda e, hq=hq: e.tensor_tensor(out=Pw[0][:, hsl(hq), :], in0=L0f[:, hsl(hq), :], in1=msk("bd32"), op=ALU.mult),
                                  reads=[("L0f", hq), "Cb"], writes=[("Pw", 0, hq)])
                            kb.op("pool", lambda e, hq=hq: e.tensor_tensor(out=Qw[0][:, hsl(hq), :], in0=N0f[:, hsl(hq), :], in1=msk("bd32"), op=ALU.mult),
                                  reads=[("N0f", hq), "Cb"], writes=[("Qw", 0, hq)])
                            kb.op("dve", lambda e, hq=hq: e.tensor_tensor(out=Ml[0][:, hsl(hq), :], in0=Pw[0][:, hsl(hq), :], in1=msk("ident"), op=ALU.add),
                                  reads=[("Pw", 0, hq), "Cb"], writes=[("Ml", 0, hq)])
                            kb.op("dve", lambda e, hq=hq: e.tensor_tensor(out=MT[0][:, hsl(hq), :], in0=Qw[0][:, hsl(hq), :], in1=msk("ident"), op=ALU.add),
                                  reads=[("Qw", 0, hq), "Cb"], writes=[("MT", 0, hq)])
                        def mm4(dst_bank, lhs, rhs, hq, rkeys):
                            kb.mm_group([(lambda e, i=i: e.matmul(psR[dst_bank][:, i * 128:(i + 1) * 128], lhsT=lhs[:, 4 * hq + i, :],
                                                                 rhs=rhs[:, 4 * hq + i, :], start=True, stop=True)) for i in range(4)],
                                        reads=rkeys, writes=[("psR", dst_bank)])
                        def evac_copy(dst, dkey, bank, hq):
                            kb.op("act", lambda e: e.activation(out=dst[:, hsl(hq), :], in_=psR[bank][:].rearrange("p (h t) -> p h t", h=4), func=AF.Copy),
                                  reads=[("psR", bank)], writes=[dkey])
                        def evac_add(dst, dkey, bank, addsrc, akey, hq):
                            kb.op("dve", lambda e: e.tensor_tensor(out=dst[:, hsl(hq), :], in0=psR[bank][:].rearrange("p (h t) -> p h t", h=4),
                                                                  in1=addsrc[:, hsl(hq), :], op=ALU.add),
                                  reads=[("psR", bank), akey], writes=[dkey])
                        cur = 0
                        for it in range(4):
                            nx = 1 - cur
                            for hq in range(2):
                                kP, kQ, kV, kU = ("Pw", cur, hq), ("Qw", cur, hq), ("Ml", cur, hq), ("MT", cur, hq)
                                kP2, kQ2, kV2, kU2 = ("Pw", nx, hq), ("Qw", nx, hq), ("Ml", nx, hq), ("MT", nx, hq)
                                b1 = nxt(); mm4(b1, Qw[cur], Pw[cur], hq, [kQ, kP]); evac_copy(Pw[nx], kP2, b1, hq)
                                b2 = nxt(); mm4(b2, Pw[cur], Qw[cur], hq, [kQ, kP]); evac_copy(Qw[nx], kQ2, b2, hq)
                                b3 = nxt(); mm4(b3, Pw[nx], MT[cur], hq, [kP2, kU]); evac_add(MT[nx], kU2, b3, MT[cur], kU, hq)
                                b4 = nxt(); mm4(b4, Qw[nx], Ml[cur], hq, [kQ2, kV]); evac_add(Ml[nx], kV2, b4, Ml[cur], kV, hq)
                            cur = nx
                        for lvl, mname in ((64, "of64"), (128, "of128")):
                            nx = 1 - cur
                            for hq in range(2):
                                kV, kU = ("Ml", cur, hq), ("MT", cur, hq)
                                kV2, kU2 = ("Ml", nx, hq), ("MT", nx, hq)
                                kb.op("pool", lambda e, hq=hq, mname=mname: e.tensor_tensor(out=Pw[0][:, hsl(hq), :], in0=L0f[:, hsl(hq), :], in1=msk(mname), op=ALU.mult),
                                      reads=[("L0f", hq), "Cb"], writes=[("Pw", 0, hq)])
                                b1 = nxt(); mm4(b1, Pw[0], MT[cur], hq, [("Pw", 0, hq), kU]); evac_copy(Pw[1], ("Pw", 1, hq), b1, hq)
                                b2 = nxt(); mm4(b2, Ml[cur], Pw[1], hq, [kV, ("Pw", 1, hq)])
                                if lvl == 128:
                                    kb.op("dve", lambda e, b2=b2, hq=hq, cur=cur, h0=h0: e.tensor_tensor(
                                        out=MTb[:, h0 + 4 * hq:h0 + 4 * hq + 4, :], in0=psR[b2][:].rearrange("p (h t) -> p h t", h=4),
                                        in1=MT[cur][:, hsl(hq), :], op=ALU.add), reads=[("psR", b2), kU], writes=["MTb"])
                                else:
                                    evac_add(MT[nx], kU2, b2, MT[cur], kU, hq)
                                    kb.op("pool", lambda e, hq=hq, mname=mname: e.tensor_tensor(out=Qw[0][:, hsl(hq), :], in0=N0f[:, hsl(hq), :], in1=msk(mname), op=ALU.mult),
                                          reads=[("N0f", hq), "Cb"], writes=[("Qw", 0, hq)])
                                    b3 = nxt(); mm4(b3, Qw[0], Ml[cur], hq, [("Qw", 0, hq), kV]); evac_copy(Qw[1], ("Qw", 1, hq), b3, hq)
                                    b4 = nxt(); mm4(b4, MT[cur], Qw[1], hq, [kU, ("Qw", 1, hq)]); evac_add(Ml[nx], kV2, b4, Ml[cur], kV, hq)
                            cur = nx
                    MTf = MTb
                    mtk = "MTb"
                    if stop_after == "Cinv":
                        kb.finish("sp"); return nc
                    for hh in range(2):
                        px = nxt()
                        fns = []
                        for h in range(8 * hh, 8 * hh + 8):
                            j_, hp_ = h // 2, h % 2
                            o = psR[px][:, (h % 8) * 64:(h % 8) * 64 + 64]
                            fns.append(lambda e, h=h, o=o: e.matmul(o, lhsT=KK_[:, h, 0, :], rhs=Vtm[:, h * 64:(h + 1) * 64],
                                                                    start=True, stop=False))
                            fns.append(lambda e, j_=j_, hp_=hp_, o=o: e.matmul(o, lhsT=AR[:, j_, c, 0, :],
                                                                               rhs=Hb[:, j_, hp_ * 64:(hp_ + 1) * 64],
                                                                               start=False, stop=True))
                        kb.mm_group(fns, reads=["KK", "Vtm", "AR", "Hb"], writes=[("psR", px)])
                        kb.op("act", lambda e, px=px, hh=hh: e.activation(out=Xs[:, hh * 512:(hh + 1) * 512], in_=psR[px][:], func=AF.Copy),
                              reads=[("psR", px)], writes=["Xs"])
                    for hh in range(2):
                        pu = nxt()
                        kb.mm_group([(lambda e, h=h, pu=pu: e.matmul(psR[pu][:, (h % 8) * 64:(h % 8) * 64 + 64], lhsT=MTf[:, h, :],
                                                                    rhs=Xs[:, h * 64:(h + 1) * 64], start=True, stop=True))
                                     for h in range(8 * hh, 8 * hh + 8)], reads=[mtk, "Xs"], writes=[("psR", pu)])
                        kb.op("act", lambda e, pu=pu, hh=hh: e.activation(out=Us[:, hh * 512:(hh + 1) * 512], in_=psR[pu][:], func=AF.Copy),
                              reads=[("psR", pu)], writes=["Us"])
                    for jq in range(2):
                        py = nxt()
                        fns = []
                        for j_ in range(4 * jq, 4 * jq + 4):
                            for hp_ in range(2):
                                h = 2 * j_ + hp_
                                o = psR[py][hp_ * 64:(hp_ + 1) * 64, (j_ % 4) * 128:(j_ % 4) * 128 + 128]
                                tpos = (0, hp_ * 64)
                                fns.append(lambda e, o=o, j_=j_, hp_=hp_, tpos=tpos: e.matmul(
                                    o, lhsT=Hb[:, j_, hp_ * 64:(hp_ + 1) * 64], rhs=AR[:, j_, c, 1, :], start=True, stop=False,
                                    tile_position=tpos))
                                fns.append(lambda e, o=o, h=h, tpos=tpos: e.matmul(
                                    o, lhsT=Us[:, h * 64:(h + 1) * 64], rhs=RB_[:, h, :], start=False, stop=False, tile_position=tpos))
                                fns.append(lambda e, o=o, h=h, tpos=tpos: e.matmul(
                                    o, lhsT=Vtm[:, h * 64:(h + 1) * 64], rhs=KK_[:, h, 1, :], start=False, stop=True, tile_position=tpos))
                        kb.mm_group(fns, reads=["Hb", "AR", "Us", "RB", "Vtm", "KK"], writes=[("psR", py)])
                        kb.op("act", lambda e, py=py, jq=jq: e.activation(out=yT[:, 4 * jq:4 * jq + 4, ccol],
                                                                          in_=psR[py][:].rearrange("p (j t) -> p j t", j=4), func=AF.Copy),
                              reads=[("psR", py)], writes=["yT"])
                    if stop_after == "Cy":
                        kb.finish("sp"); return nc
                    for jq in range(2):
                        ph = nxt()
                        fns = []
                        for j_ in range(4 * jq, 4 * jq + 4):
                            o = psR[ph][:, (j_ % 4) * 128:(j_ % 4) * 128 + 128]
                            fns.append(lambda e, o=o, j_=j_: e.matmul(o, lhsT=Btm[:, j_ * 128:(j_ + 1) * 128], rhs=Us[:, j_ * 128:(j_ + 1) * 128],
                                                                      start=True, stop=False))
                            fns.append(lambda e, o=o, j_=j_: e.matmul(o, lhsT=Ktm[:, j_ * 128:(j_ + 1) * 128], rhs=Vtm[:, j_ * 128:(j_ + 1) * 128],
                                                                      start=False, stop=True))
                        kb.mm_group(fns, reads=["Btm", "Us", "Ktm", "Vtm"], writes=[("psR", ph)])
                        hv = Hf[:, 4 * jq:4 * jq + 4, :]
                        kb.op("dve", lambda e, ph=ph, hv=hv: e.tensor_tensor(
                            out=tp[0][:].rearrange("p (j t) -> p j t", j=4), in0=psR[ph][:].rearrange("p (j t) -> p j t", j=4),
                            in1=cc("blk1").unsqueeze(1).to_broadcast([128, 4, 128]), op=ALU.mult),
                            reads=[("psR", ph), "C"], writes=["xr"])
                        kb.op("dve", lambda e, hv=hv: e.tensor_tensor(out=tp[0][:].rearrange("p (j t) -> p j t", j=4),
                                                                     in0=tp[0][:].rearrange("p (j t) -> p j t", j=4), in1=hv, op=ALU.add),
                              reads=["xr", "Hf"], writes=["xr"])
                        kb.op("dve", lambda e, hv=hv, jq=jq: e.tensor_tensor(
                            out=hv, in0=tp[0][:].rearrange("p (j t) -> p j t", j=4),
                            in1=Pc[:, 4 * jq:4 * jq + 4, c:c + 1].to_broadcast([128, 4, 128]), op=ALU.mult),
                            reads=["xr", "Pc"], writes=["Hf"])
                        kb.op("act", lambda e, hv=hv, jq=jq: e.activation(out=Hb[:, 4 * jq:4 * jq + 4, :], in_=hv, func=AF.Copy),
                              reads=["Hf"], writes=["Hb"])
                if bi == 0 and "yT0" in dbg:
                    kb.dma("sp", dbg["yT0"], yT[:], reads=["yT"])
                    kb.dma("sp", dbg["bon0"], bon[:], reads=["bon"])
                    kb.dma("sp", dbg["kT0"], kT[:], reads=["kT"])
                    kb.dma("sp", dbg["AR0"], AR[:].rearrange("p j c a t -> p (j c a t)"), reads=["AR"])
                gn_out(yT, bon, g16, n, c0, ["yT", "bon", "g16"], dict(psR=psR, nxt=nxt, tp=tp, yib=yib,
                                                                     setB=(ld[1], ld[2], yibB, "xk2", "xv2", "yibB")))
            for jq in range(2):
                pw_ = nxt()
                kb.mm_group([(lambda e, j=j, pw_=pw_: e.transpose(out=psR[pw_][:, (j % 4) * 128:(j % 4) * 128 + 128], in_=Hf[:, j, :],
                                                                  identity=cc("ident"))) for j in range(4 * jq, 4 * jq + 4)],
                            reads=["Hf", "C"], writes=[("psR", pw_)])
                kb.op("act", lambda e, pw_=pw_, jq=jq: e.activation(out=yT[:, 4 * jq:4 * jq + 4, 0:128],
                                                                    in_=psR[pw_][:].rearrange("p (j t) -> p j t", j=4), func=AF.Copy),
                      reads=[("psR", pw_)], writes=["yT"])
            for hp_ in range(2):
                kb.dma("sp", o_wkv_p.rearrange("(j a) v k -> a v j k", a=2)[hp_],
                       yT[hp_ * 64:(hp_ + 1) * 64, :, hp_ * 64:(hp_ + 1) * 64], reads=["yT"], writes=["o_wkv_p"])
            if "YI_d" in dbg:
                kb.dma("sp", dbg["YI_d"], YI_d[:, 0:SEQ], reads=[("YI", b[0]) for b in BLOCKS[:4]])
        if stop_after == "C":
            kb.finish("sp"); return nc
        kb.barrier()

        with ExitStack() as e2:
            sb2 = lambda n, sh, dt=F32: e2.enter_context(nc.sbuf_tensor(n, list(sh), dt))
            ps2_ = lambda n, sh, dt=F32: e2.enter_context(nc.psum_tensor(n, list(sh), dt))
            HALF = NS // 4
            St = [sb2("St%d" % i, [128, HALF, 8, HS]) for i in range(2)]
            opnd = [[sb2("op%d_%d" % (i, q), [128, HALF, 8, HS], F32 if q == 0 else BF16) for q in range(5)] for i in range(2)]
            tmpS = [sb2("tmpS%d" % i, [128, HALF, 8, HS]) for i in range(2)]
            sa = [sb2("sa%d" % i, [128, HALF, 8]) for i in range(2)]
            y_sT = sb2("y_sT", [128, 8, NTS])
            tp2 = [sb2("tq%d" % i, [128, NTS]) for i in range(3)]
            yib2 = sb2("yib2", [128, NTS], BF16)
            psR2 = [ps2_("psQ%d" % i, [128, 512]) for i in range(2)]
            r2 = [0]
            def nxt2():
                r2[0] += 1
                return r2[0] % 2
            for qd in range(4):
                hf_ = qd % 2
                en = "dve"
                s0 = qd * HALF
                S_ = St[hf_]
                sk = ("St", hf_)
                for hp_ in range(2):
                    kb.dma("sp", S_[hp_ * 64:(hp_ + 1) * 64, :, :, :],
                           s_wkv[s0:s0 + HALF].rearrange("s (g a) v k -> a v s g k", a=2)[hp_], writes=[sk])
                for t in range(TS):
                    r0 = t * NS + s0
                    for q in range(5):
                        for hp_ in range(2):
                            src = RW_d[q, hp_, r0:r0 + HALF, :, :]
                            kb.dma("sp" if q == 0 else "pool", opnd[hf_][q][hp_ * 64:(hp_ + 1) * 64, :, :, :],
                                   src.unsqueeze(0).to_broadcast([64, HALF, 8, HS]), reads=["RW"], writes=[("op", hf_, q)])
                    W_, K_, B_, Kk_, R_ = opnd[hf_]
                    T_ = tmpS[hf_]
                    tk_ = ("tmpS", hf_)
                    sak = ("sa", hf_)
                    vb = v_s[:, :, r0:r0 + HALF].rearrange("p g s -> p s g").unsqueeze(3).to_broadcast([128, HALF, 8, HS])
                    kb.op(en, lambda e: e.tensor_tensor(out=T_[:], in0=S_[:], in1=K_[:], op=ALU.mult),
                          reads=[sk, ("op", hf_, 1)], writes=[tk_])
                    kb.op("dve", lambda e: e.tensor_reduce(out=sa[hf_][:], in_=T_[:], axis=AX.X, op=ALU.add), reads=[tk_], writes=[sak])
                    kb.op(en, lambda e: e.tensor_tensor(out=S_[:], in0=S_[:], in1=W_[:], op=ALU.mult),
                          reads=[sk, ("op", hf_, 0)], writes=[sk])
                    kb.op(en, lambda e: e.tensor_tensor(out=T_[:], in0=B_[:], in1=sa[hf_][:].unsqueeze(3).to_broadcast([128, HALF, 8, HS]),
                                                        op=ALU.mult), reads=[sak, ("op", hf_, 2)], writes=[tk_])
                    kb.op(en, lambda e: e.tensor_tensor(out=S_[:], in0=S_[:], in1=T_[:], op=ALU.add), reads=[sk, tk_], writes=[sk])
                    kb.op(en, lambda e: e.tensor_tensor(out=T_[:], in0=Kk_[:], in1=vb, op=ALU.mult),
                          reads=["v_s", ("op", hf_, 3)], writes=[tk_])
                    kb.op(en, lambda e: e.tensor_tensor(out=S_[:], in0=S_[:], in1=T_[:], op=ALU.add), reads=[sk, tk_], writes=[sk])
                    kb.op(en, lambda e: e.tensor_tensor(out=T_[:], in0=S_[:], in1=R_[:], op=ALU.mult),
                          reads=[sk, ("op", hf_, 4)], writes=[tk_])
                    kb.op("dve", lambda e: e.tensor_reduce(out=y_sT[:, :, r0:r0 + HALF].rearrange("p g s -> p s g"), in_=T_[:],
                                                        axis=AX.X, op=ALU.add), reads=[tk_], writes=["y_sT"])
                for hp_ in range(2):
                    kb.dma("sp", o_wkv_s[s0:s0 + HALF].rearrange("s (g a) v k -> a v s g k", a=2)[hp_],
                           S_[hp_ * 64:(hp_ + 1) * 64, :, :, :], reads=[sk], writes=["o_wkv_s"])
            gn_out(y_sT, bon_s, g_s, NTS, SEQ, ["y_sT", "bon", "g16"], dict(psR=psR2, nxt=nxt2, tp=tp2, yib=yib2))
            if "YI_s" in dbg:
                kb.dma("sp", dbg["YI_s"], YI_d[:, SEQ:TT], reads=[("YI", SEQ)])
        if stop_after == "C2":
            kb.finish("sp"); return nc
        kb.barrier()

        X1_d = dt_scr("X1_d", [D, TT])
        HF_d = dt_scr("HF_d", [D, TT], BF16)
        GT_d = dt_scr("GT_d", [NE, TT], BF16)
        rb_row = nc.dram_tensor("rbias_row", [1, NE], F32, kind="ExternalInput").ap()
        sc_raw = nc.dram_tensor("sc_raw", [NS, 30, D], F32, kind="ExternalInput").ap()
        with ExitStack() as ed:
            sbd = lambda n, sh, dt=F32: ed.enter_context(nc.sbuf_tensor(n, list(sh), dt))
            psd = lambda n, sh, dt=F32: ed.enter_context(nc.psum_tensor(n, list(sh), dt))
            wor = sbd("wor", [128, 8, D], BF16); wco = sbd("wco", [128, 8, D], BF16); wou = sbd("wou", [128, 8, D], BF16)
            wr = sbd("wr", [128, 8, NE])
            rbt = sbd("rbt", [128, NE])
            gl = [sbd("gl%d" % i, [128, 544]) for i in range(2)]
            glb = [sbd("glb%d" % i, [128, 544], BF16) for i in range(2)]
            dg = [sbd("dg%d" % i, [128, CW, 128], BF16) for i in range(2)]
            dwT = sbd("dwT", [128, 8, 512]); sqd = sbd("sqd", [128, 8, 512])
            uT = sbd("uT", [128, 8, 512], BF16); yiT = sbd("yiT", [128, 8, 512], BF16); mT = sbd("mT", [128, 8, 512], BF16)
            zT = sbd("zT", [128, 8, 512]); xTd = sbd("xTd", [128, 8, 512]); hfT = sbd("hfT", [128, 8, 512], BF16)
            sga = [sbd("sga%d" % i, [128, 512], BF16) for i in range(2)]
            sgb = [sbd("sgb%d" % i, [128, 512], BF16) for i in range(2)]
            t1 = sbd("t1", [128, 512]); t2 = sbd("t2", [128, 512]); rs = sbd("rs", [128, 512])
            cvrow = sbd("cvrow", [128, D])
            sc = sbd("sc", [128, NE]); bsd = sbd("bsd", [128, NE]); top8 = sbd("top8", [128, 8]); ssum = sbd("ssum", [128, 1])
            gtb = sbd("gtb", [NE, 128], BF16)
            psD = [psd("psD%d" % i, [128, 512]) for i in range(6)]
            psX = psd("psX", [128, D])
            dcount = [0]
            def nxd():
                dcount[0] += 1
                return dcount[0] % 6
            for (wt, src, nm) in [(wor, w_o_rwkv, "wor"), (wco, w_conv_out, "wco"), (wou, w_out, "wou")]:
                for hh in range(2):
                    kb.dma("pool", wt[:, :, hh * 512:(hh + 1) * 512],
                           src[:, hh * 512:(hh + 1) * 512].rearrange("(k p) n -> p k n", p=128), writes=[nm])
            kb.dma("sp", wr[:], w_router.rearrange("(k p) n -> p k n", p=128), writes=["wr"])
            kb.dma("sp", rbt[:], rb_row.partition_broadcast(128), writes=["rbt"])

            def stats_rstd(src, n, eps, div, skey):
                kb.op("act", lambda e: e.activation(out=sqd[:, :, 0:n], in_=src[:, :, 0:n], func=AF.Square), reads=[skey], writes=["sqd"])
                p_ = nxd()
                kb.op("dve", lambda e: e.tensor_reduce(out=t2[:, 0:n], in_=sqd[:, :, 0:n].rearrange("p k n -> p n k"), axis=AX.X, op=ALU.add),
                      reads=["sqd"], writes=["t2"])
                kb.mm_group([lambda e, p_=p_: e.matmul(psD[p_][:, 0:n], lhsT=cc("ones"), rhs=t2[:, 0:n], start=True, stop=True)],
                            reads=["t2", "C"], writes=[("psD", p_)])
                kb.op("act", lambda e, p_=p_: e.activation(out=rs[:, 0:n], in_=psD[p_][:, 0:n], func=AF.Ln, scale=1.0 / div, bias=eps),
                      reads=[("psD", p_)], writes=["rs"])
                kb.op("act", lambda e: e.activation(out=rs[:, 0:n], in_=rs[:, 0:n], func=AF.Exp, scale=-0.5), reads=["rs"], writes=["rs"])

            for bi, (c0, n, sample) in enumerate(BLOCKS):
                wd = NS if sample else 1
                hist = 30 * wd
                gcl = gcol(c0, sample)
                for j in range(8):
                    g_ = gl[j % 2]
                    gk = ("gl", j % 2)
                    kb.dma("sp", g_[:, 0:hist + n], G_d[j * 128:(j + 1) * 128, gcl - hist:gcl + n],
                           reads=[("G", bi), "G_hist"] + ([("G", bi - 1)] if (bi > 0 and not sample) else []), writes=[gk])
                    ds_ = j % 2
                    kb.op("act", lambda e, g_=g_, ds_=ds_: e.activation(out=glb[ds_][:, 0:hist + n], in_=g_[:, 0:hist + n], func=AF.Copy),
                          reads=[gk], writes=[("glb", ds_)])
                    kb.op("dve", lambda e, j=j, ds_=ds_: e.tensor_tensor(
                        out=dg[ds_][:], in0=cc("ident").unsqueeze(1).to_broadcast([128, CW, 128]),
                        in1=pc(P, "dw_w", j * CW, CW).unsqueeze(2).to_broadcast([128, CW, 128]), op=ALU.mult),
                        reads=["C", "P"], writes=[("dg", ds_)])
                    pcv = nxd()
                    kb.mm_group([(lambda e, tap=tap, pcv=pcv, ds_=ds_: e.matmul(psD[pcv][:, 0:n], lhsT=dg[ds_][:, tap, :],
                                                                                rhs=glb[ds_][:, tap * wd:tap * wd + n],
                                                                                start=(tap == 0), stop=(tap == CW - 1)))
                                 for tap in range(CW)], reads=[("dg", ds_), ("glb", ds_)], writes=[("psD", pcv)])
                    kb.op("act", lambda e, pcv=pcv, j=j: e.activation(out=dwT[:, j, 0:n], in_=psD[pcv][:, 0:n], func=AF.Identity,
                                                                      bias=pc(P, "dw_b", j)), reads=[("psD", pcv), "P"], writes=[("dwT", j)])
                    if bi == 3:
                        kb.mm_group([lambda e, g_=g_, j=j: e.transpose(out=psX[0:32, j * 128:(j + 1) * 128], in_=g_[:, 510:542],
                                                                       identity=cc("ident"))], reads=[gk, "C"], writes=["psX"])
                    if sample:
                        kb.mm_group([lambda e, g_=g_, j=j: e.transpose(out=psX[0:NTS, j * 128:(j + 1) * 128], in_=g_[:, hist:hist + NTS],
                                                                       identity=cc("ident"))], reads=[gk, "C"], writes=["psX"])
                if sample:
                    kb.op("act", lambda e: e.activation(out=cvrow[0:NTS, :], in_=psX[0:NTS, :], func=AF.Copy), reads=["psX"], writes=["cvrow"])
                    for t in range(TS):
                        kb.dma("sp", o_cv_s[:, 26 + t, :], cvrow[t * NS:(t + 1) * NS, :], reads=["cvrow"], writes=["o_cv_s"])
                    kb.dma("sp", o_cv_s[:, 0:26, :], sc_raw[:, 4:30, :], writes=["o_cv_s_h"])
                if bi == 3:
                    kb.op("act", lambda e: e.activation(out=cvrow[0:32, :], in_=psX[0:32, :], func=AF.Copy), reads=["psX"], writes=["cvrow"])
                    kb.dma("sp", o_cv_p, cvrow[2:32, :], reads=["cvrow"], writes=["o_cv_p"])
                dkeys = [("dwT", j) for j in range(8)]
                pm_ = nxd()
                kb.op("dve", lambda e: e.tensor_reduce(out=t2[:, 0:n], in_=dwT[:, :, 0:n].rearrange("p k n -> p n k"), axis=AX.X, op=ALU.add),
                      reads=dkeys, writes=["t2"])
                kb.mm_group([lambda e, pm_=pm_: e.matmul(psD[pm_][:, 0:n], lhsT=cc("ones"), rhs=t2[:, 0:n], start=True, stop=True)],
                            reads=["t2", "C"], writes=[("psD", pm_)])
                kb.op("dve", lambda e, pm_=pm_: e.tensor_scalar_mul(out=t1[:, 0:n], in0=psD[pm_][:, 0:n], scalar1=-1.0 / D),
                      reads=[("psD", pm_)], writes=["t1"])
                kb.op("dve", lambda e: e.tensor_tensor(out=dwT[:, :, 0:n], in0=dwT[:, :, 0:n],
                                                      in1=t1[:, 0:n].unsqueeze(1).to_broadcast([128, 8, n]), op=ALU.add),
                      reads=dkeys + ["t1"], writes=dkeys)
                stats_rstd(dwT, n, 1e-5, D, ("dwT", 0))
                kb.op("dve", lambda e: e.tensor_tensor(out=dwT[:, :, 0:n], in0=dwT[:, :, 0:n],
                                                      in1=rs[:, 0:n].unsqueeze(1).to_broadcast([128, 8, n]), op=ALU.mult),
                      reads=dkeys + ["rs", "sqd"], writes=dkeys)
                for j in range(8):
                    kb.op("act", lambda e, j=j: e.activation(out=uT[:, j, 0:n], in_=dwT[:, j, 0:n], func=AF.Silu,
                                                             scale=pc(P, "ln_g", j), bias=pc(P, "ln_b", j)),
                          reads=[("dwT", j), "P"], writes=["uT"])
                if "uT" in dbg and bi == 0:
                    kb.dma("sp", dbg["uT"], uT[:], reads=["uT"])
                kb.dma("sp", yiT[:, :, 0:n], YI_d[:, c0:c0 + n].rearrange("(k p) c -> p k c", p=128), reads=[("YI", c0)], writes=["yiT"])
                for o in range(8):
                    oc = slice(o * 128, (o + 1) * 128)
                    pa_, pb_ = nxd(), nxd()
                    kb.mm_group([(lambda e, k=k, pa_=pa_: e.matmul(psD[pa_][:, 0:n], lhsT=wor[:, k, oc], rhs=yiT[:, k, 0:n],
                                                                  start=(k == 0), stop=(k == 7))) for k in range(8)],
                                reads=["wor", "yiT"], writes=[("psD", pa_)])
                    kb.mm_group([(lambda e, k=k, pb_=pb_: e.matmul(psD[pb_][:, 0:n], lhsT=wco[:, k, oc], rhs=uT[:, k, 0:n],
                                                                  start=(k == 0), stop=(k == 7))) for k in range(8)],
                                reads=["wco", "uT"], writes=[("psD", pb_)])
                    s_ = o % 2
                    kb.dma("sp", sga[s_][:, 0:n], SG_d[o * 128:(o + 1) * 128, c0:c0 + n], reads=[("SG", bi)], writes=[("sga", s_)])
                    kb.dma("sp", sgb[s_][:, 0:n], SG_d[D + o * 128:D + (o + 1) * 128, c0:c0 + n], reads=[("SG", bi)], writes=[("sgb", s_)])
                    kb.op("dve", lambda e, pa_=pa_, s_=s_: e.tensor_tensor(out=t1[:, 0:n], in0=psD[pa_][:, 0:n], in1=sga[s_][:, 0:n], op=ALU.mult),
                          reads=[("psD", pa_), ("sga", s_)], writes=["t1"])
                    kb.op("dve", lambda e, pb_=pb_, s_=s_: e.tensor_tensor(out=t2[:, 0:n], in0=psD[pb_][:, 0:n], in1=sgb[s_][:, 0:n], op=ALU.mult),
                          reads=[("psD", pb_), ("sgb", s_)], writes=["t2"])
                    kb.op("dve", lambda e, o=o: e.tensor_tensor(out=mT[:, o, 0:n], in0=t1[:, 0:n], in1=t2[:, 0:n], op=ALU.add),
                          reads=["t1", "t2"], writes=["mT"])
                if "mT" in dbg and bi == 0:
                    kb.dma("sp", dbg["mT"], mT[:], reads=["mT"])
                for o in range(8):
                    oc = slice(o * 128, (o + 1) * 128)
                    pz = nxd()
                    kb.mm_group([(lambda e, k=k, pz=pz: e.matmul(psD[pz][:, 0:n], lhsT=wou[:, k, oc], rhs=mT[:, k, 0:n],
                                                                start=(k == 0), stop=(k == 7))) for k in range(8)],
                                reads=["wou", "mT"], writes=[("psD", pz)])
                    kb.op("act", lambda e, pz=pz, o=o: e.activation(out=zT[:, o, 0:n], in_=psD[pz][:, 0:n], func=AF.Copy),
                          reads=[("psD", pz)], writes=["zT"])
                stats_rstd(zT, n, 1e-6, D, "zT")
                kb.dma("sp", xTd[:, :, 0:n], XT_d[:, c0:c0 + n].rearrange("(k p) c -> p k c", p=128), reads=[("XT", bi)], writes=["xTd"])
                kb.op("dve", lambda e: e.tensor_tensor(out=zT[:, :, 0:n], in0=zT[:, :, 0:n],
                                                      in1=rs[:, 0:n].unsqueeze(1).to_broadcast([128, 8, n]), op=ALU.mult),
                      reads=["zT", "rs", "sqd"], writes=["zT"])
                kb.op("dve", lambda e: e.tensor_tensor(out=view(zT[:, :, 0:n], sample), in0=view(zT[:, :, 0:n], sample),
                                                      in1=bcast_mod(Gm, 0, sample, n), op=ALU.mult), reads=["zT", "Gm"], writes=["zT"])
                kb.op("dve", lambda e: e.tensor_tensor(out=xTd[:, :, 0:n], in0=xTd[:, :, 0:n], in1=zT[:, :, 0:n], op=ALU.add),
                      reads=["zT", "xTd"], writes=["xTd"])
                kb.dma("sp", X1_d[:, c0:c0 + n].rearrange("(k p) c -> p k c", p=128), xTd[:, :, 0:n], reads=["xTd"], writes=[("X1", bi)])
                stats_rstd(xTd, n, 1e-6, D, "xTd")
                kb.op("dve", lambda e: e.tensor_tensor(out=zT[:, :, 0:n], in0=xTd[:, :, 0:n],
                                                      in1=rs[:, 0:n].unsqueeze(1).to_broadcast([128, 8, n]), op=ALU.mult),
                      reads=["xTd", "rs", "zT", "sqd"], writes=["zT"])
                kb.op("dve", lambda e: e.tensor_tensor(out=view(zT[:, :, 0:n], sample), in0=view(zT[:, :, 0:n], sample),
                                                      in1=bcast_mod(Af, 0, sample, n), op=ALU.mult), reads=["zT", "Af"], writes=["zT"])
                kb.op("dve", lambda e: e.tensor_tensor(out=view(zT[:, :, 0:n], sample), in0=view(zT[:, :, 0:n], sample),
                                                      in1=bcast_mod(modT, 24, sample, n), op=ALU.add), reads=["zT", "modT"], writes=["zT"])
                kb.op("act", lambda e: e.activation(out=hfT[:, :, 0:n], in_=zT[:, :, 0:n], func=AF.Copy), reads=["zT"], writes=["hfT"])
                kb.dma("sp", HF_d[:, c0:c0 + n].rearrange("(k p) c -> p k c", p=128), hfT[:, :, 0:n], reads=["hfT"], writes=[("HF", bi)])
                m_ = 64 if sample else 128
                for i in range(n // m_):
                    tc_ = slice(i * m_, (i + 1) * m_)
                    pr_ = nxd()
                    kb.mm_group([(lambda e, k=k, pr_=pr_: e.matmul(psD[pr_][0:m_, 0:NE], lhsT=zT[:, k, tc_], rhs=wr[:, k, :],
                                                                  start=(k == 0), stop=(k == 7))) for k in range(8)],
                                reads=["zT", "wr"], writes=[("psD", pr_)])
                    kb.op("act", lambda e, pr_=pr_: e.activation(out=sc[0:m_, :], in_=psD[pr_][0:m_, 0:NE], func=AF.Sigmoid),
                          reads=[("psD", pr_)], writes=["sc"])
                    kb.op("dve", lambda e: e.tensor_tensor(out=bsd[0:m_, :], in0=sc[0:m_, :], in1=rbt[0:m_, :], op=ALU.add),
                          reads=["sc", "rbt"], writes=["bsd"])
                    kb.op("dve", lambda e: e.max(out=top8[0:m_, :], in_=bsd[0:m_, :]), reads=["bsd"], writes=["top8"])
                    kb.op("dve", lambda e: e.tensor_scalar(out=bsd[0:m_, :], in0=bsd[0:m_, :], scalar1=top8[0:m_, 5:6], scalar2=None,
                                                          op0=ALU.is_ge), reads=["bsd", "top8"], writes=["bsd"])
                    kb.op("dve", lambda e: e.tensor_tensor(out=sc[0:m_, :], in0=sc[0:m_, :], in1=bsd[0:m_, :], op=ALU.mult),
                          reads=["sc", "bsd"], writes=["sc"])
                    kb.op("dve", lambda e: e.tensor_reduce(out=ssum[0:m_, :], in_=sc[0:m_, :], axis=AX.X, op=ALU.add),
                          reads=["sc"], writes=["ssum"])
                    kb.op("dve", lambda e: e.reciprocal(out=ssum[0:m_, :], in_=ssum[0:m_, :]), reads=["ssum"], writes=["ssum"])
                    kb.op("dve", lambda e: e.tensor_scalar(out=sc[0:m_, :], in0=sc[0:m_, :], scalar1=ssum[0:m_, 0:1], scalar2=2.5,
                                                          op0=ALU.mult, op1=ALU.mult), reads=["sc", "ssum"], writes=["sc"])
                    pt_ = nxd()
                    kb.mm_group([lambda e, pt_=pt_: e.transpose(out=psD[pt_][0:NE, 0:m_], in_=sc[0:m_, :], identity=cc("ident")[0:m_, 0:m_])],
                                reads=["sc", "C"], writes=[("psD", pt_)])
                    kb.op("act", lambda e, pt_=pt_: e.activation(out=gtb[:, 0:m_], in_=psD[pt_][0:NE, 0:m_], func=AF.Copy),
                          reads=[("psD", pt_)], writes=["gtb"])
                    kb.dma("sp", GT_d[:, c0 + i * m_:c0 + (i + 1) * m_], gtb[:, 0:m_], reads=["gtb"], writes=[("GT", bi)])
            if "X1_d" in dbg:
                kb.dma("sp", dbg["X1_d"], X1_d, reads=[("X1", b) for b in range(5)])
            if "HF_d" in dbg:
                kb.dma("sp", dbg["HF_d"], HF_d, reads=[("HF", b) for b in range(5)])
            if "GT_d" in dbg:
                kb.dma("sp", dbg["GT_d"], GT_d, reads=[("GT", b) for b in range(5)])
        if stop_after == "DE":
            kb.finish("sp"); return nc
        kb.barrier()

        sel_in = nc.dram_tensor("sel", [NE, NE * 128], F32, kind="ExternalInput").ap()
        TB = [(i * 512, 512) for i in range(4)] + [(SEQ, NTS)]
        with ExitStack() as ef:
            sbf = lambda n, sh, dt=F32: ef.enter_context(nc.sbuf_tensor(n, list(sh), dt))
            psf = lambda n, sh, dt=F32: ef.enter_context(nc.psum_tensor(n, list(sh), dt))
            hfA = sbf("hfA", [128, 8, TT], BF16)
            gTA = sbf("gTA", [NE, TT], BF16)
            acc = sbf("acc", [128, 8, TT])
            selt = [sbf("selt%d" % i, [NE, 128], BF16) for i in range(3)]
            NWB = 3
            ew = ExitStack()
            sbw = lambda n, sh, dt=F32: ew.enter_context(nc.sbuf_tensor(n, list(sh), dt))
            wg_ = [sbw("wxg%d" % i, [128, 8, DE], BF16) for i in range(NWB)]
            wu_ = [sbw("wxu%d" % i, [128, 8, DE], BF16) for i in range(NWB)]
            wd_ = [sbw("wxd%d" % i, [128, 2, D], BF16) for i in range(NWB)]
            sG = [sbw("sG%d" % i, [128, 512]) for i in range(2)]
            gsb = [sbw("gsb%d" % i, [128, 512]) for i in range(2)]
            he32 = [sbw("he32_%d" % i, [128, 512]) for i in range(2)]
            heT2 = [[[sbw("heT%d_%d_%d" % (hb, g, mi), [128, 512], BF16) for mi in range(2)] for g in range(2)] for hb in range(2)]
            psM = [psf("psM%d" % i, [128, 512]) for i in range(4)]
            psO = [psf("psO%d" % i, [128, 512]) for i in range(4)]
            mcount = [0]
            def nxm():
                mcount[0] += 1
                return mcount[0] % 4
            kb.dma("sp", hfA[:], HF_d.rearrange("(k p) c -> p k c", p=128), reads=[("HF", b) for b in range(5)], writes=["hfA"])
            kb.dma("sp", gTA[:], GT_d, reads=[("GT", b) for b in range(5)], writes=["gTA"])
            kb.op("pool", lambda e: e.memset(acc[:], 0.0), writes=["acc"])
            NEX = NE + 1
            groups = [(2 * g, min(2 * g + 2, NEX)) for g in range((NEX + 1) // 2)]
            def load_w(e):
                sl = e % NWB
                if e < NE:
                    kb.dma("pool", selt[e % 3][:], sel_in[:, e * 128:(e + 1) * 128], writes=[("selt", e % 3)])
                kb.dma("pool", wg_[sl][:], w_eg[e].rearrange("(k p) n -> p k n", p=128), writes=[("wg", sl)])
                kb.dma("pool", wu_[sl][:], w_eu[e].rearrange("(k p) n -> p k n", p=128), writes=[("wu", sl)])
                kb.dma("pool", wd_[sl][:], w_ed[e].rearrange("(m p) n -> p m n", p=128), writes=[("wd", sl)])
            e_loaded = 0
            for e in range(min(NWB, NEX)):
                load_w(e); e_loaded += 1
            tcount = 0
            bcount = 0
            pending = None
            for (ea, eb) in groups:
                for (c0, n) in TB:
                    cs_ = slice(c0, c0 + n)
                    hb = bcount % 2
                    bcount += 1
                    heT = heT2[hb]
                    for gi, e in enumerate(range(ea, eb)):
                        sl = e % NWB
                        if e < NE:
                            pgt = nxm()
                            kb.mm_group([lambda e_, e=e, pgt=pgt: e_.matmul(psM[pgt][:, 0:n], lhsT=selt[e % 3][:, :],
                                                                           rhs=gTA[:, cs_], start=True, stop=True)],
                                        reads=[("selt", e % 3), "gTA"], writes=[("psM", pgt)])
                            gsl = e % 2
                            kb.op("act", lambda e_, pgt=pgt, gsl=gsl: e_.activation(out=gsb[gsl][:, 0:n], in_=psM[pgt][:, 0:n], func=AF.Copy),
                                  reads=[("psM", pgt)], writes=[("gsb", gsl)])
                        for mi in range(2):
                            mc = slice(mi * 128, (mi + 1) * 128)
                            pg_, pu_ = nxm(), nxm()
                            kb.mm_group([(lambda e_, k=k, pg_=pg_, sl=sl: e_.matmul(psM[pg_][:, 0:n], lhsT=wg_[sl][:, k, mc], rhs=hfA[:, k, cs_],
                                                                                   start=(k == 0), stop=(k == 7))) for k in range(8)],
                                        reads=[("wg", sl), "hfA"], writes=[("psM", pg_)])
                            kb.mm_group([(lambda e_, k=k, pu_=pu_, sl=sl: e_.matmul(psM[pu_][:, 0:n], lhsT=wu_[sl][:, k, mc], rhs=hfA[:, k, cs_],
                                                                                   start=(k == 0), stop=(k == 7))) for k in range(8)],
                                        reads=[("wu", sl), "hfA"], writes=[("psM", pu_)])
                            ts_ = tcount % 2
                            tcount += 1
                            kb.op("act", lambda e_, pg_=pg_, ts_=ts_: e_.activation(out=sG[ts_][:, 0:n], in_=psM[pg_][:, 0:n], func=AF.Silu),
                                  reads=[("psM", pg_)], writes=[("sG", ts_)])
                            if e < NE:
                                kb.op("dve", lambda e_, pu_=pu_, ts_=ts_: e_.tensor_tensor(out=he32[ts_][:, 0:n], in0=psM[pu_][:, 0:n],
                                                                                         in1=sG[ts_][:, 0:n], op=ALU.mult),
                                      reads=[("psM", pu_), ("sG", ts_)], writes=[("he32", ts_)])
                                kb.op("pool", lambda e_, gsl=gsl, ts_=ts_, gi=gi, mi=mi: e_.tensor_tensor(
                                    out=heT[gi][mi][:, 0:n], in0=gsb[gsl][:, 0:n], in1=he32[ts_][:, 0:n], op=ALU.mult),
                                    reads=[("gsb", gsl), ("he32", ts_)], writes=[("heT", hb, gi, mi)])
                            else:
                                kb.op("dve", lambda e_, pu_=pu_, ts_=ts_, gi=gi, mi=mi: e_.tensor_tensor(
                                    out=heT[gi][mi][:, 0:n], in0=psM[pu_][:, 0:n], in1=sG[ts_][:, 0:n], op=ALU.mult),
                                    reads=[("psM", pu_), ("sG", ts_)], writes=[("heT", hb, gi, mi)])
                    def do_down(ea=ea, eb=eb, c0=c0, n=n, cs_=cs_, hb=hb, heT=heT):
                        ng = eb - ea
                        for op_ in range(2):
                            for oo in range(4):
                                o = op_ * 4 + oo
                                oc = slice(o * 128, (o + 1) * 128)
                                fns = []
                                cnt = 0
                                for gi, e in enumerate(range(ea, eb)):
                                    sl = e % NWB
                                    for mi in range(2):
                                        first, lastm = (cnt == 0), (cnt == 2 * ng - 1)
                                        cnt += 1
                                        fns.append(lambda e_, sl=sl, mi=mi, gi=gi, oo=oo, first=first, lastm=lastm: e_.matmul(
                                            psO[oo][:, 0:n], lhsT=wd_[sl][:, mi, oc], rhs=heT[gi][mi][:, 0:n], start=first, stop=lastm))
                                kb.mm_group(fns, reads=[("wd", e % NWB) for e in range(ea, eb)] + [("heT", hb, gi, mi) for gi in range(ng) for mi in range(2)],
                                            writes=[("psO", oo)])
                                en = "dve" if oo % 2 == 0 else "dve"
                                kb.op(en, lambda e_, o=o, oo=oo: e_.tensor_tensor(out=acc[:, o, cs_], in0=psO[oo][:, 0:n], in1=acc[:, o, cs_], op=ALU.add),
                                      reads=[("psO", oo), "acc"], writes=["acc"])
                    if pending is not None:
                        pending()
                    pending = do_down
                if pending is not None:
                    pending()
                    pending = None
                for e in range(ea, eb):
                    if e_loaded < NEX:
                        load_w(e_loaded); e_loaded += 1
            if "acc" in dbg:
                kb.dma("sp", dbg["acc"], acc[:], reads=["acc"])
            ew.close()
            kb.barrier()
            sqf = sbf("sqf", [128, 8, 512]); x1f = sbf("x1f", [128, 8, 512]); rsf = sbf("rsf", [128, 512])
            yrow = [sbf("yrow%d" % i, [128, D]) for i in range(2)]
            for bi, (c0, n, sample) in enumerate(BLOCKS):
                cs_ = slice(c0, c0 + n)
                kb.op("act", lambda e: e.activation(out=sqf[:, :, 0:n], in_=acc[:, :, cs_], func=AF.Square), reads=["acc"], writes=["sqf"])
                p_ = nxm()
                kb.op("dve", lambda e: e.tensor_reduce(out=rsf[:, 0:n], in_=sqf[:, :, 0:n].rearrange("p k n -> p n k"), axis=AX.X, op=ALU.add),
                      reads=["sqf"], writes=["rsf"])
                kb.mm_group([lambda e, p_=p_: e.matmul(psM[p_][:, 0:n], lhsT=cc("ones"), rhs=rsf[:, 0:n], start=True, stop=True)],
                            reads=["rsf", "C"], writes=[("psM", p_)])
                kb.op("act", lambda e, p_=p_: e.activation(out=rsf[:, 0:n], in_=psM[p_][:, 0:n], func=AF.Ln, scale=1.0 / D, bias=1e-6),
                      reads=[("psM", p_)], writes=["rsf"])
                kb.op("act", lambda e: e.activation(out=rsf[:, 0:n], in_=rsf[:, 0:n], func=AF.Exp, scale=-0.5), reads=["rsf"], writes=["rsf"])
                kb.dma("sp", x1f[:, :, 0:n], X1_d[:, cs_].rearrange("(k p) c -> p k c", p=128), reads=[("X1", bi)], writes=["x1f"])
                kb.op("dve", lambda e: e.tensor_tensor(out=sqf[:, :, 0:n], in0=acc[:, :, cs_],
                                                      in1=rsf[:, 0:n].unsqueeze(1).to_broadcast([128, 8, n]), op=ALU.mult),
                      reads=["acc", "rsf", "sqf"], writes=["sqf"])
                kb.op("dve", lambda e: e.tensor_tensor(out=view(sqf[:, :, 0:n], sample), in0=view(sqf[:, :, 0:n], sample),
                                                      in1=bcast_mod(Gf, 0, sample, n), op=ALU.mult), reads=["sqf", "Gf"], writes=["sqf"])
                kb.op("dve", lambda e: e.tensor_tensor(out=x1f[:, :, 0:n], in0=x1f[:, :, 0:n], in1=sqf[:, :, 0:n], op=ALU.add),
                      reads=["sqf", "x1f"], writes=["x1f"])
                m_ = 64 if sample else 128
                for i in range(n // m_):
                    ys_ = i % 2
                    for hh in range(2):
                        pt_ = psO[(2 * i + hh) % 4]
                        ptk = ("psO", (2 * i + hh) % 4)
                        kb.mm_group([(lambda e, k=k, pt_=pt_: e.transpose(out=pt_[0:m_, (k % 4) * 128:(k % 4) * 128 + 128],
                                                                          in_=x1f[:, k, i * m_:(i + 1) * m_], identity=cc("ident")))
                                     for k in range(4 * hh, 4 * hh + 4)], reads=["x1f", "C"], writes=[ptk])
                        kb.op("act" if hh == 0 else "dve",
                              (lambda e, pt_=pt_, hh=hh, ys_=ys_: e.activation(out=yrow[ys_][0:m_, hh * 512:(hh + 1) * 512], in_=pt_[0:m_, :], func=AF.Copy))
                              if hh == 0 else
                              (lambda e, pt_=pt_, hh=hh, ys_=ys_: e.tensor_copy(out=yrow[ys_][0:m_, hh * 512:(hh + 1) * 512], in_=pt_[0:m_, :])),
                              reads=[ptk], writes=[("yrow", ys_, hh)])
                    if not sample:
                        kb.dma("sp", y_p[c0 + i * 128:c0 + (i + 1) * 128, :], yrow[ys_][:, :], reads=[("yrow", ys_, 0), ("yrow", ys_, 1)],
                               writes=["y_p"])
                    else:
                        for t in range(TS):
                            kb.dma("sp", y_s[:, t, :], yrow[ys_][t * NS:(t + 1) * NS, :], reads=[("yrow", ys_, 0), ("yrow", ys_, 1)],
                                   writes=["y_s"])
        kb.finish("sp")
    return nc


def _prep_inputs(inp):
    f = lambda a: np.ascontiguousarray(np.asarray(a, dtype=np.float32))
    def col8(v):
        return f(v).reshape(-1, 128).T
    prm = np.zeros((128, NPC), np.float32)
    def put(name, arr):
        o, c = PCOLS[name]
        prm[:arr.shape[0], o:o + arr.shape[1]] = arr
    put("b_ada", f(inp["b_ada"]).reshape(48, 128).T)
    for n, k in [("mix_pre_g", "mix_pre_g"), ("mix_post_g", "mix_post_g"), ("ffn_pre_g", "ffn_pre_g"),
                 ("ffn_post_g", "ffn_post_g"), ("w0", "w0"), ("a0", "a0"), ("k_k", "k_k"), ("k_a", "k_a"),
                 ("gn_g", "gn_g"), ("gn_b", "gn_b"), ("dw_b", "dw_b"), ("ln_g", "conv_ln_g"), ("ln_b", "conv_ln_b")]:
        put(n, col8(inp[k]))
    put("r_k", col8(f(inp["r_k"]).reshape(-1)))
    mu = np.zeros(27 * 128, np.float32); mu[:NSHIFT] = f(inp["mu_shift"])
    put("mu", mu.reshape(27, 128).T)
    dww = f(inp["dw_w"])
    put("dw_w", dww.T.reshape(8, 128, CW).transpose(1, 0, 2).reshape(128, 8 * CW))
    rb = np.zeros((128, 1), np.float32)
    put("rbias", rb)
    cst = _consts()
    w_eg = np.concatenate([f(inp["w_exp_gate"]), f(inp["w_sh_gate"])[None]], 0)
    w_eu = np.concatenate([f(inp["w_exp_up"]), f(inp["w_sh_up"])[None]], 0)
    w_ed = np.concatenate([f(inp["w_exp_down"]), f(inp["w_sh_down"])[None]], 0)
    shared = {"sel": _sel(), "prm": prm, "cst": cst, "w_ada": f(inp["w_ada"]), "w_in": f(inp["w_in"]),
              "w_decay2": f(inp["w_decay2"]), "w_aaa2": f(inp["w_aaa2"]), "w_gate2": f(inp["w_gate2"]),
              "w_o_rwkv": f(inp["w_o_rwkv"]), "w_conv_out": f(inp["w_conv_out"]), "w_out": f(inp["w_out"]),
              "w_router": f(inp["w_router"]), "w_eg": w_eg, "w_eu": w_eu, "w_ed": w_ed,
              "rbias_row": f(inp["router_bias"]).reshape(1, NE)}
    maps = []
    for c in range(NCORES):
        sl = slice(c * NS, (c + 1) * NS)
        m = dict(shared)
        m["x_p"] = f(inp["x_prompt"][c])
        m["x_s"] = f(inp["x_sample"][sl])
        cTm = np.concatenate([f(inp["c_prompt"][c])[None], f(inp["c_sample"][sl])], 0).T
        m["cT"] = np.ascontiguousarray(cTm)
        m["shT"] = np.ascontiguousarray(f(inp["state_shift"][sl]).T)
        m["scT"] = np.ascontiguousarray(f(inp["state_conv"][sl]).transpose(2, 1, 0).reshape(D, 30 * NS))
        m["s_wkv"] = f(inp["state_wkv"][sl])
        m["sc_raw"] = f(inp["state_conv"][sl])
        maps.append(m)
    return maps


def kernel(**inputs):
    maps = _prep_inputs(inputs)
    nc = build()
    names = set(nc_input_names(nc))
    maps = [{k: v for k, v in m.items() if k in names} for m in maps]
    res = run_bass_kernel_spmd(nc, maps, core_ids=list(range(NCORES)))
    r = res.results
    y_p = np.stack([r[c]["y_p"] for c in range(NCORES)], 0)
    y_s = np.concatenate([r[c]["y_s"] for c in range(NCORES)], 0)
    wkv_p = np.stack([r[c]["o_wkv_p"] for c in range(NCORES)], 0)
    sh_p = np.concatenate([r[c]["o_sh_p"] for c in range(NCORES)], 0)
    cv_p = np.stack([r[c]["o_cv_p"] for c in range(NCORES)], 0)
    wkv_s = np.concatenate([r[c]["o_wkv_s"] for c in range(NCORES)], 0)
    sh_s = np.concatenate([r[c]["o_sh_s"] for c in range(NCORES)], 0)
    cv_s = np.concatenate([r[c]["o_cv_s"] for c in range(NCORES)], 0)
    return (y_p, y_s, wkv_p, sh_p, cv_p, wkv_s, sh_s, cv_s)


def nc_input_names(nc):
    return _IN_NAMES


_IN_NAMES = ["sc_raw", "sel", "rbias_row", "x_p", "x_s", "cT", "shT", "scT", "s_wkv", "prm", "cst", "w_ada", "w_in", "w_decay2", "w_aaa2",
             "w_gate2", "w_o_rwkv", "w_conv_out", "w_out", "w_router", "w_eg", "w_eu", "w_ed"]
```
